# Optimizing a Trainium2 kernel written in Bass

```python
import math
import jax, jax.numpy as jnp
from jax import lax
import numpy as np

D_MODEL = 2048
BATCH = 4
SEQ = 4096
DEPTH = 1

POOL_WIDTH = D_MODEL // 2
POOL_WINDOWS = (2, 4, 8, 16)
POOL_GROUPS = len(POOL_WINDOWS)
POOL_GROUP_DIM = POOL_WIDTH // POOL_GROUPS
SSM_WIDTH = D_MODEL // 2
SSM_GROUP_DIM = 16
SSM_GROUPS = SSM_WIDTH // SSM_GROUP_DIM
SSM_STATE = 64
DT_MIN = 1e-3
DT_MAX = 1e-1
LAMBDA_RE_MAX = -1e-4
IN_WIDTH = POOL_WIDTH + SSM_WIDTH + 2 * D_MODEL
N_EXPERTS = 32
TOP_K = 4
D_EXPERT = D_MODEL
SWIGLU_ALPHA = 1.702
SWIGLU_LIMIT = 7.0
MOE_BLOCK = 256
N_ADA = 6
RMS_EPS = 1e-6

kernel_name = "pool_s5_gated_moe_adaln_block"


def rms_norm(x, g):
    xf = x.astype(jnp.float32)
    y = xf * lax.rsqrt(jnp.mean(xf * xf, axis=-1, keepdims=True) + RMS_EPS)
    return y * g.astype(jnp.float32)


def modulate(h, shift, scale):
    return h * (1.0 + scale[:, None, :]) + shift[:, None, :]


def pool_mixer(u, pool_w, pool_scale):
    b, s, _ = u.shape
    uf = u.astype(jnp.float32).reshape(b, s, POOL_GROUPS, POOL_GROUP_DIM)
    cs = jnp.cumsum(uf, axis=1)
    pos = jnp.arange(1, s + 1, dtype=jnp.float32)
    outs = []
    for g, w in enumerate(POOL_WINDOWS):
        c_g = cs[:, :, g]
        lower = jnp.pad(c_g[:, : s - w], ((0, 0), (w, 0), (0, 0)))
        count = jnp.minimum(pos, float(w))[None, :, None]
        outs.append((c_g - lower) / count - uf[:, :, g])
    pooled = jnp.stack(outs, axis=2)
    mixed = jnp.einsum('bsgc,gcd->bsgd', pooled, pool_w.astype(jnp.float32))
    return mixed.reshape(b, s, POOL_WIDTH) * pool_scale.astype(jnp.float32)


def _complex_linear_combine(left, right):
    a1r, a1i, b1r, b1i = left
    a2r, a2i, b2r, b2i = right
    ar = a2r * a1r - a2i * a1i
    ai = a2r * a1i + a2i * a1r
    br = a2r * b1r - a2i * b1i + b2r
    bi = a2r * b1i + a2i * b1r + b2i
    return (ar, ai, br, bi)


def s5_mixer(u, lam_re, lam_im, log_dt, b_re, b_im, c_re, c_im, d_skip):
    b, s, _ = u.shape
    f32 = jnp.float32
    uf = u.astype(f32).reshape(b, s, SSM_GROUPS, SSM_GROUP_DIM)
    dt = jnp.exp(log_dt.astype(f32))[:, None]
    lre = jnp.minimum(lam_re.astype(f32), LAMBDA_RE_MAX)
    lim = lam_im.astype(f32)
    mag = jnp.exp(lre * dt)
    ang = lim * dt
    ab_re = mag * jnp.cos(ang)
    ab_im = mag * jnp.sin(ang)
    den = lre * lre + lim * lim
    nr = ab_re - 1.0
    ni = ab_im
    f_re = (nr * lre + ni * lim) / den
    f_im = (ni * lre - nr * lim) / den
    br_, bi_ = b_re.astype(f32), b_im.astype(f32)
    bb_re = f_re[..., None] * br_ - f_im[..., None] * bi_
    bb_im = f_re[..., None] * bi_ + f_im[..., None] * br_
    bu_re = jnp.einsum('bsgh,gph->bsgp', uf, bb_re)
    bu_im = jnp.einsum('bsgh,gph->bsgp', uf, bb_im)
    a_re = jnp.broadcast_to(ab_re[None, None], (1, s, SSM_GROUPS, SSM_STATE))
    a_im = jnp.broadcast_to(ab_im[None, None], (1, s, SSM_GROUPS, SSM_STATE))
    _, _, xr, xi = lax.associative_scan(_complex_linear_combine, (a_re, a_im, bu_re, bu_im), axis=1)
    y = (jnp.einsum('bsgp,ghp->bsgh', xr, c_re.astype(f32))
         - jnp.einsum('bsgp,ghp->bsgh', xi, c_im.astype(f32)))
    y = y + d_skip.astype(f32).reshape(SSM_GROUPS, SSM_GROUP_DIM) * uf
    return y.reshape(b, s, SSM_WIDTH)


def hybrid_mixer(h, w_in, pool_w, pool_scale, w_pool_out, lam_re, lam_im, log_dt,
                 b_re, b_im, c_re, c_im, d_skip, w_glu, b_glu, w_out):
    z = h @ w_in
    u_pool, u_ssm, g_pool, g_ssm = jnp.split(
        z, [POOL_WIDTH, POOL_WIDTH + SSM_WIDTH, POOL_WIDTH + SSM_WIDTH + D_MODEL], axis=-1)
    y_pool = pool_mixer(u_pool, pool_w, pool_scale) @ w_pool_out
    y_s = jax.nn.gelu(s5_mixer(u_ssm, lam_re, lam_im, log_dt, b_re, b_im, c_re, c_im, d_skip))
    glu = y_s @ w_glu + b_glu
    glu_a, glu_b = jnp.split(glu, 2, axis=-1)
    y_ssm = glu_a * jax.nn.sigmoid(glu_b)
    merged = jax.nn.sigmoid(g_pool) * y_pool + jax.nn.sigmoid(g_ssm) * y_ssm
    return merged @ w_out


def moe_ffn(h, w_router, b_router, w1, b1, w2, b2):
    b, s, d = h.shape
    t = b * s
    hf = h.reshape(t, d)
    logits = hf.astype(jnp.float32) @ w_router.astype(jnp.float32) + b_router.astype(jnp.float32)
    top_val, top_idx = lax.top_k(logits, TOP_K)
    weights = jax.nn.softmax(top_val, axis=-1)
    n_assign = t * TOP_K
    flat_e = top_idx.reshape(-1).astype(jnp.int32)
    flat_t = jnp.repeat(jnp.arange(t, dtype=jnp.int32), TOP_K)
    flat_w = weights.reshape(-1)
    order = jnp.argsort(flat_e)
    se = flat_e[order]
    counts = jnp.zeros((N_EXPERTS,), jnp.int32).at[flat_e].add(1)
    starts = jnp.cumsum(counts) - counts
    pcounts = (counts + MOE_BLOCK - 1) // MOE_BLOCK * MOE_BLOCK
    pends = jnp.cumsum(pcounts)
    pstarts = pends - pcounts
    rank = jnp.arange(n_assign, dtype=jnp.int32) - starts[se]
    dest = pstarts[se] + rank
    n_blocks = -(-(n_assign + N_EXPERTS * (MOE_BLOCK - 1)) // MOE_BLOCK)
    n_pad = n_blocks * MOE_BLOCK
    buf_tok = jnp.zeros((n_pad,), jnp.int32).at[dest].set(flat_t[order])
    buf_w = jnp.zeros((n_pad,), jnp.float32).at[dest].set(flat_w[order])
    block_starts = jnp.arange(n_blocks, dtype=jnp.int32) * MOE_BLOCK
    block_e = jnp.minimum(jnp.searchsorted(pends, block_starts, side='right'), N_EXPERTS - 1)

    def run_block(args):
        tok, wgt, e = args
        xb = hf[tok]
        gu = xb @ w1[e] + b1[e]
        x_glu = jnp.minimum(gu[:, :D_EXPERT], SWIGLU_LIMIT)
        x_lin = jnp.clip(gu[:, D_EXPERT:], -SWIGLU_LIMIT, SWIGLU_LIMIT)
        act = x_glu * jax.nn.sigmoid(SWIGLU_ALPHA * x_glu) * (x_lin + 1.0)
        y = act @ w2[e] + b2[e]
        return y * wgt[:, None].astype(y.dtype)

    ys = lax.map(run_block, (buf_tok.reshape(n_blocks, MOE_BLOCK),
                             buf_w.reshape(n_blocks, MOE_BLOCK), block_e))
    out = jnp.zeros((t, d), ys.dtype).at[buf_tok].add(ys.reshape(n_pad, d))
    return out.reshape(b, s, d)


def setup_inputs(seed: int = 0) -> dict:
    key = jax.random.key(seed)
    ks = jax.random.split(key, 32)
    f32 = jnp.float32

    def nrm(k, shape, scale):
        return jax.random.normal(k, shape, f32) * scale

    L, D, G, P, H = DEPTH, D_MODEL, SSM_GROUPS, SSM_STATE, SSM_GROUP_DIM
    E, F = N_EXPERTS, D_EXPERT
    lam_im = (math.pi * jnp.arange(P, dtype=f32))[None, None, :] + nrm(ks[10], (L, G, P), 0.01)
    log_dt = jax.random.uniform(ks[11], (L, G), f32, math.log(DT_MIN), math.log(DT_MAX))
    return {
        "x": nrm(ks[0], (BATCH, SEQ, D), 1.0),
        "c": nrm(ks[1], (BATCH, D), 1.0),
        "ada_w": nrm(ks[2], (L, D, N_ADA * D), 0.3 * D ** -0.5),
        "ada_b": nrm(ks[3], (L, N_ADA * D), 0.02),
        "norm1_g": 1.0 + nrm(ks[4], (L, D), 0.02),
        "w_in": nrm(ks[5], (L, D, IN_WIDTH), D ** -0.5),
        "pool_w": nrm(ks[6], (L, POOL_GROUPS, POOL_GROUP_DIM, POOL_GROUP_DIM), POOL_GROUP_DIM ** -0.5),
        "pool_scale": 1.0 + nrm(ks[7], (L, POOL_WIDTH), 0.1),
        "w_pool_out": nrm(ks[8], (L, POOL_WIDTH, D), POOL_WIDTH ** -0.5),
        "ssm_lam_re": -0.5 + nrm(ks[9], (L, G, P), 0.01),
        "ssm_lam_im": lam_im,
        "ssm_log_dt": log_dt,
        "ssm_b_re": nrm(ks[12], (L, G, P, H), (2.0 * H) ** -0.5),
        "ssm_b_im": nrm(ks[13], (L, G, P, H), (2.0 * H) ** -0.5),
        "ssm_c_re": nrm(ks[14], (L, G, H, P), (2.0 * P) ** -0.5),
        "ssm_c_im": nrm(ks[15], (L, G, H, P), (2.0 * P) ** -0.5),
        "ssm_d": nrm(ks[16], (L, SSM_WIDTH), 1.0),
        "w_glu": nrm(ks[17], (L, SSM_WIDTH, 2 * D), SSM_WIDTH ** -0.5),
        "b_glu": nrm(ks[18], (L, 2 * D), 0.01),
        "w_out": nrm(ks[19], (L, D, D), D ** -0.5),
        "norm2_g": 1.0 + nrm(ks[20], (L, D), 0.02),
        "w_router": nrm(ks[21], (L, D, E), D ** -0.5),
        "b_router": nrm(ks[22], (L, E), 0.01),
        "w1": nrm(ks[23], (L, E, D, 2 * F), D ** -0.5),
        "b1": nrm(ks[24], (L, E, 2 * F), 0.01),
        "w2": nrm(ks[25], (L, E, F, D), F ** -0.5),
        "b2": nrm(ks[26], (L, E, D), 0.01),
        "final_ada_w": nrm(ks[27], (D, 2 * D), 0.3 * D ** -0.5),
        "final_ada_b": nrm(ks[28], (2 * D,), 0.02),
        "final_norm_g": 1.0 + nrm(ks[29], (D,), 0.02),
    }


def reference(x, c, ada_w, ada_b, norm1_g, w_in, pool_w, pool_scale, w_pool_out,
              ssm_lam_re, ssm_lam_im, ssm_log_dt, ssm_b_re, ssm_b_im, ssm_c_re, ssm_c_im, ssm_d,
              w_glu, b_glu, w_out, norm2_g, w_router, b_router, w1, b1, w2, b2,
              final_ada_w, final_ada_b, final_norm_g):
    c_act = jax.nn.silu(c.astype(jnp.float32))
    for l in range(DEPTH):
        mod = c_act @ ada_w[l] + ada_b[l]
        sh_m, sc_m, gt_m, sh_f, sc_f, gt_f = jnp.split(mod, N_ADA, axis=-1)
        h = modulate(rms_norm(x, norm1_g[l]), sh_m, sc_m)
        mix = hybrid_mixer(h, w_in[l], pool_w[l], pool_scale[l], w_pool_out[l],
                           ssm_lam_re[l], ssm_lam_im[l], ssm_log_dt[l], ssm_b_re[l], ssm_b_im[l],
                           ssm_c_re[l], ssm_c_im[l], ssm_d[l], w_glu[l], b_glu[l], w_out[l])
        x = x + (gt_m[:, None, :] * mix).astype(x.dtype)
        h2 = modulate(rms_norm(x, norm2_g[l]), sh_f, sc_f)
        ffn = moe_ffn(h2, w_router[l], b_router[l], w1[l], b1[l], w2[l], b2[l])
        x = x + (gt_f[:, None, :] * ffn).astype(x.dtype)
    fmod = c_act @ final_ada_w + final_ada_b
    sh_o, sc_o = jnp.split(fmod, 2, axis=-1)
    y = modulate(rms_norm(x, final_norm_g), sh_o, sc_o)
    return y.astype(x.dtype)
```

```python
import math
import numpy as np
import concourse.bass as bass
import concourse.mybir as mybir
from concourse.bass_utils import run_bass_kernel_spmd

F32 = mybir.dt.float32
BF16 = mybir.dt.bfloat16
AF = mybir.ActivationFunctionType
ALU = mybir.AluOpType

D = 2048
T = 2048
NT = 16
NE = 32
L1 = 16
NC1 = T // L1


class Slot:
    __slots__ = ("name", "w", "rs")

    def __init__(self, name=""):
        self.name = name
        self.w = None
        self.rs = []


class Op:
    __slots__ = ("eng", "fn", "deps", "need", "cnt", "dma", "dsem", "dcnt", "prev")

    def __init__(self, eng, fn, dma):
        self.eng = eng
        self.fn = fn
        self.deps = []
        self.need = False
        self.cnt = None
        self.dma = dma
        self.dsem = None
        self.dcnt = None
        self.prev = 0


class Prog:
    ENG = ("pe", "act", "dve", "pool", "sp")

    def __init__(self, nc, ndma=8):
        self.nc = nc
        self.ops = []
        self.last = {}
        self.dcur = {"sp": 0, "act": 0, "pool": 0}
        self.dlast = {}
        self.h = {"pe": nc.tensor, "act": nc.scalar, "dve": nc.vector, "pool": nc.gpsimd, "sp": nc.sync}
        self.ndma = ndma

    def op(self, eng, fn, reads=(), writes=(), dma=False):
        o = Op(eng, fn, dma)
        deps = set()
        for s in reads:
            if s.w is not None:
                deps.add(s.w)
        for s in writes:
            if s.w is not None:
                deps.add(s.w)
            for r in s.rs:
                deps.add(r)
        for d in deps:
            if (not d.dma) and (not dma) and d.eng == eng and eng == "pe":
                continue
            o.deps.append(d)
            d.need = True
        for s in reads:
            if dma:
                s.rs.append(o)
            else:
                s.rs = [r for r in s.rs if r.dma or r.eng != eng]
                s.rs.append(o)
        for s in writes:
            s.w = o
            s.rs = []
        self.ops.append(o)
        if dma:
            i = self.dcur[eng] % (3 if eng == "pool" else self.ndma)
            self.dcur[eng] += 1
            o.dsem = (eng, i)
            self.dlast[o.dsem] = o
        else:
            self.last[eng] = o
        return o

    def barrier(self):
        lst = list(self.last.values()) + list(self.dlast.values())
        for d in lst:
            d.need = True
        self.ops.append(("barrier", lst))

    def emit(self):
        nc = self.nc
        sems = {e: nc.alloc_semaphore("s_" + e) for e in self.ENG}
        dq = ("sp", "act", "pool")
        dsems = {e: [nc.alloc_semaphore("d_%s_%d" % (e, i)) for i in range(self.ndma)] for e in dq}
        cnt = {e: 0 for e in self.ENG}
        dcount = {e: [0] * self.ndma for e in dq}
        for o in self.ops:
            if isinstance(o, tuple):
                continue
            if o.dma:
                i = o.dsem[1]
                o.prev = dcount[o.eng][i]
                dcount[o.eng][i] += 16
                o.dcnt = dcount[o.eng][i]
            elif o.need:
                cnt[o.eng] += 1
                o.cnt = cnt[o.eng]
        seen = {}
        pending = {e: [] for e in self.ENG}
        for o in self.ops:
            if isinstance(o, tuple):
                for e in self.ENG:
                    pending[e] = list(o[1])
                continue
            h = self.h[o.eng]
            waits = {}
            dl = o.deps
            if pending[o.eng]:
                dl = dl + pending[o.eng]
                pending[o.eng] = []
            for d in dl:
                if d.dma:
                    key = ("d",) + d.dsem
                    val = d.dcnt
                    sh = dsems[d.dsem[0]][d.dsem[1]]
                else:
                    key = ("c", d.eng)
                    val = d.cnt
                    sh = sems[d.eng]
                if seen.get((o.eng, key), 0) >= val:
                    continue
                if key not in waits or waits[key][1] < val:
                    waits[key] = (sh, val)
            if o.dma and o.prev > 0:
                key = ("d",) + o.dsem
                if seen.get((o.eng, key), 0) < o.prev:
                    if key not in waits or waits[key][1] < o.prev:
                        waits[key] = (dsems[o.dsem[0]][o.dsem[1]], o.prev)
            for key, (sh, val) in waits.items():
                h.wait_ge(sh, val)
                seen[(o.eng, key)] = val
            ins = o.fn()
            if o.dma:
                ins.then_inc(dsems[o.dsem[0]][o.dsem[1]], 16)
            elif o.need:
                ins.then_inc(sems[o.eng], 1)
        for e in dq:
            for i in range(self.ndma):
                if dcount[e][i] > 0:
                    nc.sync.wait_ge(dsems[e][i], dcount[e][i])
        self.ops = None


class Arena:
    def __init__(self, ap, words):
        self.ap = ap
        self.words = words
        self.off = 0

    def f32(self, n):
        assert self.off + n <= self.words, ("sbuf arena overflow", self.off, n)
        a = self.ap[:, self.off:self.off + n]
        self.off += n
        return a

    def bf16(self, n):
        w = (n + 1) // 2
        assert self.off + w <= self.words, ("sbuf arena overflow", self.off, n)
        a = self.ap[:, self.off:self.off + w].bitcast(BF16)
        self.off += w
        return a


def build(dbg=False):
    okind = "ExternalOutput" if dbg else "Internal"
    nc = bass.Bass("TRN2", target_bir_lowering=False)

    def din(name, shape):
        return nc.dram_tensor(name, list(shape), F32, kind="ExternalInput").ap()

    x_cur = din("x_cur", [T, D])
    x_prev = din("x_prev", [T, D])
    flag_d = din("flag", [128, 1])
    cT_d = din("cT", [128, 16])
    ident_d = din("ident", [128, 128])
    ada_w = din("ada_w", [D, 6 * D])
    ada_b_bc = din("ada_b_bc", [128, 6 * D])
    fada_w = din("final_ada_w", [D, 2 * D])
    fada_b_bc = din("final_ada_b_bc", [128, 2 * D])
    g1_bc = din("g1_bc", [128, D])
    g2_bc = din("g2_bc", [128, D])
    g3_bc = din("g3_bc", [128, D])
    w_in = din("w_in", [D, 6144])
    pool_w_l = din("pool_w_l", [128, 4, 2, 256])
    pscale_l = din("pscale_l", [128, 8])
    rc_bc = din("rc_bc", [128, 4, T])
    w_pool_out = din("w_pool_out", [1024, D])
    lamre_l = din("lamre_l", [128, 32])
    lamim_l = din("lamim_l", [128, 32])
    logdt_l = din("logdt_l", [128, 32])
    B_l = din("B_l", [128, 8, 2, 2, 128])
    CTre_l = din("CTre_l", [128, 32, 128])
    CTim_l = din("CTim_l", [128, 32, 128])
    ssmd_l = din("ssmd_l", [128, 8])
    w_glu = din("w_glu", [1024, 2 * D])
    bglu_l = din("bglu_l", [128, 32])
    w_out = din("w_out", [D, D])
    w_router = din("w_router", [D, NE])
    b_router = din("b_router", [1, NE])
    if not dbg:
        w1 = din("w1", [NE, D, 2 * D])
        b1 = din("b1", [NE, 2 * D])
        w2 = din("w2", [NE, D, D])
        b2 = din("b2", [NE, D])
    out_d = nc.dram_tensor("out", [T, D], F32, kind="ExternalOutput").ap()
    modbc = nc.dram_tensor("modbc", [128, 8 * D], F32, kind=okind).ap()
    x2_d = nc.dram_tensor("x2s", [T, D], F32, kind=okind).ap()
    mT_d = nc.dram_tensor("mTs", [16, 128, T], BF16, kind=okind).ap()
    if dbg:
        ys_dump = nc.dram_tensor("ys_dump", [128, 8, T], BF16, kind=okind).ap()
        pm2_dump = nc.dram_tensor("pm2_dump", [128, 8, T], BF16, kind=okind).ap()
        hT_dump = nc.dram_tensor("hT_dump", [128, 16, T], BF16, kind=okind).ap()
        xs_dump = nc.dram_tensor("xs_dump", [128, 2, 32, 129], F32, kind=okind).ap()
        gate_dump = nc.dram_tensor("gate_dump", [128, NT, NE], F32, kind=okind).ap()

    P = Prog(nc)
    W = 52000
    with nc.sbuf_tensor("arena", [128, W], F32) as ar_t, nc.psum_tensor("ps", [128, 8, 512], F32) as ps:
        A = Arena(ar_t, W)

        def reset(mark):
            A.off = mark
            P.barrier()
        PS = [Slot("ps%d" % k) for k in range(8)]
        bank_ctr = [0]

        def nb(lo=0, hi=8):
            k = lo + bank_ctr[0] % (hi - lo)
            bank_ctr[0] += 1
            return k

        def dma(eng, out, in_, reads=(), writes=(), **kw):
            h = P.h[eng]
            return P.op(eng, lambda: h.dma_start(out=out, in_=in_, **kw), reads=reads, writes=writes, dma=True)

        def mm(out, lhsT, rhs, start, stop, reads, writes):
            return P.op("pe", lambda: nc.tensor.matmul(out, lhsT=lhsT, rhs=rhs, start=start, stop=stop),
                        reads=reads, writes=writes)

        def act(out, in_, func, reads, writes, **kw):
            return P.op("act", lambda: nc.scalar.activation(out=out, in_=in_, func=func, **kw),
                        reads=reads, writes=writes)

        def stt(out, in0, scalar, in1, op0, op1, reads, writes):
            return P.op("dve", lambda: nc.vector.scalar_tensor_tensor(out=out, in0=in0, scalar=scalar, in1=in1,
                                                                       op0=op0, op1=op1), reads=reads, writes=writes)

        def tt(out, in0, in1, op, reads, writes, eng="dve"):
            h = P.h[eng]
            return P.op(eng, lambda: h.tensor_tensor(out=out, in0=in0, in1=in1, op=op), reads=reads, writes=writes)

        def ts(out, in0, s1, s2, op0, op1, reads, writes):
            if s2 is None:
                return P.op("dve", lambda: nc.vector.tensor_scalar(out=out, in0=in0, scalar1=s1, scalar2=None, op0=op0),
                            reads=reads, writes=writes)
            return P.op("dve", lambda: nc.vector.tensor_scalar(out=out, in0=in0, scalar1=s1, scalar2=s2, op0=op0, op1=op1),
                        reads=reads, writes=writes)

        hT = A.bf16(16 * T).rearrange("p (k t) -> p k t", k=16)
        S_hT = Slot("hT")
        ident = A.bf16(128)
        S_c = Slot("consts")
        ones_b = A.bf16(128)
        flag = A.f32(1)
        small = A.f32(32 * 24).rearrange("p (a b) -> p a b", b=32)
        S_small = Slot("small")
        dma("pool", ident, ident_d, writes=[S_c])
        dma("sp", flag, flag_d, writes=[S_c])
        P.op("dve", lambda: nc.vector.memset(ones_b, 1.0), writes=[S_c])
        carry = A.f32(64).rearrange("p (r a) -> p r a", r=2)
        halo = A.f32(8 * 16).rearrange("p (c h) -> p c h", c=8)
        S_halo = Slot("halo")
        mark_low = A.off
        ys = A.bf16(8 * T).rearrange("p (c t) -> p c t", c=8)
        S_ys = Slot("ys")
        mark_ys = A.off
        mark0 = A.off

        cT = A.f32(16)
        csg = A.f32(16)
        carep = A.f32(16 * 128).rearrange("p (k m) -> p k m", k=16)
        wblk = A.f32(16 * 512).rearrange("p (k n) -> p k n", k=16)
        bblk = A.f32(512)
        oblk = A.f32(512)
        S_ca, S_wblk, S_bblk, S_oblk, S_mod = Slot(), Slot(), Slot(), Slot(), Slot("modbc")
        dma("sp", cT, cT_d, writes=[S_ca])
        act(csg, cT, AF.Sigmoid, [S_ca], [S_ca])
        tt(cT, cT, csg, ALU.mult, [S_ca], [S_ca])
        P.op("dve", lambda: nc.vector.tensor_copy(out=carep, in_=cT.unsqueeze(2).to_broadcast([128, 16, 128])),
             reads=[S_ca], writes=[S_ca])
        for (wd, bd, nblk, off) in ((ada_w, ada_b_bc, 24, 0), (fada_w, fada_b_bc, 8, 6 * D)):
            wv = wd.rearrange("(k p) n -> p k n", p=128)
            for n in range(nblk):
                dma("sp", wblk, wv[:, :, n * 512:(n + 1) * 512], writes=[S_wblk])
                dma("sp", bblk, bd[:, n * 512:(n + 1) * 512], writes=[S_bblk])
                k = nb()
                for kc in range(16):
                    mm(ps[:, k, :], carep[:, kc, :], wblk[:, kc, :], kc == 0, kc == 15, [S_ca, S_wblk], [PS[k]])
                tt(oblk, ps[:, k, :], bblk, ALU.add, [PS[k], S_bblk], [S_oblk])
                dma("sp", modbc[:, off + n * 512: off + (n + 1) * 512], oblk, reads=[S_oblk], writes=[S_mod])
        reset(mark0)

        def load_GS(gbc, sc_off, sh_off):
            G = A.f32(D)
            S = A.f32(D)
            tmp = A.f32(D)
            sl = Slot()
            dma("sp", G, gbc, writes=[sl])
            dma("sp", tmp, modbc[:, sc_off:sc_off + D], reads=[S_mod], writes=[sl])
            dma("sp", S, modbc[:, sh_off:sh_off + D], reads=[S_mod], writes=[sl])
            stt(G, tmp, 1.0, G, ALU.add, ALU.mult, [sl], [sl])
            return G, S, sl

        def norm_rows(src, G, S, sl_gs, S_src, consume, nbuf_mark=None):
            xs = [A.f32(D)]
            sq = A.f32(D)
            st = A.f32(4)
            S_x = [Slot()]
            S_sq, S_st = Slot(), Slot()
            for i in range(NT):
                xb, sx = xs[0], S_x[0]
                dma("sp", xb, src[i * 128:(i + 1) * 128, :], reads=(S_src(i) if callable(S_src) else [S_src]), writes=[sx])
                act(sq, xb, AF.Square, [sx], [S_sq, S_st], accum_out=st[:, 0:1])
                ts(st[:, 1:2], st[:, 0:1], 1.0 / D, 1e-6, ALU.mult, ALU.add, [S_st], [S_st])
                act(st[:, 2:3], st[:, 1:2], AF.Sqrt, [S_st], [S_st])
                P.op("dve", lambda: nc.vector.reciprocal(out=st[:, 3:4], in_=st[:, 2:3]), reads=[S_st], writes=[S_st])
                stt(sq, xb, st[:, 3:4], G, ALU.mult, ALU.mult, [sx, S_st, sl_gs], [S_sq])
                tt(xb, sq, S, ALU.add, [S_sq, sl_gs], [sx])
                consume(i, xb, sx)

        def to_hT(i, h, sh, hb, S_hb):
            act(hb, h, AF.Copy, [sh], [S_hb])
            for half in range(2):
                k = nb()
                pb = ps[:, k, :].bitcast(BF16).rearrange("p (a b) -> p a b", a=8)
                for j in range(8):
                    kc = half * 8 + j
                    P.op("pe", lambda pb=pb, j=j, kc=kc: nc.tensor.transpose(pb[:, j, :], hb[:, kc * 128:(kc + 1) * 128], ident),
                         reads=[S_hb, S_c], writes=[PS[k]])
                P.op("dve", lambda pb=pb, half=half: nc.vector.tensor_copy(out=hT[:, half * 8:(half + 1) * 8, i * 128:(i + 1) * 128], in_=pb),
                     reads=[PS[k]], writes=[S_hT])

        def norm_to_hT(src, S_src, gbc, sc_off, sh_off, extra=None):
            m = A.off
            G, S, sl = load_GS(gbc, sc_off, sh_off)
            hb = A.bf16(D)
            S_hb = Slot()

            def consume(i, h, sh):
                to_hT(i, h, sh, hb, S_hb)
                if extra is not None:
                    extra(i)
            norm_rows(src, G, S, sl, S_src, consume)
            reset(m)

        (LRE, LIM, DT, AR, AI, NAI, A16R, A16I, FRE, FIM, NFRE, NFIM, T0, T1, T2, T3, T4) = range(17)

        def sm(i):
            return small[:, i, :]
        dma("sp", sm(LRE), lamre_l, writes=[S_small])
        dma("sp", sm(LIM), lamim_l, writes=[S_small])
        dma("sp", sm(DT), logdt_l, writes=[S_small])
        RS, WS = [S_small], [S_small]
        act(sm(DT), sm(DT), AF.Exp, RS, WS)
        ts(sm(LRE), sm(LRE), -1e-4, None, ALU.min, None, RS, WS)
        tt(sm(T0), sm(LRE), sm(DT), ALU.mult, RS, WS)
        act(sm(T0), sm(T0), AF.Exp, RS, WS)
        tt(sm(T1), sm(LIM), sm(DT), ALU.mult, RS, WS)
        halfpi = A.f32(1)
        P.op("dve", lambda: nc.vector.memset(halfpi, math.pi / 2), writes=WS)
        act(sm(T2), sm(T1), AF.Sin, RS, WS, scale=1.0 / 64)
        act(sm(T3), sm(T1), AF.Sin, RS, WS, scale=1.0 / 64, bias=halfpi[:, 0:1])

        def csquare(cr, ci, t):
            tt(sm(t), sm(cr), sm(ci), ALU.mult, RS, WS)
            tt(sm(cr), sm(cr), sm(cr), ALU.mult, RS, WS)
            tt(sm(ci), sm(ci), sm(ci), ALU.mult, RS, WS)
            tt(sm(cr), sm(cr), sm(ci), ALU.subtract, RS, WS)
            ts(sm(ci), sm(t), 2.0, None, ALU.mult, None, RS, WS)
        for _ in range(6):
            csquare(T3, T2, T4)
        tt(sm(AR), sm(T0), sm(T3), ALU.mult, RS, WS)
        tt(sm(AI), sm(T0), sm(T2), ALU.mult, RS, WS)
        ts(sm(NAI), sm(AI), -1.0, None, ALU.mult, None, RS, WS)
        P.op("dve", lambda: nc.vector.tensor_copy(out=sm(A16R), in_=sm(AR)), reads=RS, writes=WS)
        P.op("dve", lambda: nc.vector.tensor_copy(out=sm(A16I), in_=sm(AI)), reads=RS, writes=WS)
        for _ in range(4):
            csquare(A16R, A16I, T4)
        tt(sm(T0), sm(LRE), sm(LRE), ALU.mult, RS, WS)
        tt(sm(T1), sm(LIM), sm(LIM), ALU.mult, RS, WS)
        tt(sm(T0), sm(T0), sm(T1), ALU.add, RS, WS)
        P.op("dve", lambda: nc.vector.reciprocal(out=sm(T0), in_=sm(T0)), reads=RS, writes=WS)
        ts(sm(T1), sm(AR), -1.0, None, ALU.add, None, RS, WS)
        tt(sm(T2), sm(T1), sm(LRE), ALU.mult, RS, WS)
        tt(sm(T3), sm(AI), sm(LIM), ALU.mult, RS, WS)
        tt(sm(T2), sm(T2), sm(T3), ALU.add, RS, WS)
        tt(sm(FRE), sm(T2), sm(T0), ALU.mult, RS, WS)
        tt(sm(T2), sm(AI), sm(LRE), ALU.mult, RS, WS)
        tt(sm(T3), sm(T1), sm(LIM), ALU.mult, RS, WS)
        tt(sm(T2), sm(T2), sm(T3), ALU.subtract, RS, WS)
        tt(sm(FIM), sm(T2), sm(T0), ALU.mult, RS, WS)
        ts(sm(NFRE), sm(FRE), -1.0, None, ALU.mult, None, RS, WS)
        ts(sm(NFIM), sm(FIM), -1.0, None, ALU.mult, None, RS, WS)

        Cb = A.bf16(32 * 2 * 128).rearrange("p (a r m) -> p a r m", a=32, r=2)
        Bsb = A.bf16(8 * 2 * 2 * 128).rearrange("p (c v r m) -> p c v r m", c=8, v=2, r=2)
        ssmd = A.f32(8)
        S_cb = Slot("Cb")
        dma("pool", Bsb, B_l, writes=[S_cb])
        dma("sp", ssmd, ssmd_l, writes=[S_cb])
        m = A.off
        cre = A.f32(32 * 128).rearrange("p (a m) -> p a m", a=32)
        cim = A.f32(32 * 128).rearrange("p (a m) -> p a m", a=32)
        ctmp = A.f32(128)
        S_cl = Slot()
        dma("sp", cre, CTre_l, writes=[S_cl])
        dma("sp", cim, CTim_l, writes=[S_cl])
        for pr in range(32):
            ts(ctmp, cim[:, pr, :], small[:, FIM, pr:pr + 1], None, ALU.mult, None, [S_cl, S_small], [S_cl])
            stt(Cb[:, pr, 0, :], cre[:, pr, :], small[:, FRE, pr:pr + 1], ctmp, ALU.mult, ALU.subtract, [S_cl, S_small], [S_cb])
            ts(ctmp, cim[:, pr, :], small[:, NFRE, pr:pr + 1], None, ALU.mult, None, [S_cl, S_small], [S_cl])
            stt(Cb[:, pr, 1, :], cre[:, pr, :], small[:, NFIM, pr:pr + 1], ctmp, ALU.mult, ALU.add, [S_cl, S_small], [S_cb])
        reset(m)
        XSr = A.f32(32 * 129).rearrange("p (a c) -> p a c", a=32)
        XSi = A.f32(32 * 129).rearrange("p (a c) -> p a c", a=32)
        S_xs = Slot("XS")
        mark1 = A.off

        def zT_chunk(col0, ubf, S_u, wb, S_wb, tcs=range(4), fp32_out=None, S_f=None):
            dma("pool", wb, w_in.rearrange("(k p) n -> p k n", p=128)[:, :, col0:col0 + 128], writes=[S_wb])
            for tc in tcs:
                k = nb(4, 8)
                for kc in range(16):
                    mm(ps[:, k, :], wb[:, kc, :], hT[:, kc, tc * 512:(tc + 1) * 512], kc == 0, kc == 15, [S_wb, S_hT], [PS[k]])
                if fp32_out is None:
                    act(ubf[:, tc * 512:(tc + 1) * 512], ps[:, k, :], AF.Copy, [PS[k]], [S_u])
                else:
                    act(fp32_out[:, 16 + tc * 512:16 + (tc + 1) * 512], ps[:, k, :], AF.Copy, [PS[k]], [S_f])

        def ssm_pass(mode):
            m = A.off
            wb = A.bf16(16 * 128).rearrange("p (k n) -> p k n", k=16)
            ubf = A.bf16(T)
            vr = A.f32(T)
            vi = A.f32(T)
            S_wb, S_u, S_v = Slot(), Slot(), Slot()
            vr3 = vr.rearrange("p (c j) -> p c j", j=L1)
            vi3 = vi.rearrange("p (c j) -> p c j", j=L1)
            if mode == "B":
                xbr = A.bf16(T)
                xbi = A.bf16(T)
                gtmp = A.f32(512)
                S_xb, S_g = Slot(), Slot()
            for cc in range(8):
                zT_chunk(1024 + cc * 128, ubf, S_u, wb, S_wb)
                for q in range(4):
                    pr = cc * 4 + q
                    for tc in range(4):
                        for ri, v in ((0, vr), (1, vi)):
                            k = nb(4, 8)
                            p0, p1, vv = ((0, 32, 0), (32, 64, 0), (64, 96, 0), (64, 128, 1))[q]
                            mm(ps[:, k, :],
                               Bsb[p0:p1, cc, vv, ri, :], ubf[p0:p1, tc * 512:(tc + 1) * 512],
                               True, True, [S_cb, S_u], [PS[k]])
                            act(v[:, tc * 512:(tc + 1) * 512], ps[:, k, :], AF.Copy, [PS[k]], [S_v])
                    ar_, ai_, nai_ = small[:, AR, pr:pr + 1], small[:, AI, pr:pr + 1], small[:, NAI, pr:pr + 1]
                    RV = [S_v, S_small]
                    if mode == "B":
                        xpr, xpi = XSr[:, pr, 0:NC1], XSi[:, pr, 0:NC1]
                        RX = [S_v, S_small, S_xs]
                        stt(vr3[:, :, 0], xpi, nai_, vr3[:, :, 0], ALU.mult, ALU.add, RX, [S_v])
                        stt(vr3[:, :, 0], xpr, ar_, vr3[:, :, 0], ALU.mult, ALU.add, RX, [S_v])
                        stt(vi3[:, :, 0], xpr, ai_, vi3[:, :, 0], ALU.mult, ALU.add, RX, [S_v])
                        stt(vi3[:, :, 0], xpi, ar_, vi3[:, :, 0], ALU.mult, ALU.add, RX, [S_v])
                    for j in range(1, L1):
                        stt(vr3[:, :, j], vi3[:, :, j - 1], nai_, vr3[:, :, j], ALU.mult, ALU.add, RV, [S_v])
                        stt(vi3[:, :, j], vr3[:, :, j - 1], ai_, vi3[:, :, j], ALU.mult, ALU.add, RV, [S_v])
                        stt(vr3[:, :, j], vr3[:, :, j - 1], ar_, vr3[:, :, j], ALU.mult, ALU.add, RV, [S_v])
                        stt(vi3[:, :, j], vi3[:, :, j - 1], ar_, vi3[:, :, j], ALU.mult, ALU.add, RV, [S_v])
                    if mode == "A":
                        P.op("dve", lambda pr=pr: nc.vector.tensor_copy(out=XSr[:, pr, 1:NC1 + 1], in_=vr3[:, :, L1 - 1]),
                             reads=[S_v], writes=[S_xs])
                        P.op("dve", lambda pr=pr: nc.vector.tensor_copy(out=XSi[:, pr, 1:NC1 + 1], in_=vi3[:, :, L1 - 1]),
                             reads=[S_v], writes=[S_xs])
                    else:
                        act(xbr, vr, AF.Copy, [S_v], [S_xb])
                        act(xbi, vi, AF.Copy, [S_v], [S_xb])
                        for tc in range(4):
                            mm(ps[:, tc, :], Cb[:, pr, 0, :], xbr[:, tc * 512:(tc + 1) * 512], q == 0, False, [S_cb, S_xb], [PS[tc]])
                            mm(ps[:, tc, :], Cb[:, pr, 1, :], xbi[:, tc * 512:(tc + 1) * 512], False, q == 3, [S_cb, S_xb], [PS[tc]])
                if mode == "B":
                    for tc in range(4):
                        stt(gtmp, ubf[:, tc * 512:(tc + 1) * 512], ssmd[:, cc:cc + 1], ps[:, tc, :], ALU.mult, ALU.add,
                            [S_u, S_cb, PS[tc]], [S_g])
                        act(ys[:, cc, tc * 512:(tc + 1) * 512], gtmp, AF.Gelu, [S_g], [S_ys])
            reset(m)

        def level2():
            m = A.off
            t = A.f32(4 * 32).rearrange("p (a b) -> p a b", a=4)
            S_t = Slot()
            a16r, a16i = small[:, A16R, :], small[:, A16I, :]
            R = [S_xs, S_small, S_t]
            for c in range(NC1):
                tt(t[:, 0, :], a16r, XSr[:, :, c], ALU.mult, R, [S_t])
                tt(t[:, 1, :], a16i, XSi[:, :, c], ALU.mult, R, [S_t])
                tt(t[:, 2, :], a16r, XSi[:, :, c], ALU.mult, R, [S_t])
                tt(t[:, 3, :], a16i, XSr[:, :, c], ALU.mult, R, [S_t])
                tt(t[:, 0, :], t[:, 0, :], t[:, 1, :], ALU.subtract, R, [S_t])
                tt(t[:, 2, :], t[:, 2, :], t[:, 3, :], ALU.add, R, [S_t])
                tt(XSr[:, :, c + 1], XSr[:, :, c + 1], t[:, 0, :], ALU.add, R, [S_xs])
                tt(XSi[:, :, c + 1], XSi[:, :, c + 1], t[:, 2, :], ALU.add, R, [S_xs])
            reset(m)

        S_xprev, S_xcur = Slot("xprev"), Slot("xcur")
        norm_to_hT(x_prev, S_xprev, g1_bc, 1 * D, 0 * D)
        P.op("dve", lambda: nc.vector.memset(XSr[:, :, 0:1], 0.0), writes=[S_xs])
        P.op("dve", lambda: nc.vector.memset(XSi[:, :, 0:1], 0.0), writes=[S_xs])
        ssm_pass("A")
        level2()
        ts(carry[:, 0, :], XSr[:, :, NC1], flag[:, 0:1], None, ALU.mult, None, [S_xs, S_c], [S_halo])
        ts(carry[:, 1, :], XSi[:, :, NC1], flag[:, 0:1], None, ALU.mult, None, [S_xs, S_c], [S_halo])
        m = A.off
        wbp = A.bf16(16 * 128).rearrange("p (k n) -> p k n", k=16)
        S_wbp = Slot()
        for pc in range(8):
            dma("pool", wbp, w_in.rearrange("(k p) n -> p k n", p=128)[:, :, pc * 128:(pc + 1) * 128], writes=[S_wbp])
            k = nb(4, 8)
            for kc in range(16):
                mm(ps[:, k, 0:16], wbp[:, kc, :], hT[:, kc, T - 16:T], kc == 0, kc == 15, [S_wbp, S_hT], [PS[k]])
            ts(halo[:, pc, :], ps[:, k, 0:16], flag[:, 0:1], None, ALU.mult, None, [PS[k], S_c], [S_halo])
        reset(m)

        norm_to_hT(x_cur, S_xcur, g1_bc, 1 * D, 0 * D)
        P.op("dve", lambda: nc.vector.tensor_copy(out=XSr[:, :, 0], in_=carry[:, 0, :]), reads=[S_halo], writes=[S_xs])
        P.op("dve", lambda: nc.vector.tensor_copy(out=XSi[:, :, 0], in_=carry[:, 1, :]), reads=[S_halo], writes=[S_xs])
        ssm_pass("A")
        level2()
        if dbg:
            S_dump0 = Slot()
            dma("sp", xs_dump[:, 0], XSr, reads=[S_xs], writes=[S_dump0])
            dma("sp", xs_dump[:, 1], XSi, reads=[S_xs], writes=[S_dump0])
        ssm_pass("B")

        reset(mark_ys)
        pm2 = A.bf16(8 * T).rearrange("p (c t) -> p c t", c=8)
        S_pm2 = Slot("pm2")
        m = A.off
        wbp = A.bf16(16 * 128).rearrange("p (k n) -> p k n", k=16)
        pw = A.bf16(4 * 2 * 256).rearrange("p (g k d) -> p g k d", g=4, k=2)
        psc = A.f32(8)
        U = A.f32(T + 16)
        V = A.f32(T + 16)
        Wb = A.f32(T + 16)
        rc = A.f32(T)
        pooled = A.bf16(2 * T).rearrange("p (c t) -> p c t", c=2)
        S_wbp, S_pw, S_U, S_V, S_W, S_rc, S_pl = Slot(), Slot(), Slot(), Slot(), Slot(), Slot(), Slot()
        dma("pool", pw, pool_w_l, writes=[S_pw])
        dma("sp", psc, pscale_l, writes=[S_pw])
        N = T + 16
        for g in range(4):
            dma("sp", rc, rc_bc[:, g, :], writes=[S_rc])
            for gc in range(2):
                pc = g * 2 + gc
                zT_chunk(pc * 128, None, None, wbp, S_wbp, fp32_out=U, S_f=S_U)
                P.op("dve", lambda pc=pc: nc.vector.tensor_copy(out=U[:, 0:16], in_=halo[:, pc, :]), reads=[S_halo], writes=[S_U])
                src, ssrc = U, S_U
                bufs = [(V, S_V), (Wb, S_W)]
                sh = 1
                for lev in range(g + 1):
                    dst, sdst = bufs[lev % 2]
                    tt(dst[:, sh:N], src[:, sh:N], src[:, 0:N - sh], ALU.add, [ssrc], [sdst])
                    src, ssrc = dst, sdst
                    sh *= 2
                dst, sdst = bufs[(g + 1) % 2]
                tt(dst[:, 16:N], src[:, 16:N], rc, ALU.mult, [ssrc, S_rc], [sdst])
                tt(pooled[:, gc, :], dst[:, 16:N], U[:, 16:N], ALU.subtract, [sdst, S_U], [S_pl])
            for dch in range(2):
                for tc in range(4):
                    k = nb()
                    for kch in range(2):
                        mm(ps[:, k, :], pw[:, g, kch, dch * 128:(dch + 1) * 128], pooled[:, kch, tc * 512:(tc + 1) * 512],
                           kch == 0, kch == 1, [S_pw, S_pl], [PS[k]])
                    ts(pm2[:, g * 2 + dch, tc * 512:(tc + 1) * 512], ps[:, k, :], psc[:, g * 2 + dch:g * 2 + dch + 1], None,
                       ALU.mult, None, [PS[k], S_pw], [S_pm2])
        reset(m)

        m = A.off
        bglu = A.f32(32)
        S_bg = Slot()
        dma("sp", bglu, bglu_l, writes=[S_bg])
        wgp = A.bf16(16 * 256).rearrange("p (k n) -> p k n", k=16)
        wgs = A.bf16(16 * 256).rearrange("p (k n) -> p k n", k=16)
        wpo = A.bf16(8 * 256).rearrange("p (k n) -> p k n", k=8)
        wga = A.bf16(8 * 256).rearrange("p (k n) -> p k n", k=8)
        wgb = A.bf16(8 * 256).rearrange("p (k n) -> p k n", k=8)
        S_w3 = Slot()
        e_sgp, e_sgs, e_sgb, e_m1, e_ya = [A.f32(512) for _ in range(5)]
        mo = [A.bf16(512), A.bf16(512)]
        S_e = Slot()
        S_mo = [Slot(), Slot()]
        S_mT = Slot("mT")
        w_in_v = w_in.rearrange("(k p) n -> p k n", p=128)
        wpo_v = w_pool_out.rearrange("(k p) n -> p k n", p=128)
        wgl_v = w_glu.rearrange("(k p) n -> p k n", p=128)
        moi = 0
        for blk in range(8):
            c0 = blk * 256
            dma("pool", wgp, w_in_v[:, :, 2048 + c0:2048 + c0 + 256], writes=[S_w3])
            dma("pool", wgs, w_in_v[:, :, 4096 + c0:4096 + c0 + 256], writes=[S_w3])
            dma("pool", wpo, wpo_v[:, :, c0:c0 + 256], writes=[S_w3])
            dma("pool", wga, wgl_v[:, :, c0:c0 + 256], writes=[S_w3])
            dma("pool", wgb, wgl_v[:, :, 2048 + c0:2048 + c0 + 256], writes=[S_w3])
            for tc in range(4):
                tsl = slice(tc * 512, (tc + 1) * 512)
                for d2 in range(2):
                    dd = blk * 2 + d2
                    ws = slice(d2 * 128, (d2 + 1) * 128)
                    kgp, kgs, kyp, kga, kgb = nb(), nb(), nb(), nb(), nb()
                    for kc in range(16):
                        mm(ps[:, kgp, :], wgp[:, kc, ws], hT[:, kc, tsl], kc == 0, kc == 15, [S_w3, S_hT], [PS[kgp]])
                    for kc in range(16):
                        mm(ps[:, kgs, :], wgs[:, kc, ws], hT[:, kc, tsl], kc == 0, kc == 15, [S_w3, S_hT], [PS[kgs]])
                    for kc in range(8):
                        mm(ps[:, kyp, :], wpo[:, kc, ws], pm2[:, kc, tsl], kc == 0, kc == 7, [S_w3, S_pm2], [PS[kyp]])
                    for kc in range(8):
                        mm(ps[:, kga, :], wga[:, kc, ws], ys[:, kc, tsl], kc == 0, kc == 7, [S_w3, S_ys], [PS[kga]])
                    for kc in range(8):
                        mm(ps[:, kgb, :], wgb[:, kc, ws], ys[:, kc, tsl], kc == 0, kc == 7, [S_w3, S_ys], [PS[kgb]])
                    act(e_sgp, ps[:, kgp, :], AF.Sigmoid, [PS[kgp]], [S_e])
                    act(e_sgs, ps[:, kgs, :], AF.Sigmoid, [PS[kgs]], [S_e])
                    act(e_sgb, ps[:, kgb, :], AF.Sigmoid, [PS[kgb], S_bg], [S_e], bias=bglu[:, 16 + dd:17 + dd])
                    tt(e_m1, e_sgp, ps[:, kyp, :], ALU.mult, [S_e, PS[kyp]], [S_e])
                    stt(e_ya, ps[:, kga, :], bglu[:, dd:dd + 1], e_sgb, ALU.add, ALU.mult, [PS[kga], S_bg, S_e], [S_e])
                    tt(e_ya, e_ya, e_sgs, ALU.mult, [S_e], [S_e])
                    mb, smb = mo[moi % 2], S_mo[moi % 2]
                    moi += 1
                    tt(mb, e_m1, e_ya, ALU.add, [S_e], [smb])
                    dma("sp", mT_d[dd, :, tsl], mb, reads=[smb], writes=[S_mT])
        if dbg:
            S_dump = Slot()
            dma("sp", ys_dump, ys, reads=[S_ys], writes=[S_dump])
            dma("sp", pm2_dump, pm2, reads=[S_pm2], writes=[S_dump])
            dma("sp", hT_dump, hT, reads=[S_hT], writes=[S_dump])
        reset(m)

        m = A.off
        mTt = A.bf16(16 * 512).rearrange("p (k t) -> p k t", k=16)
        wo = A.bf16(16 * 512).rearrange("p (k n) -> p k n", k=16)
        gtm = A.f32(D)
        xs4 = [A.f32(512), A.f32(512)]
        x1p = [A.f32(512), A.f32(512)]
        S_mTt, S_wo, S_gtm = Slot(), Slot(), Slot()
        S_xs4 = [Slot(), Slot()]
        S_x1p = [Slot(), Slot()]
        S_x2 = Slot("x2")
        dma("sp", gtm, modbc[:, 2 * D:3 * D], reads=[S_mod], writes=[S_gtm])
        wo_v = w_out.rearrange("(k p) n -> p k n", p=128)
        ci = 0
        for tc in range(4):
            dma("sp", mTt, mT_d.rearrange("k p t -> p k t")[:, :, tc * 512:(tc + 1) * 512], reads=[S_mT], writes=[S_mTt])
            for cb in range(4):
                csl = slice(cb * 512, (cb + 1) * 512)
                dma("pool", wo, wo_v[:, :, csl], writes=[S_wo])
                for il in range(4):
                    i = tc * 4 + il
                    k = nb()
                    for kc in range(16):
                        mm(ps[:, k, :], mTt[:, kc, il * 128:(il + 1) * 128], wo[:, kc, :], kc == 0, kc == 15, [S_mTt, S_wo], [PS[k]])
                    xb_, sxb = xs4[ci % 2], S_xs4[ci % 2]
                    xo, sxo = x1p[ci % 2], S_x1p[ci % 2]
                    ci += 1
                    dma("sp", xb_, x_cur[i * 128:(i + 1) * 128, csl], writes=[sxb])
                    tt(xo, ps[:, k, :], gtm[:, csl], ALU.mult, [PS[k], S_gtm], [sxo])
                    tt(xo, xo, xb_, ALU.add, [sxo, sxb], [sxo])
                    dma("sp", x2_d[i * 128:(i + 1) * 128, csl], xo, reads=[sxo], writes=[S_x2])
        reset(m)

        reset(mark_low)
        gate = A.f32(NT * NE).rearrange("p (i e) -> p i e", i=NT)
        S_gate = Slot("gate")
        wr = A.bf16(16 * NE).rearrange("p (k e) -> p k e", k=16)
        brow = A.bf16(NE)
        lg = A.f32(NE)
        m8 = A.f32(8)
        ngm = A.f32(2)
        msk = A.f32(NE)
        S_wr, S_lg = Slot(), Slot()
        dma("pool", wr, w_router.rearrange("(k p) e -> p k e", p=128), writes=[S_wr])
        dma("pool", brow[0:1, :], b_router, writes=[S_wr])

        def router(i):
            k = nb()
            mm(ps[:, k, 0:NE], ones_b[0:1, :], brow[0:1, :], True, False, [S_c, S_wr], [PS[k]])
            for kc in range(16):
                mm(ps[:, k, 0:NE], hT[:, kc, i * 128:(i + 1) * 128], wr[:, kc, :], False, kc == 15, [S_hT, S_wr], [PS[k]])
            P.op("dve", lambda: nc.vector.tensor_copy(out=lg, in_=ps[:, k, 0:NE]), reads=[PS[k]], writes=[S_lg])
            P.op("dve", lambda: nc.vector.max(out=m8, in_=lg), reads=[S_lg], writes=[S_lg])
            ts(msk, lg, m8[:, 3:4], None, ALU.is_ge, None, [S_lg], [S_lg])
            ts(ngm[:, 0:1], m8[:, 0:1], -1.0, None, ALU.mult, None, [S_lg], [S_lg])
            act(lg, lg, AF.Exp, [S_lg], [S_lg], bias=ngm[:, 0:1])
            tt(lg, lg, msk, ALU.mult, [S_lg], [S_lg])
            P.op("dve", lambda: nc.vector.reduce_sum(out=ngm[:, 1:2], in_=lg, axis=mybir.AxisListType.X), reads=[S_lg], writes=[S_lg])
            P.op("dve", lambda: nc.vector.reciprocal(out=ngm[:, 1:2], in_=ngm[:, 1:2]), reads=[S_lg], writes=[S_lg])
            ts(gate[:, i, :], lg, ngm[:, 1:2], None, ALU.mult, None, [S_lg], [S_gate])

        norm_to_hT(x2_d, S_x2, g2_bc, 4 * D, 3 * D, extra=router)

        actT = A.bf16(NT * 16 * 128).rearrange("p (i f t) -> p i f t", i=NT, f=16)
        S_actT = Slot("actT")
        gtf = A.f32(D)
        S_gtf = Slot()
        dma("sp", gtf, modbc[:, 5 * D:6 * D], reads=[S_mod], writes=[S_gtf])
        w1g = A.bf16(16 * 256).rearrange("p (k n) -> p k n", k=16)
        w1l = A.bf16(16 * 256).rearrange("p (k n) -> p k n", k=16)
        w2b = A.bf16(16 * 512).rearrange("p (k n) -> p k n", k=16)
        b1r = A.bf16(2 * D)
        b2r = A.bf16(D)
        S_w1, S_w2, S_b = Slot(), Slot(), Slot()
        xg, sg_, xl = A.f32(256), A.f32(256), A.f32(256)
        ab = A.bf16(256)
        S_ev = Slot()
        yst = [A.f32(512) for _ in range(4)]
        S_yst = [Slot() for _ in range(4)]
        S_x2r = [[Slot() for _ in range(4)] for _ in range(NT)]
        for i in range(NT):
            for d4 in range(4):
                S_x2r[i][d4].w = S_x2.w
        yi = 0
        if dbg:
            dma("sp", gate_dump, gate, reads=[S_gate], writes=[Slot()])
        for e in range(0 if dbg else NE):
            dma("pool", b1r[0:1, :], b1[e:e + 1, :], writes=[S_b])
            dma("pool", b2r[0:1, :], b2[e:e + 1, :], writes=[S_b])
            w1v = w1[e].rearrange("(k p) n -> p k n", p=128)
            w2v = w2[e].rearrange("(k p) n -> p k n", p=128)
            for fs in range(8):
                f0 = fs * 256
                dma("pool", w1g, w1v[:, :, f0:f0 + 256], writes=[S_w1])
                dma("pool", w1l, w1v[:, :, D + f0:D + f0 + 256], writes=[S_w1])
                for i in range(NT):
                    k = nb()
                    for (wblk_, c0, b0) in ((w1g, 0, f0), (w1l, 256, D + f0)):
                        mm(ps[:, k, c0:c0 + 256], ones_b[0:1, :], b1r[0:1, b0:b0 + 256], True, False, [S_c, S_b], [PS[k]])
                        for kc in range(16):
                            mm(ps[:, k, c0:c0 + 256], hT[:, kc, i * 128:(i + 1) * 128], wblk_[:, kc, :], False, kc == 15,
                               [S_hT, S_w1], [PS[k]])
                    ts(xg, ps[:, k, 0:256], 7.0, None, ALU.min, None, [PS[k]], [S_ev])
                    act(sg_, xg, AF.Sigmoid, [S_ev], [S_ev], scale=1.702)
                    ts(xl, ps[:, k, 256:512], 7.0, -7.0, ALU.min, ALU.max, [PS[k]], [S_ev])
                    stt(xl, xl, 1.0, xg, ALU.add, ALU.mult, [S_ev], [S_ev])
                    tt(ab, xl, sg_, ALU.mult, [S_ev], [S_ev])
                    k2 = nb()
                    pb = ps[:, k2, :].bitcast(BF16).rearrange("p (a b) -> p a b", a=8)
                    for j in range(2):
                        P.op("pe", lambda pb=pb, j=j: nc.tensor.transpose(pb[:, j, :], ab[:, j * 128:(j + 1) * 128], ident),
                             reads=[S_ev, S_c], writes=[PS[k2]])
                    act(actT[:, i, fs * 2:fs * 2 + 2, :], pb[:, 0:2, :], AF.Copy, [PS[k2]], [S_actT])
            for d4 in range(4):
                dsl = slice(d4 * 512, (d4 + 1) * 512)
                dma("pool", w2b, w2v[:, :, dsl], writes=[S_w2])
                for i in range(NT):
                    k = nb()
                    mm(ps[:, k, :], ones_b[0:1, :], b2r[0:1, dsl], True, False, [S_c, S_b], [PS[k]])
                    for fc in range(16):
                        mm(ps[:, k, :], actT[:, i, fc, :], w2b[:, fc, :], False, fc == 15, [S_actT, S_w2], [PS[k]])
                    yb, syb = yst[yi % 4], S_yst[yi % 4]
                    yi += 1
                    stt(yb, ps[:, k, :], gate[:, i, e:e + 1], gtf[:, dsl], ALU.mult, ALU.mult, [PS[k], S_gate, S_gtf], [syb])
                    dma("pool", x2_d[i * 128:(i + 1) * 128, dsl], yb, reads=[syb, S_x2r[i][d4]], writes=[S_x2r[i][d4]],
                        accum_op=ALU.add)

        reset(mark_low)
        G3, S3, sl3 = load_GS(g3_bc, 7 * D, 6 * D)
        S_out = Slot()

        def fin_consume(i, h, sh):
            dma("sp", out_d[i * 128:(i + 1) * 128, :], h, reads=[sh], writes=[S_out])
        norm_rows(x2_d, G3, S3, sl3, (lambda i: S_x2r[i]), fin_consume)

        P.emit()
    return nc


def _prep_shared(inp):
    f = np.float32
    s = {}
    s["ident"] = np.eye(128, dtype=f)
    s["ada_w"] = np.ascontiguousarray(inp["ada_w"][0])
    s["ada_b_bc"] = np.ascontiguousarray(np.broadcast_to(inp["ada_b"][0][None, :], (128, 6 * D)))
    s["final_ada_w"] = np.ascontiguousarray(inp["final_ada_w"])
    s["final_ada_b_bc"] = np.ascontiguousarray(np.broadcast_to(inp["final_ada_b"][None, :], (128, 2 * D)))
    s["g1_bc"] = np.ascontiguousarray(np.broadcast_to(inp["norm1_g"][0][None, :], (128, D)))
    s["g2_bc"] = np.ascontiguousarray(np.broadcast_to(inp["norm2_g"][0][None, :], (128, D)))
    s["g3_bc"] = np.ascontiguousarray(np.broadcast_to(inp["final_norm_g"][None, :], (128, D)))
    s["w_in"] = np.ascontiguousarray(inp["w_in"][0])
    pw = inp["pool_w"][0]
    s["pool_w_l"] = np.ascontiguousarray(pw.reshape(4, 2, 128, 256).transpose(2, 0, 1, 3))
    s["pscale_l"] = np.ascontiguousarray(inp["pool_scale"][0].reshape(8, 128).T)
    s["w_pool_out"] = np.ascontiguousarray(inp["w_pool_out"][0])

    def pairl(a):
        return np.ascontiguousarray(a.reshape(32, 2, 64).transpose(1, 2, 0).reshape(128, 32))
    s["lamre_l"] = pairl(inp["ssm_lam_re"][0])
    s["lamim_l"] = pairl(inp["ssm_lam_im"][0])
    s["logdt_l"] = pairl(np.repeat(inp["ssm_log_dt"][0][:, None], 64, axis=1))
    bre, bim = inp["ssm_b_re"][0], inp["ssm_b_im"][0]
    Bl = np.zeros((128, 8, 2, 2, 128), f)
    cre, cim = inp["ssm_c_re"][0], inp["ssm_c_im"][0]
    Cr = np.zeros((128, 32, 128), f)
    Ci = np.zeros((128, 32, 128), f)
    for g in range(64):
        pr, gl = g // 2, g % 2
        cc, q = pr // 4, pr % 4
        r0 = 32 * q + 16 * gl
        vv = 1 if q == 3 else 0
        Bl[r0:r0 + 16, cc, vv, 0, gl * 64:(gl + 1) * 64] = bre[g].T
        Bl[r0:r0 + 16, cc, vv, 1, gl * 64:(gl + 1) * 64] = bim[g].T
        m0 = 32 * q + 16 * gl
        Cr[gl * 64:(gl + 1) * 64, pr, m0:m0 + 16] = cre[g].T
        Ci[gl * 64:(gl + 1) * 64, pr, m0:m0 + 16] = cim[g].T
    s["B_l"], s["CTre_l"], s["CTim_l"] = Bl, Cr, Ci
    s["ssmd_l"] = np.ascontiguousarray(inp["ssm_d"][0].reshape(8, 128).T)
    s["w_glu"] = np.ascontiguousarray(inp["w_glu"][0])
    s["bglu_l"] = np.ascontiguousarray(inp["b_glu"][0].reshape(32, 128).T)
    s["w_out"] = np.ascontiguousarray(inp["w_out"][0])
    s["w_router"] = np.ascontiguousarray(inp["w_router"][0])
    s["b_router"] = np.ascontiguousarray(inp["b_router"][0][None, :])
    s["w1"] = np.ascontiguousarray(inp["w1"][0])
    s["b1"] = np.ascontiguousarray(inp["b1"][0])
    s["w2"] = np.ascontiguousarray(inp["w2"][0])
    s["b2"] = np.ascontiguousarray(inp["b2"][0])
    return s


def kernel(**inp):
    inp = {k: np.asarray(v) for k, v in inp.items()}
    f = np.float32
    shared = _prep_shared(inp)
    x, c = inp["x"], inp["c"]
    in_maps = []
    win = np.array([2.0, 4.0, 8.0, 16.0], f)
    for core in range(8):
        b, half = core // 2, core % 2
        mp = dict(shared)
        mp["x_cur"] = np.ascontiguousarray(x[b, half * T:(half + 1) * T])
        mp["x_prev"] = np.ascontiguousarray(x[b, 0:T]) if half == 1 else np.zeros((T, D), f)
        mp["flag"] = np.full((128, 1), float(half), f)
        mp["cT"] = np.ascontiguousarray(c[b].reshape(16, 128).T)
        pos = np.arange(1, T + 1, dtype=f) + (T if half == 1 else 0)
        rc = (1.0 / np.minimum(pos[None, :], win[:, None])).astype(f)
        mp["rc_bc"] = np.ascontiguousarray(np.broadcast_to(rc[None], (128, 4, T)))
        in_maps.append(mp)
    nc = build()
    res = run_bass_kernel_spmd(nc, in_maps, core_ids=list(range(8)))
    out = np.zeros((4, 2 * T, D), f)
    for core in range(8):
        b, half = core // 2, core % 2
        out[b, half * T:(half + 1) * T] = res.results[core]["out"]
    return out
```

```python
import math
import numpy as np
import concourse.bass as bass
import concourse.mybir as mybir
from concourse.bass_utils import run_bass_kernel_spmd

F32 = mybir.dt.float32
BF16 = mybir.dt.bfloat16
AF = mybir.ActivationFunctionType
ALU = mybir.AluOpType

D = 2048
T = 2048
NT = 16
NE = 32
L1 = 16
NC1 = T // L1


class Slot:
    __slots__ = ("name", "w", "rs")

    def __init__(self, name=""):
        self.name = name
        self.w = None
        self.rs = []


class Op:
    __slots__ = ("eng", "fn", "deps", "need", "cnt", "dma", "dsem", "dcnt", "prev")

    def __init__(self, eng, fn, dma):
        self.eng = eng
        self.fn = fn
        self.deps = []
        self.need = False
        self.cnt = None
        self.dma = dma
        self.dsem = None
        self.dcnt = None
        self.prev = 0


class Prog:
    ENG = ("pe", "act", "dve", "pool", "sp")

    def __init__(self, nc, ndma=8):
        self.nc = nc
        self.ops = []
        self.last = {}
        self.dcur = {"sp": 0, "act": 0, "pool": 0}
        self.dlast = {}
        self.h = {"pe": nc.tensor, "act": nc.scalar, "dve": nc.vector, "pool": nc.gpsimd, "sp": nc.sync}
        self.ndma = ndma

    NDMA = {("sp", ""): 8, ("act", ""): 2, ("pool", ""): 3, ("pool", "w"): 3, ("pool", "a"): 4}

    def op(self, eng, fn, reads=(), writes=(), dma=False, cls=""):
        o = Op(eng, fn, dma)
        deps = set()
        for s in reads:
            if s.w is not None:
                deps.add(s.w)
        for s in writes:
            if s.w is not None:
                deps.add(s.w)
            for r in s.rs:
                deps.add(r)
        for d in deps:
            if (not d.dma) and (not dma) and d.eng == eng and eng == "pe":
                continue
            o.deps.append(d)
            d.need = True
        for s in reads:
            if dma:
                s.rs.append(o)
            else:
                s.rs = [r for r in s.rs if r.dma or r.eng != eng]
                s.rs.append(o)
        for s in writes:
            s.w = o
            s.rs = []
        self.ops.append(o)
        if dma:
            q = (eng, cls)
            i = self.dcur.get(q, 0) % self.NDMA[q]
            self.dcur[q] = self.dcur.get(q, 0) + 1
            o.dsem = (q, i)
            self.dlast[o.dsem] = o
        else:
            self.last[eng] = o
        return o

    def barrier(self):
        lst = list(self.last.values()) + list(self.dlast.values())
        for d in lst:
            d.need = True
        self.ops.append(("barrier", lst))

    def emit(self):
        nc = self.nc
        sems = {e: nc.alloc_semaphore("s_" + e) for e in self.ENG}
        dq = list(self.NDMA.keys())
        dsems = {q: [nc.alloc_semaphore("d_%s%s_%d" % (q[0], q[1], i)) for i in range(self.NDMA[q])] for q in dq}
        cnt = {e: 0 for e in self.ENG}
        dcount = {q: [0] * self.NDMA[q] for q in dq}
        for o in self.ops:
            if isinstance(o, tuple):
                continue
            if o.dma:
                q, i = o.dsem
                o.prev = dcount[q][i]
                dcount[q][i] += 16
                o.dcnt = dcount[q][i]
            elif o.need:
                cnt[o.eng] += 1
                o.cnt = cnt[o.eng]
        seen = {}
        pending = {e: [] for e in self.ENG}
        for o in self.ops:
            if isinstance(o, tuple):
                for e in self.ENG:
                    pending[e] = list(o[1])
                continue
            h = self.h[o.eng]
            waits = {}
            dl = o.deps
            if pending[o.eng]:
                dl = dl + pending[o.eng]
                pending[o.eng] = []
            for d in dl:
                if d.dma:
                    key = ("d", d.dsem)
                    val = d.dcnt
                    sh = dsems[d.dsem[0]][d.dsem[1]]
                else:
                    key = ("c", d.eng)
                    val = d.cnt
                    sh = sems[d.eng]
                if seen.get((o.eng, key), 0) >= val:
                    continue
                if key not in waits or waits[key][1] < val:
                    waits[key] = (sh, val)
            if o.dma and o.prev > 0:
                key = ("d", o.dsem)
                if seen.get((o.eng, key), 0) < o.prev:
                    if key not in waits or waits[key][1] < o.prev:
                        waits[key] = (dsems[o.dsem[0]][o.dsem[1]], o.prev)
            for key, (sh, val) in waits.items():
                h.wait_ge(sh, val)
                seen[(o.eng, key)] = val
            ins = o.fn()
            if o.dma:
                ins.then_inc(dsems[o.dsem[0]][o.dsem[1]], 16)
            elif o.need:
                ins.then_inc(sems[o.eng], 1)
        for q in dq:
            for i in range(self.NDMA[q]):
                if dcount[q][i] > 0:
                    nc.sync.wait_ge(dsems[q][i], dcount[q][i])
        self.ops = None


class Arena:
    def __init__(self, ap, words):
        self.ap = ap
        self.words = words
        self.off = 0

    def f32(self, n):
        assert self.off + n <= self.words, ("sbuf arena overflow", self.off, n)
        a = self.ap[:, self.off:self.off + n]
        self.off += n
        return a

    def bf16(self, n):
        w = (n + 1) // 2
        assert self.off + w <= self.words, ("sbuf arena overflow", self.off, n)
        a = self.ap[:, self.off:self.off + w].bitcast(BF16)
        self.off += w
        return a


def build(dbg=False):
    okind = "ExternalOutput" if dbg else "Internal"
    nc = bass.Bass("TRN2", target_bir_lowering=False)

    def din(name, shape):
        return nc.dram_tensor(name, list(shape), F32, kind="ExternalInput").ap()

    x_cur = din("x_cur", [T, D])
    x_prev = din("x_prev", [T, D])
    flag_d = din("flag", [128, 1])
    cT_d = din("cT", [128, 16])
    ident_d = din("ident", [128, 128])
    ada_w = din("ada_w", [D, 6 * D])
    ada_b_bc = din("ada_b_bc", [128, 6 * D])
    fada_w = din("final_ada_w", [D, 2 * D])
    fada_b_bc = din("final_ada_b_bc", [128, 2 * D])
    g1_bc = din("g1_bc", [128, D])
    g2_bc = din("g2_bc", [128, D])
    g3_bc = din("g3_bc", [128, D])
    w_in = din("w_in", [D, 6144])
    pool_w_l = din("pool_w_l", [128, 4, 2, 256])
    pscale_l = din("pscale_l", [128, 8])
    rc_bc = din("rc_bc", [128, 4, T])
    w_pool_out = din("w_pool_out", [1024, D])
    lamre_l = din("lamre_l", [128, 32])
    lamim_l = din("lamim_l", [128, 32])
    logdt_l = din("logdt_l", [128, 32])
    B_l = din("B_l", [128, 8, 2, 2, 128])
    CTre_l = din("CTre_l", [128, 32, 128])
    CTim_l = din("CTim_l", [128, 32, 128])
    ssmd_l = din("ssmd_l", [128, 8])
    w_glu = din("w_glu", [1024, 2 * D])
    bglu_l = din("bglu_l", [128, 32])
    w_out = din("w_out", [D, D])
    w_router = din("w_router", [D, NE])
    b_router = din("b_router", [1, NE])
    if not dbg:
        w1 = din("w1", [NE, D, 2 * D])
        b1 = din("b1", [NE, 2 * D])
        w2 = din("w2", [NE, D, D])
        b2 = din("b2", [NE, D])
    out_d = nc.dram_tensor("out", [T, D], F32, kind="ExternalOutput").ap()
    modbc = nc.dram_tensor("modbc", [128, 8 * D], F32, kind=okind).ap()
    x2_d = nc.dram_tensor("x2s", [T, D], F32, kind=okind).ap()
    mT_d = nc.dram_tensor("mTs", [16, 128, T], BF16, kind=okind).ap()
    if dbg:
        ys_dump = nc.dram_tensor("ys_dump", [128, 8, T], BF16, kind=okind).ap()
        pm2_dump = nc.dram_tensor("pm2_dump", [128, 8, T], BF16, kind=okind).ap()
        hT_dump = nc.dram_tensor("hT_dump", [128, 16, T], BF16, kind=okind).ap()
        xs_dump = nc.dram_tensor("xs_dump", [128, 2, 32, 129], F32, kind=okind).ap()
        gate_dump = nc.dram_tensor("gate_dump", [128, NT, NE], F32, kind=okind).ap()

    P = Prog(nc)
    W = 53200
    with nc.sbuf_tensor("arena", [128, W], F32) as ar_t, nc.psum_tensor("ps", [128, 8, 512], F32) as ps:
        A = Arena(ar_t, W)

        def reset(mark):
            A.off = mark
            P.barrier()
        PS = [Slot("ps%d" % k) for k in range(8)]
        bank_ctr = [0]

        def nb(lo=0, hi=8):
            k = lo + bank_ctr[0] % (hi - lo)
            bank_ctr[0] += 1
            return k

        def dma(eng, out, in_, reads=(), writes=(), cls="", **kw):
            h = P.h[eng]
            return P.op(eng, lambda: h.dma_start(out=out, in_=in_, **kw), reads=reads, writes=writes, dma=True, cls=cls)

        def mm(out, lhsT, rhs, start, stop, reads, writes):
            return P.op("pe", lambda: nc.tensor.matmul(out, lhsT=lhsT, rhs=rhs, start=start, stop=stop),
                        reads=reads, writes=writes)

        def act(out, in_, func, reads, writes, **kw):
            return P.op("act", lambda: nc.scalar.activation(out=out, in_=in_, func=func, **kw),
                        reads=reads, writes=writes)

        def stt(out, in0, scalar, in1, op0, op1, reads, writes):
            return P.op("dve", lambda: nc.vector.scalar_tensor_tensor(out=out, in0=in0, scalar=scalar, in1=in1,
                                                                       op0=op0, op1=op1), reads=reads, writes=writes)

        def tt(out, in0, in1, op, reads, writes, eng="dve"):
            h = P.h[eng]
            return P.op(eng, lambda: h.tensor_tensor(out=out, in0=in0, in1=in1, op=op), reads=reads, writes=writes)

        def ts(out, in0, s1, s2, op0, op1, reads, writes):
            if s2 is None:
                return P.op("dve", lambda: nc.vector.tensor_scalar(out=out, in0=in0, scalar1=s1, scalar2=None, op0=op0),
                            reads=reads, writes=writes)
            return P.op("dve", lambda: nc.vector.tensor_scalar(out=out, in0=in0, scalar1=s1, scalar2=s2, op0=op0, op1=op1),
                        reads=reads, writes=writes)

        hT = A.bf16(16 * T).rearrange("p (k t) -> p k t", k=16)
        S_hT = Slot("hT")
        ident = A.bf16(128)
        S_c = Slot("consts")
        ones_b = A.bf16(128)
        flag = A.f32(1)
        S_small = Slot("small")
        dma("pool", ident, ident_d, writes=[S_c])
        dma("sp", flag, flag_d, writes=[S_c])
        P.op("dve", lambda: nc.vector.memset(ones_b, 1.0), writes=[S_c])
        mark_low = A.off
        small = A.f32(32 * 24).rearrange("p (a b) -> p a b", b=32)
        carry = A.f32(64).rearrange("p (r a) -> p r a", r=2)
        halo = A.f32(8 * 16).rearrange("p (c h) -> p c h", c=8)
        S_halo = Slot("halo")
        ys = A.bf16(8 * T).rearrange("p (c t) -> p c t", c=8)
        S_ys = Slot("ys")
        mark_ys = A.off
        mark0 = A.off

        cT = A.f32(16)
        csg = A.f32(16)
        carep = A.f32(16 * 128).rearrange("p (k m) -> p k m", k=16)
        wblk = A.f32(16 * 512).rearrange("p (k n) -> p k n", k=16)
        bblk = A.f32(512)
        oblk = A.f32(512)
        S_ca, S_wblk, S_bblk, S_oblk, S_mod = Slot(), Slot(), Slot(), Slot(), Slot("modbc")
        dma("sp", cT, cT_d, writes=[S_ca])
        act(csg, cT, AF.Sigmoid, [S_ca], [S_ca])
        tt(cT, cT, csg, ALU.mult, [S_ca], [S_ca])
        P.op("dve", lambda: nc.vector.tensor_copy(out=carep, in_=cT.unsqueeze(2).to_broadcast([128, 16, 128])),
             reads=[S_ca], writes=[S_ca])
        for (wd, bd, nblk, off) in ((ada_w, ada_b_bc, 24, 0), (fada_w, fada_b_bc, 8, 6 * D)):
            wv = wd.rearrange("(k p) n -> p k n", p=128)
            for n in range(nblk):
                dma("sp", wblk, wv[:, :, n * 512:(n + 1) * 512], writes=[S_wblk])
                dma("sp", bblk, bd[:, n * 512:(n + 1) * 512], writes=[S_bblk])
                k = nb()
                for kc in range(16):
                    mm(ps[:, k, :], carep[:, kc, :], wblk[:, kc, :], kc == 0, kc == 15, [S_ca, S_wblk], [PS[k]])
                tt(oblk, ps[:, k, :], bblk, ALU.add, [PS[k], S_bblk], [S_oblk])
                dma("sp", modbc[:, off + n * 512: off + (n + 1) * 512], oblk, reads=[S_oblk], writes=[S_mod])
        reset(mark0)

        def load_GS(gbc, sc_off, sh_off):
            G = A.f32(D)
            S = A.f32(D)
            tmp = A.f32(D)
            sl = Slot()
            dma("sp", G, gbc, writes=[sl])
            dma("sp", tmp, modbc[:, sc_off:sc_off + D], reads=[S_mod], writes=[sl])
            dma("sp", S, modbc[:, sh_off:sh_off + D], reads=[S_mod], writes=[sl])
            stt(G, tmp, 1.0, G, ALU.add, ALU.mult, [sl], [sl])
            return G, S, sl

        def norm_rows(src, G, S, sl_gs, S_src, consume, nbuf_mark=None):
            xs = [A.f32(D)]
            sq = A.f32(D)
            st = A.f32(4)
            S_x = [Slot()]
            S_sq, S_st = Slot(), Slot()
            for i in range(NT):
                xb, sx = xs[0], S_x[0]
                dma("sp", xb, src[i * 128:(i + 1) * 128, :], reads=(S_src(i) if callable(S_src) else [S_src]), writes=[sx])
                act(sq, xb, AF.Square, [sx], [S_sq, S_st], accum_out=st[:, 0:1])
                ts(st[:, 1:2], st[:, 0:1], 1.0 / D, 1e-6, ALU.mult, ALU.add, [S_st], [S_st])
                act(st[:, 2:3], st[:, 1:2], AF.Sqrt, [S_st], [S_st])
                P.op("dve", lambda: nc.vector.reciprocal(out=st[:, 3:4], in_=st[:, 2:3]), reads=[S_st], writes=[S_st])
                stt(sq, xb, st[:, 3:4], G, ALU.mult, ALU.mult, [sx, S_st, sl_gs], [S_sq])
                tt(xb, sq, S, ALU.add, [S_sq, sl_gs], [sx])
                consume(i, xb, sx)

        def to_hT(i, h, sh, hb, S_hb):
            act(hb, h, AF.Copy, [sh], [S_hb])
            for half in range(2):
                k = nb()
                pb = ps[:, k, :].bitcast(BF16).rearrange("p (a b) -> p a b", a=8)
                for j in range(8):
                    kc = half * 8 + j
                    P.op("pe", lambda pb=pb, j=j, kc=kc: nc.tensor.transpose(pb[:, j, :], hb[:, kc * 128:(kc + 1) * 128], ident),
                         reads=[S_hb, S_c], writes=[PS[k]])
                P.op("dve", lambda pb=pb, half=half: nc.vector.tensor_copy(out=hT[:, half * 8:(half + 1) * 8, i * 128:(i + 1) * 128], in_=pb),
                     reads=[PS[k]], writes=[S_hT])

        def norm_to_hT(src, S_src, gbc, sc_off, sh_off, extra=None):
            m = A.off
            G, S, sl = load_GS(gbc, sc_off, sh_off)
            hb = A.bf16(D)
            S_hb = Slot()

            def consume(i, h, sh):
                to_hT(i, h, sh, hb, S_hb)
                if extra is not None:
                    extra(i)
            norm_rows(src, G, S, sl, S_src, consume)
            reset(m)

        (LRE, LIM, DT, AR, AI, NAI, A16R, A16I, FRE, FIM, NFRE, NFIM, T0, T1, T2, T3, T4) = range(17)

        def sm(i):
            return small[:, i, :]
        dma("sp", sm(LRE), lamre_l, writes=[S_small])
        dma("sp", sm(LIM), lamim_l, writes=[S_small])
        dma("sp", sm(DT), logdt_l, writes=[S_small])
        RS, WS = [S_small], [S_small]
        act(sm(DT), sm(DT), AF.Exp, RS, WS)
        ts(sm(LRE), sm(LRE), -1e-4, None, ALU.min, None, RS, WS)
        tt(sm(T0), sm(LRE), sm(DT), ALU.mult, RS, WS)
        act(sm(T0), sm(T0), AF.Exp, RS, WS)
        tt(sm(T1), sm(LIM), sm(DT), ALU.mult, RS, WS)
        halfpi = A.f32(1)
        P.op("dve", lambda: nc.vector.memset(halfpi, math.pi / 2), writes=WS)
        act(sm(T2), sm(T1), AF.Sin, RS, WS, scale=1.0 / 64)
        act(sm(T3), sm(T1), AF.Sin, RS, WS, scale=1.0 / 64, bias=halfpi[:, 0:1])

        def csquare(cr, ci, t):
            tt(sm(t), sm(cr), sm(ci), ALU.mult, RS, WS)
            tt(sm(cr), sm(cr), sm(cr), ALU.mult, RS, WS)
            tt(sm(ci), sm(ci), sm(ci), ALU.mult, RS, WS)
            tt(sm(cr), sm(cr), sm(ci), ALU.subtract, RS, WS)
            ts(sm(ci), sm(t), 2.0, None, ALU.mult, None, RS, WS)
        for _ in range(6):
            csquare(T3, T2, T4)
        tt(sm(AR), sm(T0), sm(T3), ALU.mult, RS, WS)
        tt(sm(AI), sm(T0), sm(T2), ALU.mult, RS, WS)
        ts(sm(NAI), sm(AI), -1.0, None, ALU.mult, None, RS, WS)
        P.op("dve", lambda: nc.vector.tensor_copy(out=sm(A16R), in_=sm(AR)), reads=RS, writes=WS)
        P.op("dve", lambda: nc.vector.tensor_copy(out=sm(A16I), in_=sm(AI)), reads=RS, writes=WS)
        for _ in range(4):
            csquare(A16R, A16I, T4)
        tt(sm(T0), sm(LRE), sm(LRE), ALU.mult, RS, WS)
        tt(sm(T1), sm(LIM), sm(LIM), ALU.mult, RS, WS)
        tt(sm(T0), sm(T0), sm(T1), ALU.add, RS, WS)
        P.op("dve", lambda: nc.vector.reciprocal(out=sm(T0), in_=sm(T0)), reads=RS, writes=WS)
        ts(sm(T1), sm(AR), -1.0, None, ALU.add, None, RS, WS)
        tt(sm(T2), sm(T1), sm(LRE), ALU.mult, RS, WS)
        tt(sm(T3), sm(AI), sm(LIM), ALU.mult, RS, WS)
        tt(sm(T2), sm(T2), sm(T3), ALU.add, RS, WS)
        tt(sm(FRE), sm(T2), sm(T0), ALU.mult, RS, WS)
        tt(sm(T2), sm(AI), sm(LRE), ALU.mult, RS, WS)
        tt(sm(T3), sm(T1), sm(LIM), ALU.mult, RS, WS)
        tt(sm(T2), sm(T2), sm(T3), ALU.subtract, RS, WS)
        tt(sm(FIM), sm(T2), sm(T0), ALU.mult, RS, WS)
        ts(sm(NFRE), sm(FRE), -1.0, None, ALU.mult, None, RS, WS)
        ts(sm(NFIM), sm(FIM), -1.0, None, ALU.mult, None, RS, WS)

        Cb = A.bf16(32 * 2 * 128).rearrange("p (a r m) -> p a r m", a=32, r=2)
        Bsb = A.bf16(8 * 2 * 2 * 128).rearrange("p (c v r m) -> p c v r m", c=8, v=2, r=2)
        ssmd = A.f32(8)
        S_cb = Slot("Cb")
        dma("pool", Bsb, B_l, writes=[S_cb])
        dma("sp", ssmd, ssmd_l, writes=[S_cb])
        m = A.off
        cre = A.f32(32 * 128).rearrange("p (a m) -> p a m", a=32)
        cim = A.f32(32 * 128).rearrange("p (a m) -> p a m", a=32)
        ctmp = A.f32(128)
        S_cl = Slot()
        dma("sp", cre, CTre_l, writes=[S_cl])
        dma("sp", cim, CTim_l, writes=[S_cl])
        for pr in range(32):
            ts(ctmp, cim[:, pr, :], small[:, FIM, pr:pr + 1], None, ALU.mult, None, [S_cl, S_small], [S_cl])
            stt(Cb[:, pr, 0, :], cre[:, pr, :], small[:, FRE, pr:pr + 1], ctmp, ALU.mult, ALU.subtract, [S_cl, S_small], [S_cb])
            ts(ctmp, cim[:, pr, :], small[:, NFRE, pr:pr + 1], None, ALU.mult, None, [S_cl, S_small], [S_cl])
            stt(Cb[:, pr, 1, :], cre[:, pr, :], small[:, NFIM, pr:pr + 1], ctmp, ALU.mult, ALU.add, [S_cl, S_small], [S_cb])
        reset(m)
        XSr = A.f32(32 * 129).rearrange("p (a c) -> p a c", a=32)
        XSi = A.f32(32 * 129).rearrange("p (a c) -> p a c", a=32)
        S_xs = Slot("XS")
        mark1 = A.off

        def zT_chunk(col0, ubf, S_u, wb, S_wb, tcs=range(4), fp32_out=None, S_f=None):
            dma("pool", wb, w_in.rearrange("(k p) n -> p k n", p=128)[:, :, col0:col0 + 128], writes=[S_wb])
            for tc in tcs:
                k = nb(4, 8)
                for kc in range(16):
                    mm(ps[:, k, :], wb[:, kc, :], hT[:, kc, tc * 512:(tc + 1) * 512], kc == 0, kc == 15, [S_wb, S_hT], [PS[k]])
                if fp32_out is None:
                    act(ubf[:, tc * 512:(tc + 1) * 512], ps[:, k, :], AF.Copy, [PS[k]], [S_u])
                else:
                    act(fp32_out[:, 16 + tc * 512:16 + (tc + 1) * 512], ps[:, k, :], AF.Copy, [PS[k]], [S_f])

        def ssm_pass(mode):
            m = A.off
            wb = A.bf16(16 * 128).rearrange("p (k n) -> p k n", k=16)
            ubf = A.bf16(T)
            vr = A.f32(T)
            vi = A.f32(T)
            S_wb, S_u, S_v = Slot(), Slot(), Slot()
            vr3 = vr.rearrange("p (c j) -> p c j", j=L1)
            vi3 = vi.rearrange("p (c j) -> p c j", j=L1)
            if mode == "B":
                xbr = A.bf16(T)
                xbi = A.bf16(T)
                gtmp = A.f32(512)
                S_xb, S_g = Slot(), Slot()
            for cc in range(8):
                zT_chunk(1024 + cc * 128, ubf, S_u, wb, S_wb)
                for q in range(4):
                    pr = cc * 4 + q
                    for tc in range(4):
                        for ri, v in ((0, vr), (1, vi)):
                            k = nb(4, 8)
                            p0, p1, vv = ((0, 32, 0), (32, 64, 0), (64, 96, 0), (64, 128, 1))[q]
                            mm(ps[:, k, :],
                               Bsb[p0:p1, cc, vv, ri, :], ubf[p0:p1, tc * 512:(tc + 1) * 512],
                               True, True, [S_cb, S_u], [PS[k]])
                            act(v[:, tc * 512:(tc + 1) * 512], ps[:, k, :], AF.Copy, [PS[k]], [S_v])
                    ar_, ai_, nai_ = small[:, AR, pr:pr + 1], small[:, AI, pr:pr + 1], small[:, NAI, pr:pr + 1]
                    RV = [S_v, S_small]
                    if mode == "B":
                        xpr, xpi = XSr[:, pr, 0:NC1], XSi[:, pr, 0:NC1]
                        RX = [S_v, S_small, S_xs]
                        stt(vr3[:, :, 0], xpi, nai_, vr3[:, :, 0], ALU.mult, ALU.add, RX, [S_v])
                        stt(vr3[:, :, 0], xpr, ar_, vr3[:, :, 0], ALU.mult, ALU.add, RX, [S_v])
                        stt(vi3[:, :, 0], xpr, ai_, vi3[:, :, 0], ALU.mult, ALU.add, RX, [S_v])
                        stt(vi3[:, :, 0], xpi, ar_, vi3[:, :, 0], ALU.mult, ALU.add, RX, [S_v])
                    for j in range(1, L1):
                        stt(vr3[:, :, j], vi3[:, :, j - 1], nai_, vr3[:, :, j], ALU.mult, ALU.add, RV, [S_v])
                        stt(vi3[:, :, j], vr3[:, :, j - 1], ai_, vi3[:, :, j], ALU.mult, ALU.add, RV, [S_v])
                        stt(vr3[:, :, j], vr3[:, :, j - 1], ar_, vr3[:, :, j], ALU.mult, ALU.add, RV, [S_v])
                        stt(vi3[:, :, j], vi3[:, :, j - 1], ar_, vi3[:, :, j], ALU.mult, ALU.add, RV, [S_v])
                    if mode == "A":
                        P.op("dve", lambda pr=pr: nc.vector.tensor_copy(out=XSr[:, pr, 1:NC1 + 1], in_=vr3[:, :, L1 - 1]),
                             reads=[S_v], writes=[S_xs])
                        P.op("dve", lambda pr=pr: nc.vector.tensor_copy(out=XSi[:, pr, 1:NC1 + 1], in_=vi3[:, :, L1 - 1]),
                             reads=[S_v], writes=[S_xs])
                    else:
                        act(xbr, vr, AF.Copy, [S_v], [S_xb])
                        act(xbi, vi, AF.Copy, [S_v], [S_xb])
                        for tc in range(4):
                            mm(ps[:, tc, :], Cb[:, pr, 0, :], xbr[:, tc * 512:(tc + 1) * 512], q == 0, False, [S_cb, S_xb], [PS[tc]])
                            mm(ps[:, tc, :], Cb[:, pr, 1, :], xbi[:, tc * 512:(tc + 1) * 512], False, q == 3, [S_cb, S_xb], [PS[tc]])
                if mode == "B":
                    for tc in range(4):
                        stt(gtmp, ubf[:, tc * 512:(tc + 1) * 512], ssmd[:, cc:cc + 1], ps[:, tc, :], ALU.mult, ALU.add,
                            [S_u, S_cb, PS[tc]], [S_g])
                        act(ys[:, cc, tc * 512:(tc + 1) * 512], gtmp, AF.Gelu, [S_g], [S_ys])
            reset(m)

        def level2():
            m = A.off
            t = A.f32(4 * 32).rearrange("p (a b) -> p a b", a=4)
            S_t = Slot()
            a16r, a16i = small[:, A16R, :], small[:, A16I, :]
            R = [S_xs, S_small, S_t]
            for c in range(NC1):
                tt(t[:, 0, :], a16r, XSr[:, :, c], ALU.mult, R, [S_t])
                tt(t[:, 1, :], a16i, XSi[:, :, c], ALU.mult, R, [S_t])
                tt(t[:, 2, :], a16r, XSi[:, :, c], ALU.mult, R, [S_t])
                tt(t[:, 3, :], a16i, XSr[:, :, c], ALU.mult, R, [S_t])
                tt(t[:, 0, :], t[:, 0, :], t[:, 1, :], ALU.subtract, R, [S_t])
                tt(t[:, 2, :], t[:, 2, :], t[:, 3, :], ALU.add, R, [S_t])
                tt(XSr[:, :, c + 1], XSr[:, :, c + 1], t[:, 0, :], ALU.add, R, [S_xs])
                tt(XSi[:, :, c + 1], XSi[:, :, c + 1], t[:, 2, :], ALU.add, R, [S_xs])
            reset(m)

        S_xprev, S_xcur = Slot("xprev"), Slot("xcur")
        norm_to_hT(x_prev, S_xprev, g1_bc, 1 * D, 0 * D)
        P.op("dve", lambda: nc.vector.memset(XSr[:, :, 0:1], 0.0), writes=[S_xs])
        P.op("dve", lambda: nc.vector.memset(XSi[:, :, 0:1], 0.0), writes=[S_xs])
        ssm_pass("A")
        level2()
        ts(carry[:, 0, :], XSr[:, :, NC1], flag[:, 0:1], None, ALU.mult, None, [S_xs, S_c], [S_halo])
        ts(carry[:, 1, :], XSi[:, :, NC1], flag[:, 0:1], None, ALU.mult, None, [S_xs, S_c], [S_halo])
        m = A.off
        wbp = A.bf16(16 * 128).rearrange("p (k n) -> p k n", k=16)
        S_wbp = Slot()
        for pc in range(8):
            dma("pool", wbp, w_in.rearrange("(k p) n -> p k n", p=128)[:, :, pc * 128:(pc + 1) * 128], writes=[S_wbp])
            k = nb(4, 8)
            for kc in range(16):
                mm(ps[:, k, 0:16], wbp[:, kc, :], hT[:, kc, T - 16:T], kc == 0, kc == 15, [S_wbp, S_hT], [PS[k]])
            ts(halo[:, pc, :], ps[:, k, 0:16], flag[:, 0:1], None, ALU.mult, None, [PS[k], S_c], [S_halo])
        reset(m)

        norm_to_hT(x_cur, S_xcur, g1_bc, 1 * D, 0 * D)
        P.op("dve", lambda: nc.vector.tensor_copy(out=XSr[:, :, 0], in_=carry[:, 0, :]), reads=[S_halo], writes=[S_xs])
        P.op("dve", lambda: nc.vector.tensor_copy(out=XSi[:, :, 0], in_=carry[:, 1, :]), reads=[S_halo], writes=[S_xs])
        ssm_pass("A")
        level2()
        if dbg:
            S_dump0 = Slot()
            dma("sp", xs_dump[:, 0], XSr, reads=[S_xs], writes=[S_dump0])
            dma("sp", xs_dump[:, 1], XSi, reads=[S_xs], writes=[S_dump0])
        ssm_pass("B")

        reset(mark_ys)
        pm2 = A.bf16(8 * T).rearrange("p (c t) -> p c t", c=8)
        S_pm2 = Slot("pm2")
        m = A.off
        wbp = A.bf16(16 * 128).rearrange("p (k n) -> p k n", k=16)
        pw = A.bf16(4 * 2 * 256).rearrange("p (g k d) -> p g k d", g=4, k=2)
        psc = A.f32(8)
        U = A.f32(T + 16)
        V = A.f32(T + 16)
        Wb = A.f32(T + 16)
        rc = A.f32(T)
        pooled = A.bf16(2 * T).rearrange("p (c t) -> p c t", c=2)
        S_wbp, S_pw, S_U, S_V, S_W, S_rc, S_pl = Slot(), Slot(), Slot(), Slot(), Slot(), Slot(), Slot()
        dma("pool", pw, pool_w_l, writes=[S_pw])
        dma("sp", psc, pscale_l, writes=[S_pw])
        N = T + 16
        for g in range(4):
            dma("sp", rc, rc_bc[:, g, :], writes=[S_rc])
            for gc in range(2):
                pc = g * 2 + gc
                zT_chunk(pc * 128, None, None, wbp, S_wbp, fp32_out=U, S_f=S_U)
                P.op("dve", lambda pc=pc: nc.vector.tensor_copy(out=U[:, 0:16], in_=halo[:, pc, :]), reads=[S_halo], writes=[S_U])
                src, ssrc = U, S_U
                bufs = [(V, S_V), (Wb, S_W)]
                sh = 1
                for lev in range(g + 1):
                    dst, sdst = bufs[lev % 2]
                    tt(dst[:, sh:N], src[:, sh:N], src[:, 0:N - sh], ALU.add, [ssrc], [sdst])
                    src, ssrc = dst, sdst
                    sh *= 2
                dst, sdst = bufs[(g + 1) % 2]
                tt(dst[:, 16:N], src[:, 16:N], rc, ALU.mult, [ssrc, S_rc], [sdst])
                tt(pooled[:, gc, :], dst[:, 16:N], U[:, 16:N], ALU.subtract, [sdst, S_U], [S_pl])
            for dch in range(2):
                for tc in range(4):
                    k = nb()
                    for kch in range(2):
                        mm(ps[:, k, :], pw[:, g, kch, dch * 128:(dch + 1) * 128], pooled[:, kch, tc * 512:(tc + 1) * 512],
                           kch == 0, kch == 1, [S_pw, S_pl], [PS[k]])
                    ts(pm2[:, g * 2 + dch, tc * 512:(tc + 1) * 512], ps[:, k, :], psc[:, g * 2 + dch:g * 2 + dch + 1], None,
                       ALU.mult, None, [PS[k], S_pw], [S_pm2])
        reset(m)

        m = A.off
        bglu = A.f32(32)
        S_bg = Slot()
        dma("sp", bglu, bglu_l, writes=[S_bg])
        wgp = A.bf16(16 * 256).rearrange("p (k n) -> p k n", k=16)
        wgs = A.bf16(16 * 256).rearrange("p (k n) -> p k n", k=16)
        wpo = A.bf16(8 * 256).rearrange("p (k n) -> p k n", k=8)
        wga = A.bf16(8 * 256).rearrange("p (k n) -> p k n", k=8)
        wgb = A.bf16(8 * 256).rearrange("p (k n) -> p k n", k=8)
        S_w3 = Slot()
        e_sgp, e_sgs, e_sgb, e_m1, e_ya = [A.f32(512) for _ in range(5)]
        mo = [A.bf16(512), A.bf16(512)]
        S_e = Slot()
        S_mo = [Slot(), Slot()]
        S_mT = Slot("mT")
        w_in_v = w_in.rearrange("(k p) n -> p k n", p=128)
        wpo_v = w_pool_out.rearrange("(k p) n -> p k n", p=128)
        wgl_v = w_glu.rearrange("(k p) n -> p k n", p=128)
        moi = 0
        for blk in range(8):
            c0 = blk * 256
            dma("pool", wgp, w_in_v[:, :, 2048 + c0:2048 + c0 + 256], writes=[S_w3])
            dma("pool", wgs, w_in_v[:, :, 4096 + c0:4096 + c0 + 256], writes=[S_w3])
            dma("pool", wpo, wpo_v[:, :, c0:c0 + 256], writes=[S_w3])
            dma("pool", wga, wgl_v[:, :, c0:c0 + 256], writes=[S_w3])
            dma("pool", wgb, wgl_v[:, :, 2048 + c0:2048 + c0 + 256], writes=[S_w3])
            for tc in range(4):
                tsl = slice(tc * 512, (tc + 1) * 512)
                for d2 in range(2):
                    dd = blk * 2 + d2
                    ws = slice(d2 * 128, (d2 + 1) * 128)
                    kgp, kgs, kyp, kga, kgb = nb(), nb(), nb(), nb(), nb()
                    for kc in range(16):
                        mm(ps[:, kgp, :], wgp[:, kc, ws], hT[:, kc, tsl], kc == 0, kc == 15, [S_w3, S_hT], [PS[kgp]])
                    for kc in range(16):
                        mm(ps[:, kgs, :], wgs[:, kc, ws], hT[:, kc, tsl], kc == 0, kc == 15, [S_w3, S_hT], [PS[kgs]])
                    for kc in range(8):
                        mm(ps[:, kyp, :], wpo[:, kc, ws], pm2[:, kc, tsl], kc == 0, kc == 7, [S_w3, S_pm2], [PS[kyp]])
                    for kc in range(8):
                        mm(ps[:, kga, :], wga[:, kc, ws], ys[:, kc, tsl], kc == 0, kc == 7, [S_w3, S_ys], [PS[kga]])
                    for kc in range(8):
                        mm(ps[:, kgb, :], wgb[:, kc, ws], ys[:, kc, tsl], kc == 0, kc == 7, [S_w3, S_ys], [PS[kgb]])
                    act(e_sgp, ps[:, kgp, :], AF.Sigmoid, [PS[kgp]], [S_e])
                    act(e_sgs, ps[:, kgs, :], AF.Sigmoid, [PS[kgs]], [S_e])
                    act(e_sgb, ps[:, kgb, :], AF.Sigmoid, [PS[kgb], S_bg], [S_e], bias=bglu[:, 16 + dd:17 + dd])
                    tt(e_m1, e_sgp, ps[:, kyp, :], ALU.mult, [S_e, PS[kyp]], [S_e])
                    stt(e_ya, ps[:, kga, :], bglu[:, dd:dd + 1], e_sgb, ALU.add, ALU.mult, [PS[kga], S_bg, S_e], [S_e])
                    tt(e_ya, e_ya, e_sgs, ALU.mult, [S_e], [S_e])
                    mb, smb = mo[moi % 2], S_mo[moi % 2]
                    moi += 1
                    tt(mb, e_m1, e_ya, ALU.add, [S_e], [smb])
                    dma("sp", mT_d[dd, :, tsl], mb, reads=[smb], writes=[S_mT])
        if dbg:
            S_dump = Slot()
            dma("sp", ys_dump, ys, reads=[S_ys], writes=[S_dump])
            dma("sp", pm2_dump, pm2, reads=[S_pm2], writes=[S_dump])
            dma("sp", hT_dump, hT, reads=[S_hT], writes=[S_dump])
        reset(m)

        m = A.off
        mTt = A.bf16(16 * 512).rearrange("p (k t) -> p k t", k=16)
        wo = A.bf16(16 * 512).rearrange("p (k n) -> p k n", k=16)
        gtm = A.f32(D)
        xs4 = [A.f32(512), A.f32(512)]
        x1p = [A.f32(512), A.f32(512)]
        S_mTt, S_wo, S_gtm = Slot(), Slot(), Slot()
        S_xs4 = [Slot(), Slot()]
        S_x1p = [Slot(), Slot()]
        S_x2 = Slot("x2")
        dma("sp", gtm, modbc[:, 2 * D:3 * D], reads=[S_mod], writes=[S_gtm])
        wo_v = w_out.rearrange("(k p) n -> p k n", p=128)
        ci = 0
        for tc in range(4):
            dma("sp", mTt, mT_d.rearrange("k p t -> p k t")[:, :, tc * 512:(tc + 1) * 512], reads=[S_mT], writes=[S_mTt])
            for cb in range(4):
                csl = slice(cb * 512, (cb + 1) * 512)
                dma("pool", wo, wo_v[:, :, csl], writes=[S_wo])
                for il in range(4):
                    i = tc * 4 + il
                    k = nb()
                    for kc in range(16):
                        mm(ps[:, k, :], mTt[:, kc, il * 128:(il + 1) * 128], wo[:, kc, :], kc == 0, kc == 15, [S_mTt, S_wo], [PS[k]])
                    xb_, sxb = xs4[ci % 2], S_xs4[ci % 2]
                    xo, sxo = x1p[ci % 2], S_x1p[ci % 2]
                    ci += 1
                    dma("sp", xb_, x_cur[i * 128:(i + 1) * 128, csl], writes=[sxb])
                    tt(xo, ps[:, k, :], gtm[:, csl], ALU.mult, [PS[k], S_gtm], [sxo])
                    tt(xo, xo, xb_, ALU.add, [sxo, sxb], [sxo])
                    dma("sp", x2_d[i * 128:(i + 1) * 128, csl], xo, reads=[sxo], writes=[S_x2])
        reset(m)

        reset(mark_low)
        gate = A.f32(NT * NE).rearrange("p (i e) -> p i e", i=NT)
        S_gate = Slot("gate")
        wr = A.bf16(16 * NE).rearrange("p (k e) -> p k e", k=16)
        brow = A.bf16(NE)
        lg = A.f32(NE)
        m8 = A.f32(8)
        ngm = A.f32(2)
        msk = A.f32(NE)
        S_wr, S_lg = Slot(), Slot()
        dma("pool", wr, w_router.rearrange("(k p) e -> p k e", p=128), writes=[S_wr])
        dma("pool", brow[0:1, :], b_router, writes=[S_wr])

        def router(i):
            k = nb()
            mm(ps[:, k, 0:NE], ones_b[0:1, :], brow[0:1, :], True, False, [S_c, S_wr], [PS[k]])
            for kc in range(16):
                mm(ps[:, k, 0:NE], hT[:, kc, i * 128:(i + 1) * 128], wr[:, kc, :], False, kc == 15, [S_hT, S_wr], [PS[k]])
            P.op("dve", lambda: nc.vector.tensor_copy(out=lg, in_=ps[:, k, 0:NE]), reads=[PS[k]], writes=[S_lg])
            P.op("dve", lambda: nc.vector.max(out=m8, in_=lg), reads=[S_lg], writes=[S_lg])
            ts(msk, lg, m8[:, 3:4], None, ALU.is_ge, None, [S_lg], [S_lg])
            ts(ngm[:, 0:1], m8[:, 0:1], -1.0, None, ALU.mult, None, [S_lg], [S_lg])
            act(lg, lg, AF.Exp, [S_lg], [S_lg], bias=ngm[:, 0:1])
            tt(lg, lg, msk, ALU.mult, [S_lg], [S_lg])
            P.op("dve", lambda: nc.vector.reduce_sum(out=ngm[:, 1:2], in_=lg, axis=mybir.AxisListType.X), reads=[S_lg], writes=[S_lg])
            P.op("dve", lambda: nc.vector.reciprocal(out=ngm[:, 1:2], in_=ngm[:, 1:2]), reads=[S_lg], writes=[S_lg])
            ts(gate[:, i, :], lg, ngm[:, 1:2], None, ALU.mult, None, [S_lg], [S_gate])

        norm_to_hT(x2_d, S_x2, g2_bc, 4 * D, 3 * D, extra=router)

        actT = A.bf16(NT * 16 * 128).rearrange("p (i f t) -> p i f t", i=NT, f=16)
        S_actT = Slot("actT")
        gtf = A.f32(D)
        S_gtf = Slot()
        dma("sp", gtf, modbc[:, 5 * D:6 * D], reads=[S_mod], writes=[S_gtf])
        w1buf = [(A.bf16(16 * 256).rearrange("p (k n) -> p k n", k=16), A.bf16(16 * 256).rearrange("p (k n) -> p k n", k=16))
                 for _ in range(2)]
        w2buf = [A.bf16(16 * 256).rearrange("p (k n) -> p k n", k=16) for _ in range(2)]
        S_w1b = [Slot(), Slot()]
        S_w2b = [Slot(), Slot()]
        b1r = A.bf16(2 * D)
        b2r = A.bf16(D)
        S_b1, S_b2 = Slot(), Slot()
        xg, sg_, xl = A.f32(256), A.f32(256), A.f32(256)
        ab = [A.bf16(256), A.bf16(256)]
        S_ev = Slot()
        S_ab = [Slot(), Slot()]
        yst = [A.f32(256) for _ in range(2)]
        S_yst = [Slot() for _ in range(2)]
        S_x2r = [[Slot() for _ in range(8)] for _ in range(NT)]
        for i in range(NT):
            for d8 in range(8):
                S_x2r[i][d8].w = S_x2.w
        blocks = []
        for e in range(0 if dbg else NE):
            for j in range(8):
                blocks.append(["w1", e, j, 0])
            for j in range(8):
                blocks.append(["w2", e, j, 0])
        pcnt = {"w1": 0, "w2": 0}

        def issue_load(bi):
            if bi >= len(blocks):
                return
            kind, e, j, _ = blocks[bi]
            par = pcnt[kind] % 2
            pcnt[kind] += 1
            blocks[bi][3] = par
            if kind == "w1":
                w1v = w1[e].rearrange("(k p) n -> p k n", p=128)
                if j == 0:
                    dma("pool", b1r[0:1, :], b1[e:e + 1, :], writes=[S_b1], cls="w")
                f0 = j * 256
                dma("pool", w1buf[par][0], w1v[:, :, f0:f0 + 256], writes=[S_w1b[par]], cls="w")
                dma("pool", w1buf[par][1], w1v[:, :, D + f0:D + f0 + 256], writes=[S_w1b[par]], cls="w")
            else:
                w2v = w2[e].rearrange("(k p) n -> p k n", p=128)
                if j == 0:
                    dma("pool", b2r[0:1, :], b2[e:e + 1, :], writes=[S_b2], cls="w")
                dma("pool", w2buf[par], w2v[:, :, j * 256:(j + 1) * 256], writes=[S_w2b[par]], cls="w")

        if dbg:
            dma("sp", gate_dump, gate, reads=[S_gate], writes=[Slot()])
        issue_load(0)
        yi = 0
        abi = 0
        for bi in range(len(blocks)):
            issue_load(bi + 1)
            kind, e, j, par = blocks[bi]
            if kind == "w1":
                fs, f0 = j, j * 256
                pend = None

                def flush(pend):
                    i_, ab_, sab_ = pend
                    k2 = nb()
                    pb = ps[:, k2, :].bitcast(BF16).rearrange("p (a b) -> p a b", a=8)
                    for jj in range(2):
                        P.op("pe", lambda pb=pb, jj=jj, ab_=ab_: nc.tensor.transpose(pb[:, jj, :], ab_[:, jj * 128:(jj + 1) * 128], ident),
                             reads=[sab_, S_c], writes=[PS[k2]])
                    act(actT[:, i_, fs * 2:fs * 2 + 2, :], pb[:, 0:2, :], AF.Copy, [PS[k2]], [S_actT])
                for i in range(NT):
                    k = nb()
                    for (wblk_, c0, b0) in ((w1buf[par][0], 0, f0), (w1buf[par][1], 256, D + f0)):
                        mm(ps[:, k, c0:c0 + 256], ones_b[0:1, :], b1r[0:1, b0:b0 + 256], True, False, [S_c, S_b1], [PS[k]])
                        for kc in range(16):
                            mm(ps[:, k, c0:c0 + 256], hT[:, kc, i * 128:(i + 1) * 128], wblk_[:, kc, :], False, kc == 15,
                               [S_hT, S_w1b[par]], [PS[k]])
                    ab_, sab_ = ab[abi % 2], S_ab[abi % 2]
                    abi += 1
                    ts(xg, ps[:, k, 0:256], 7.0, None, ALU.min, None, [PS[k]], [S_ev])
                    act(sg_, xg, AF.Sigmoid, [S_ev], [S_ev], scale=1.702)
                    ts(xl, ps[:, k, 256:512], 7.0, -7.0, ALU.min, ALU.max, [PS[k]], [S_ev])
                    stt(xl, xl, 1.0, xg, ALU.add, ALU.mult, [S_ev], [S_ev])
                    tt(ab_, xl, sg_, ALU.mult, [S_ev], [sab_])
                    if pend is not None:
                        flush(pend)
                    pend = (i, ab_, sab_)
                flush(pend)
            else:
                dsl = slice(j * 256, (j + 1) * 256)
                for i in range(NT):
                    k = nb()
                    mm(ps[:, k, 0:256], ones_b[0:1, :], b2r[0:1, dsl], True, False, [S_c, S_b2], [PS[k]])
                    for fc in range(16):
                        mm(ps[:, k, 0:256], actT[:, i, fc, :], w2buf[par][:, fc, :], False, fc == 15, [S_actT, S_w2b[par]], [PS[k]])
                    yb, syb = yst[yi % 2], S_yst[yi % 2]
                    yi += 1
                    stt(yb, ps[:, k, 0:256], gate[:, i, e:e + 1], gtf[:, dsl], ALU.mult, ALU.mult, [PS[k], S_gate, S_gtf], [syb])
                    dma("pool", x2_d[i * 128:(i + 1) * 128, dsl], yb, reads=[syb, S_x2r[i][j]], writes=[S_x2r[i][j]],
                        accum_op=ALU.add, cls="a")

        reset(mark_low)
        G3, S3, sl3 = load_GS(g3_bc, 7 * D, 6 * D)
        S_out = Slot()

        def fin_consume(i, h, sh):
            dma("sp", out_d[i * 128:(i + 1) * 128, :], h, reads=[sh], writes=[S_out])
        norm_rows(x2_d, G3, S3, sl3, (lambda i: S_x2r[i]), fin_consume)

        P.emit()
    return nc


def _prep_shared(inp):
    f = np.float32
    s = {}
    s["ident"] = np.eye(128, dtype=f)
    s["ada_w"] = np.ascontiguousarray(inp["ada_w"][0])
    s["ada_b_bc"] = np.ascontiguousarray(np.broadcast_to(inp["ada_b"][0][None, :], (128, 6 * D)))
    s["final_ada_w"] = np.ascontiguousarray(inp["final_ada_w"])
    s["final_ada_b_bc"] = np.ascontiguousarray(np.broadcast_to(inp["final_ada_b"][None, :], (128, 2 * D)))
    s["g1_bc"] = np.ascontiguousarray(np.broadcast_to(inp["norm1_g"][0][None, :], (128, D)))
    s["g2_bc"] = np.ascontiguousarray(np.broadcast_to(inp["norm2_g"][0][None, :], (128, D)))
    s["g3_bc"] = np.ascontiguousarray(np.broadcast_to(inp["final_norm_g"][None, :], (128, D)))
    s["w_in"] = np.ascontiguousarray(inp["w_in"][0])
    pw = inp["pool_w"][0]
    s["pool_w_l"] = np.ascontiguousarray(pw.reshape(4, 2, 128, 256).transpose(2, 0, 1, 3))
    s["pscale_l"] = np.ascontiguousarray(inp["pool_scale"][0].reshape(8, 128).T)
    s["w_pool_out"] = np.ascontiguousarray(inp["w_pool_out"][0])

    def pairl(a):
        return np.ascontiguousarray(a.reshape(32, 2, 64).transpose(1, 2, 0).reshape(128, 32))
    s["lamre_l"] = pairl(inp["ssm_lam_re"][0])
    s["lamim_l"] = pairl(inp["ssm_lam_im"][0])
    s["logdt_l"] = pairl(np.repeat(inp["ssm_log_dt"][0][:, None], 64, axis=1))
    bre, bim = inp["ssm_b_re"][0], inp["ssm_b_im"][0]
    Bl = np.zeros((128, 8, 2, 2, 128), f)
    cre, cim = inp["ssm_c_re"][0], inp["ssm_c_im"][0]
    Cr = np.zeros((128, 32, 128), f)
    Ci = np.zeros((128, 32, 128), f)
    for g in range(64):
        pr, gl = g // 2, g % 2
        cc, q = pr // 4, pr % 4
        r0 = 32 * q + 16 * gl
        vv = 1 if q == 3 else 0
        Bl[r0:r0 + 16, cc, vv, 0, gl * 64:(gl + 1) * 64] = bre[g].T
        Bl[r0:r0 + 16, cc, vv, 1, gl * 64:(gl + 1) * 64] = bim[g].T
        m0 = 32 * q + 16 * gl
        Cr[gl * 64:(gl + 1) * 64, pr, m0:m0 + 16] = cre[g].T
        Ci[gl * 64:(gl + 1) * 64, pr, m0:m0 + 16] = cim[g].T
    s["B_l"], s["CTre_l"], s["CTim_l"] = Bl, Cr, Ci
    s["ssmd_l"] = np.ascontiguousarray(inp["ssm_d"][0].reshape(8, 128).T)
    s["w_glu"] = np.ascontiguousarray(inp["w_glu"][0])
    s["bglu_l"] = np.ascontiguousarray(inp["b_glu"][0].reshape(32, 128).T)
    s["w_out"] = np.ascontiguousarray(inp["w_out"][0])
    s["w_router"] = np.ascontiguousarray(inp["w_router"][0])
    s["b_router"] = np.ascontiguousarray(inp["b_router"][0][None, :])
    s["w1"] = np.ascontiguousarray(inp["w1"][0])
    s["b1"] = np.ascontiguousarray(inp["b1"][0])
    s["w2"] = np.ascontiguousarray(inp["w2"][0])
    s["b2"] = np.ascontiguousarray(inp["b2"][0])
    return s


def kernel(**inp):
    inp = {k: np.asarray(v) for k, v in inp.items()}
    f = np.float32
    shared = _prep_shared(inp)
    x, c = inp["x"], inp["c"]
    in_maps = []
    win = np.array([2.0, 4.0, 8.0, 16.0], f)
    for core in range(8):
        b, half = core // 2, core % 2
        mp = dict(shared)
        mp["x_cur"] = np.ascontiguousarray(x[b, half * T:(half + 1) * T])
        mp["x_prev"] = np.ascontiguousarray(x[b, 0:T]) if half == 1 else np.zeros((T, D), f)
        mp["flag"] = np.full((128, 1), float(half), f)
        mp["cT"] = np.ascontiguousarray(c[b].reshape(16, 128).T)
        pos = np.arange(1, T + 1, dtype=f) + (T if half == 1 else 0)
        rc = (1.0 / np.minimum(pos[None, :], win[:, None])).astype(f)
        mp["rc_bc"] = np.ascontiguousarray(np.broadcast_to(rc[None], (128, 4, T)))
        in_maps.append(mp)
    nc = build()
    res = run_bass_kernel_spmd(nc, in_maps, core_ids=list(range(8)))
    out = np.zeros((4, 2 * T, D), f)
    for core in range(8):
        b, half = core // 2, core % 2
        out[b, half * T:(half + 1) * T] = res.results[core]["out"]
    return out
```

```python
import math
import numpy as np
import concourse.bass as bass
import concourse.mybir as mybir
from concourse.bass_utils import run_bass_kernel_spmd

F32 = mybir.dt.float32
BF16 = mybir.dt.bfloat16
AF = mybir.ActivationFunctionType
ALU = mybir.AluOpType

D = 2048
T = 2048
NT = 16
NE = 32
L1 = 16
NC1 = T // L1


class Slot:
    __slots__ = ("name", "w", "rs")

    def __init__(self, name=""):
        self.name = name
        self.w = None
        self.rs = []


class Op:
    __slots__ = ("eng", "fn", "deps", "need", "cnt", "dma", "dsem", "dcnt", "prev")

    def __init__(self, eng, fn, dma):
        self.eng = eng
        self.fn = fn
        self.deps = []
        self.need = False
        self.cnt = None
        self.dma = dma
        self.dsem = None
        self.dcnt = None
        self.prev = 0


class Prog:
    ENG = ("pe", "act", "dve", "pool", "sp")

    def __init__(self, nc, ndma=8):
        self.nc = nc
        self.ops = []
        self.last = {}
        self.dcur = {"sp": 0, "act": 0, "pool": 0}
        self.dlast = {}
        self.h = {"pe": nc.tensor, "act": nc.scalar, "dve": nc.vector, "pool": nc.gpsimd, "sp": nc.sync}
        self.ndma = ndma

    NDMA = {("sp", ""): 8, ("act", ""): 2, ("pool", ""): 3, ("pool", "w"): 3, ("pool", "a"): 6}

    def op(self, eng, fn, reads=(), writes=(), dma=False, cls=""):
        o = Op(eng, fn, dma)
        deps = set()
        for s in reads:
            if s.w is not None:
                deps.add(s.w)
        for s in writes:
            if s.w is not None:
                deps.add(s.w)
            for r in s.rs:
                deps.add(r)
        for d in deps:
            if (not d.dma) and (not dma) and d.eng == eng and eng == "pe":
                continue
            o.deps.append(d)
            d.need = True
        for s in reads:
            if dma:
                s.rs.append(o)
            else:
                s.rs = [r for r in s.rs if r.dma or r.eng != eng]
                s.rs.append(o)
        for s in writes:
            s.w = o
            s.rs = []
        self.ops.append(o)
        if dma:
            q = (eng, cls)
            i = self.dcur.get(q, 0) % self.NDMA[q]
            self.dcur[q] = self.dcur.get(q, 0) + 1
            o.dsem = (q, i)
            self.dlast[o.dsem] = o
        else:
            self.last[eng] = o
        return o

    def barrier(self):
        lst = list(self.last.values()) + list(self.dlast.values())
        for d in lst:
            d.need = True
        self.ops.append(("barrier", lst))

    def emit(self):
        nc = self.nc
        sems = {e: nc.alloc_semaphore("s_" + e) for e in self.ENG}
        dq = list(self.NDMA.keys())
        dsems = {q: [nc.alloc_semaphore("d_%s%s_%d" % (q[0], q[1], i)) for i in range(self.NDMA[q])] for q in dq}
        cnt = {e: 0 for e in self.ENG}
        dcount = {q: [0] * self.NDMA[q] for q in dq}
        for o in self.ops:
            if isinstance(o, tuple):
                continue
            if o.dma:
                q, i = o.dsem
                o.prev = dcount[q][i]
                dcount[q][i] += 16
                o.dcnt = dcount[q][i]
            elif o.need:
                cnt[o.eng] += 1
                o.cnt = cnt[o.eng]
        seen = {}
        pending = {e: [] for e in self.ENG}
        for o in self.ops:
            if isinstance(o, tuple):
                for e in self.ENG:
                    pending[e] = list(o[1])
                continue
            h = self.h[o.eng]
            waits = {}
            dl = o.deps
            if pending[o.eng]:
                dl = dl + pending[o.eng]
                pending[o.eng] = []
            for d in dl:
                if d.dma:
                    key = ("d", d.dsem)
                    val = d.dcnt
                    sh = dsems[d.dsem[0]][d.dsem[1]]
                else:
                    key = ("c", d.eng)
                    val = d.cnt
                    sh = sems[d.eng]
                if seen.get((o.eng, key), 0) >= val:
                    continue
                if key not in waits or waits[key][1] < val:
                    waits[key] = (sh, val)
            if o.dma and o.prev > 0:
                key = ("d", o.dsem)
                if seen.get((o.eng, key), 0) < o.prev:
                    if key not in waits or waits[key][1] < o.prev:
                        waits[key] = (dsems[o.dsem[0]][o.dsem[1]], o.prev)
            for key, (sh, val) in waits.items():
                h.wait_ge(sh, val)
                seen[(o.eng, key)] = val
            ins = o.fn()
            if o.dma:
                ins.then_inc(dsems[o.dsem[0]][o.dsem[1]], 16)
            elif o.need:
                ins.then_inc(sems[o.eng], 1)
        for q in dq:
            for i in range(self.NDMA[q]):
                if dcount[q][i] > 0:
                    nc.sync.wait_ge(dsems[q][i], dcount[q][i])
        self.ops = None


class Arena:
    def __init__(self, ap, words):
        self.ap = ap
        self.words = words
        self.off = 0

    def f32(self, n):
        assert self.off + n <= self.words, ("sbuf arena overflow", self.off, n)
        a = self.ap[:, self.off:self.off + n]
        self.off += n
        return a

    def bf16(self, n):
        w = (n + 1) // 2
        assert self.off + w <= self.words, ("sbuf arena overflow", self.off, n)
        a = self.ap[:, self.off:self.off + w].bitcast(BF16)
        self.off += w
        return a


def build(dbg=False):
    okind = "ExternalOutput" if dbg else "Internal"
    nc = bass.Bass("TRN2", target_bir_lowering=False)

    def din(name, shape):
        return nc.dram_tensor(name, list(shape), F32, kind="ExternalInput").ap()

    x_cur = din("x_cur", [T, D])
    x_prev = din("x_prev", [T, D])
    flag_d = din("flag", [128, 1])
    cT_d = din("cT", [128, 16])
    ident_d = din("ident", [128, 128])
    ada_w = din("ada_w", [D, 6 * D])
    ada_b_bc = din("ada_b_bc", [128, 6 * D])
    fada_w = din("final_ada_w", [D, 2 * D])
    fada_b_bc = din("final_ada_b_bc", [128, 2 * D])
    g1_bc = din("g1_bc", [128, D])
    g2_bc = din("g2_bc", [128, D])
    g3_bc = din("g3_bc", [128, D])
    w_in = din("w_in", [D, 6144])
    pool_w_l = din("pool_w_l", [128, 4, 2, 256])
    pscale_l = din("pscale_l", [128, 8])
    rc_bc = din("rc_bc", [128, 4, T])
    w_pool_out = din("w_pool_out", [1024, D])
    lamre_l = din("lamre_l", [128, 32])
    lamim_l = din("lamim_l", [128, 32])
    logdt_l = din("logdt_l", [128, 32])
    B_l = din("B_l", [128, 8, 2, 2, 128])
    CTre_l = din("CTre_l", [128, 32, 128])
    CTim_l = din("CTim_l", [128, 32, 128])
    ssmd_l = din("ssmd_l", [128, 8])
    w_glu = din("w_glu", [1024, 2 * D])
    bglu_l = din("bglu_l", [128, 32])
    w_out = din("w_out", [D, D])
    w_router = din("w_router", [D, NE])
    b_router = din("b_router", [1, NE])
    if not dbg:
        w1 = din("w1", [NE, D, 2 * D])
        b1 = din("b1", [NE, 2 * D])
        w2 = din("w2", [NE, D, D])
        b2 = din("b2", [NE, D])
    out_d = nc.dram_tensor("out", [T, D], F32, kind="ExternalOutput").ap()
    modbc = nc.dram_tensor("modbc", [128, 8 * D], F32, kind=okind).ap()
    x2_d = nc.dram_tensor("x2s", [T, D], F32, kind=okind).ap()
    mT_d = nc.dram_tensor("mTs", [16, 128, T], BF16, kind=okind).ap()
    if dbg:
        ys_dump = nc.dram_tensor("ys_dump", [128, 8, T], BF16, kind=okind).ap()
        pm2_dump = nc.dram_tensor("pm2_dump", [128, 8, T], BF16, kind=okind).ap()
        hT_dump = nc.dram_tensor("hT_dump", [128, 16, T], BF16, kind=okind).ap()
        xs_dump = nc.dram_tensor("xs_dump", [128, 2, 32, 129], F32, kind=okind).ap()
        gate_dump = nc.dram_tensor("gate_dump", [128, NT, NE], F32, kind=okind).ap()

    P = Prog(nc)
    W = 53200
    with nc.sbuf_tensor("arena", [128, W], F32) as ar_t, nc.psum_tensor("ps", [128, 8, 512], F32) as ps:
        A = Arena(ar_t, W)

        def reset(mark):
            A.off = mark
            P.barrier()
        PS = [Slot("ps%d" % k) for k in range(8)]
        bank_ctr = [0]

        def nb(lo=0, hi=8):
            k = lo + bank_ctr[0] % (hi - lo)
            bank_ctr[0] += 1
            return k

        def dma(eng, out, in_, reads=(), writes=(), cls="", **kw):
            h = P.h[eng]
            return P.op(eng, lambda: h.dma_start(out=out, in_=in_, **kw), reads=reads, writes=writes, dma=True, cls=cls)

        def mm(out, lhsT, rhs, start, stop, reads, writes):
            return P.op("pe", lambda: nc.tensor.matmul(out, lhsT=lhsT, rhs=rhs, start=start, stop=stop),
                        reads=reads, writes=writes)

        def act(out, in_, func, reads, writes, **kw):
            return P.op("act", lambda: nc.scalar.activation(out=out, in_=in_, func=func, **kw),
                        reads=reads, writes=writes)

        def stt(out, in0, scalar, in1, op0, op1, reads, writes):
            return P.op("dve", lambda: nc.vector.scalar_tensor_tensor(out=out, in0=in0, scalar=scalar, in1=in1,
                                                                       op0=op0, op1=op1), reads=reads, writes=writes)

        def tt(out, in0, in1, op, reads, writes, eng="dve"):
            h = P.h[eng]
            return P.op(eng, lambda: h.tensor_tensor(out=out, in0=in0, in1=in1, op=op), reads=reads, writes=writes)

        def ts(out, in0, s1, s2, op0, op1, reads, writes):
            if s2 is None:
                return P.op("dve", lambda: nc.vector.tensor_scalar(out=out, in0=in0, scalar1=s1, scalar2=None, op0=op0),
                            reads=reads, writes=writes)
            return P.op("dve", lambda: nc.vector.tensor_scalar(out=out, in0=in0, scalar1=s1, scalar2=s2, op0=op0, op1=op1),
                        reads=reads, writes=writes)

        hT = A.bf16(16 * T).rearrange("p (k t) -> p k t", k=16)
        S_hT = Slot("hT")
        ident = A.bf16(128)
        S_c = Slot("consts")
        ones_b = A.bf16(128)
        flag = A.f32(1)
        S_small = Slot("small")
        dma("pool", ident, ident_d, writes=[S_c])
        dma("sp", flag, flag_d, writes=[S_c])
        P.op("dve", lambda: nc.vector.memset(ones_b, 1.0), writes=[S_c])
        mark_low = A.off
        small = A.f32(32 * 24).rearrange("p (a b) -> p a b", b=32)
        carry = A.f32(64).rearrange("p (r a) -> p r a", r=2)
        halo = A.f32(8 * 16).rearrange("p (c h) -> p c h", c=8)
        S_halo = Slot("halo")
        ys = A.bf16(8 * T).rearrange("p (c t) -> p c t", c=8)
        S_ys = Slot("ys")
        mark_ys = A.off
        mark0 = A.off

        cT = A.f32(16)
        csg = A.f32(16)
        carep = A.f32(16 * 128).rearrange("p (k m) -> p k m", k=16)
        wblk = A.f32(16 * 512).rearrange("p (k n) -> p k n", k=16)
        bblk = A.f32(512)
        oblk = A.f32(512)
        S_ca, S_wblk, S_bblk, S_oblk, S_mod = Slot(), Slot(), Slot(), Slot(), Slot("modbc")
        dma("sp", cT, cT_d, writes=[S_ca])
        act(csg, cT, AF.Sigmoid, [S_ca], [S_ca])
        tt(cT, cT, csg, ALU.mult, [S_ca], [S_ca])
        P.op("dve", lambda: nc.vector.tensor_copy(out=carep, in_=cT.unsqueeze(2).to_broadcast([128, 16, 128])),
             reads=[S_ca], writes=[S_ca])
        for (wd, bd, nblk, off) in ((ada_w, ada_b_bc, 24, 0), (fada_w, fada_b_bc, 8, 6 * D)):
            wv = wd.rearrange("(k p) n -> p k n", p=128)
            for n in range(nblk):
                dma("sp", wblk, wv[:, :, n * 512:(n + 1) * 512], writes=[S_wblk])
                dma("sp", bblk, bd[:, n * 512:(n + 1) * 512], writes=[S_bblk])
                k = nb()
                for kc in range(16):
                    mm(ps[:, k, :], carep[:, kc, :], wblk[:, kc, :], kc == 0, kc == 15, [S_ca, S_wblk], [PS[k]])
                tt(oblk, ps[:, k, :], bblk, ALU.add, [PS[k], S_bblk], [S_oblk])
                dma("sp", modbc[:, off + n * 512: off + (n + 1) * 512], oblk, reads=[S_oblk], writes=[S_mod])
        reset(mark0)

        def load_GS(gbc, sc_off, sh_off):
            G = A.f32(D)
            S = A.f32(D)
            tmp = A.f32(D)
            sl = Slot()
            dma("sp", G, gbc, writes=[sl])
            dma("sp", tmp, modbc[:, sc_off:sc_off + D], reads=[S_mod], writes=[sl])
            dma("sp", S, modbc[:, sh_off:sh_off + D], reads=[S_mod], writes=[sl])
            stt(G, tmp, 1.0, G, ALU.add, ALU.mult, [sl], [sl])
            return G, S, sl

        def norm_rows(src, G, S, sl_gs, S_src, consume, nbuf_mark=None):
            xs = [A.f32(D)]
            sq = A.f32(D)
            st = A.f32(4)
            S_x = [Slot()]
            S_sq, S_st = Slot(), Slot()
            for i in range(NT):
                xb, sx = xs[0], S_x[0]
                dma("sp", xb, src[i * 128:(i + 1) * 128, :], reads=(S_src(i) if callable(S_src) else [S_src]), writes=[sx])
                act(sq, xb, AF.Square, [sx], [S_sq, S_st], accum_out=st[:, 0:1])
                ts(st[:, 1:2], st[:, 0:1], 1.0 / D, 1e-6, ALU.mult, ALU.add, [S_st], [S_st])
                act(st[:, 2:3], st[:, 1:2], AF.Sqrt, [S_st], [S_st])
                P.op("dve", lambda: nc.vector.reciprocal(out=st[:, 3:4], in_=st[:, 2:3]), reads=[S_st], writes=[S_st])
                stt(sq, xb, st[:, 3:4], G, ALU.mult, ALU.mult, [sx, S_st, sl_gs], [S_sq])
                tt(xb, sq, S, ALU.add, [S_sq, sl_gs], [sx])
                consume(i, xb, sx)

        def to_hT(i, h, sh, hb, S_hb):
            act(hb, h, AF.Copy, [sh], [S_hb])
            for half in range(2):
                k = nb()
                pb = ps[:, k, :].bitcast(BF16).rearrange("p (a b) -> p a b", a=8)
                for j in range(8):
                    kc = half * 8 + j
                    P.op("pe", lambda pb=pb, j=j, kc=kc: nc.tensor.transpose(pb[:, j, :], hb[:, kc * 128:(kc + 1) * 128], ident),
                         reads=[S_hb, S_c], writes=[PS[k]])
                P.op("dve", lambda pb=pb, half=half: nc.vector.tensor_copy(out=hT[:, half * 8:(half + 1) * 8, i * 128:(i + 1) * 128], in_=pb),
                     reads=[PS[k]], writes=[S_hT])

        def norm_to_hT(src, S_src, gbc, sc_off, sh_off, extra=None):
            m = A.off
            G, S, sl = load_GS(gbc, sc_off, sh_off)
            hb = A.bf16(D)
            S_hb = Slot()

            def consume(i, h, sh):
                to_hT(i, h, sh, hb, S_hb)
                if extra is not None:
                    extra(i)
            norm_rows(src, G, S, sl, S_src, consume)
            reset(m)

        (LRE, LIM, DT, AR, AI, NAI, A16R, A16I, FRE, FIM, NFRE, NFIM, T0, T1, T2, T3, T4) = range(17)

        def sm(i):
            return small[:, i, :]
        dma("sp", sm(LRE), lamre_l, writes=[S_small])
        dma("sp", sm(LIM), lamim_l, writes=[S_small])
        dma("sp", sm(DT), logdt_l, writes=[S_small])
        RS, WS = [S_small], [S_small]
        act(sm(DT), sm(DT), AF.Exp, RS, WS)
        ts(sm(LRE), sm(LRE), -1e-4, None, ALU.min, None, RS, WS)
        tt(sm(T0), sm(LRE), sm(DT), ALU.mult, RS, WS)
        act(sm(T0), sm(T0), AF.Exp, RS, WS)
        tt(sm(T1), sm(LIM), sm(DT), ALU.mult, RS, WS)
        halfpi = A.f32(1)
        P.op("dve", lambda: nc.vector.memset(halfpi, math.pi / 2), writes=WS)
        act(sm(T2), sm(T1), AF.Sin, RS, WS, scale=1.0 / 64)
        act(sm(T3), sm(T1), AF.Sin, RS, WS, scale=1.0 / 64, bias=halfpi[:, 0:1])

        def csquare(cr, ci, t):
            tt(sm(t), sm(cr), sm(ci), ALU.mult, RS, WS)
            tt(sm(cr), sm(cr), sm(cr), ALU.mult, RS, WS)
            tt(sm(ci), sm(ci), sm(ci), ALU.mult, RS, WS)
            tt(sm(cr), sm(cr), sm(ci), ALU.subtract, RS, WS)
            ts(sm(ci), sm(t), 2.0, None, ALU.mult, None, RS, WS)
        for _ in range(6):
            csquare(T3, T2, T4)
        tt(sm(AR), sm(T0), sm(T3), ALU.mult, RS, WS)
        tt(sm(AI), sm(T0), sm(T2), ALU.mult, RS, WS)
        ts(sm(NAI), sm(AI), -1.0, None, ALU.mult, None, RS, WS)
        P.op("dve", lambda: nc.vector.tensor_copy(out=sm(A16R), in_=sm(AR)), reads=RS, writes=WS)
        P.op("dve", lambda: nc.vector.tensor_copy(out=sm(A16I), in_=sm(AI)), reads=RS, writes=WS)
        for _ in range(4):
            csquare(A16R, A16I, T4)
        tt(sm(T0), sm(LRE), sm(LRE), ALU.mult, RS, WS)
        tt(sm(T1), sm(LIM), sm(LIM), ALU.mult, RS, WS)
        tt(sm(T0), sm(T0), sm(T1), ALU.add, RS, WS)
        P.op("dve", lambda: nc.vector.reciprocal(out=sm(T0), in_=sm(T0)), reads=RS, writes=WS)
        ts(sm(T1), sm(AR), -1.0, None, ALU.add, None, RS, WS)
        tt(sm(T2), sm(T1), sm(LRE), ALU.mult, RS, WS)
        tt(sm(T3), sm(AI), sm(LIM), ALU.mult, RS, WS)
        tt(sm(T2), sm(T2), sm(T3), ALU.add, RS, WS)
        tt(sm(FRE), sm(T2), sm(T0), ALU.mult, RS, WS)
        tt(sm(T2), sm(AI), sm(LRE), ALU.mult, RS, WS)
        tt(sm(T3), sm(T1), sm(LIM), ALU.mult, RS, WS)
        tt(sm(T2), sm(T2), sm(T3), ALU.subtract, RS, WS)
        tt(sm(FIM), sm(T2), sm(T0), ALU.mult, RS, WS)
        ts(sm(NFRE), sm(FRE), -1.0, None, ALU.mult, None, RS, WS)
        ts(sm(NFIM), sm(FIM), -1.0, None, ALU.mult, None, RS, WS)

        Cb = A.bf16(32 * 2 * 128).rearrange("p (a r m) -> p a r m", a=32, r=2)
        Bsb = A.bf16(8 * 2 * 2 * 128).rearrange("p (c v r m) -> p c v r m", c=8, v=2, r=2)
        ssmd = A.f32(8)
        S_cb = Slot("Cb")
        dma("pool", Bsb, B_l, writes=[S_cb])
        dma("sp", ssmd, ssmd_l, writes=[S_cb])
        m = A.off
        cre = A.f32(32 * 128).rearrange("p (a m) -> p a m", a=32)
        cim = A.f32(32 * 128).rearrange("p (a m) -> p a m", a=32)
        ctmp = A.f32(128)
        S_cl = Slot()
        dma("sp", cre, CTre_l, writes=[S_cl])
        dma("sp", cim, CTim_l, writes=[S_cl])
        for pr in range(32):
            ts(ctmp, cim[:, pr, :], small[:, FIM, pr:pr + 1], None, ALU.mult, None, [S_cl, S_small], [S_cl])
            stt(Cb[:, pr, 0, :], cre[:, pr, :], small[:, FRE, pr:pr + 1], ctmp, ALU.mult, ALU.subtract, [S_cl, S_small], [S_cb])
            ts(ctmp, cim[:, pr, :], small[:, NFRE, pr:pr + 1], None, ALU.mult, None, [S_cl, S_small], [S_cl])
            stt(Cb[:, pr, 1, :], cre[:, pr, :], small[:, NFIM, pr:pr + 1], ctmp, ALU.mult, ALU.add, [S_cl, S_small], [S_cb])
        reset(m)
        XSr = A.f32(32 * 129).rearrange("p (a c) -> p a c", a=32)
        XSi = A.f32(32 * 129).rearrange("p (a c) -> p a c", a=32)
        S_xs = Slot("XS")
        mark1 = A.off

        def zT_chunk(col0, ubf, S_u, wb, S_wb, tcs=range(4), fp32_out=None, S_f=None):
            dma("pool", wb, w_in.rearrange("(k p) n -> p k n", p=128)[:, :, col0:col0 + 128], writes=[S_wb])
            for tc in tcs:
                k = nb(4, 8)
                for kc in range(16):
                    mm(ps[:, k, :], wb[:, kc, :], hT[:, kc, tc * 512:(tc + 1) * 512], kc == 0, kc == 15, [S_wb, S_hT], [PS[k]])
                if fp32_out is None:
                    act(ubf[:, tc * 512:(tc + 1) * 512], ps[:, k, :], AF.Copy, [PS[k]], [S_u])
                else:
                    act(fp32_out[:, 16 + tc * 512:16 + (tc + 1) * 512], ps[:, k, :], AF.Copy, [PS[k]], [S_f])

        def ssm_pass(mode):
            m = A.off
            wb = A.bf16(16 * 128).rearrange("p (k n) -> p k n", k=16)
            ubf = A.bf16(T)
            vr = A.f32(T)
            vi = A.f32(T)
            S_wb, S_u, S_v = Slot(), Slot(), Slot()
            vr3 = vr.rearrange("p (c j) -> p c j", j=L1)
            vi3 = vi.rearrange("p (c j) -> p c j", j=L1)
            if mode == "B":
                xbr = A.bf16(T)
                xbi = A.bf16(T)
                gtmp = A.f32(512)
                S_xb, S_g = Slot(), Slot()
            for cc in range(8):
                zT_chunk(1024 + cc * 128, ubf, S_u, wb, S_wb)
                for q in range(4):
                    pr = cc * 4 + q
                    for tc in range(4):
                        for ri, v in ((0, vr), (1, vi)):
                            k = nb(4, 8)
                            p0, p1, vv = ((0, 32, 0), (32, 64, 0), (64, 96, 0), (64, 128, 1))[q]
                            mm(ps[:, k, :],
                               Bsb[p0:p1, cc, vv, ri, :], ubf[p0:p1, tc * 512:(tc + 1) * 512],
                               True, True, [S_cb, S_u], [PS[k]])
                            act(v[:, tc * 512:(tc + 1) * 512], ps[:, k, :], AF.Copy, [PS[k]], [S_v])
                    ar_, ai_, nai_ = small[:, AR, pr:pr + 1], small[:, AI, pr:pr + 1], small[:, NAI, pr:pr + 1]
                    RV = [S_v, S_small]
                    if mode == "B":
                        xpr, xpi = XSr[:, pr, 0:NC1], XSi[:, pr, 0:NC1]
                        RX = [S_v, S_small, S_xs]
                        stt(vr3[:, :, 0], xpi, nai_, vr3[:, :, 0], ALU.mult, ALU.add, RX, [S_v])
                        stt(vr3[:, :, 0], xpr, ar_, vr3[:, :, 0], ALU.mult, ALU.add, RX, [S_v])
                        stt(vi3[:, :, 0], xpr, ai_, vi3[:, :, 0], ALU.mult, ALU.add, RX, [S_v])
                        stt(vi3[:, :, 0], xpi, ar_, vi3[:, :, 0], ALU.mult, ALU.add, RX, [S_v])
                    for j in range(1, L1):
                        stt(vr3[:, :, j], vi3[:, :, j - 1], nai_, vr3[:, :, j], ALU.mult, ALU.add, RV, [S_v])
                        stt(vi3[:, :, j], vr3[:, :, j - 1], ai_, vi3[:, :, j], ALU.mult, ALU.add, RV, [S_v])
                        stt(vr3[:, :, j], vr3[:, :, j - 1], ar_, vr3[:, :, j], ALU.mult, ALU.add, RV, [S_v])
                        stt(vi3[:, :, j], vi3[:, :, j - 1], ar_, vi3[:, :, j], ALU.mult, ALU.add, RV, [S_v])
                    if mode == "A":
                        P.op("dve", lambda pr=pr: nc.vector.tensor_copy(out=XSr[:, pr, 1:NC1 + 1], in_=vr3[:, :, L1 - 1]),
                             reads=[S_v], writes=[S_xs])
                        P.op("dve", lambda pr=pr: nc.vector.tensor_copy(out=XSi[:, pr, 1:NC1 + 1], in_=vi3[:, :, L1 - 1]),
                             reads=[S_v], writes=[S_xs])
                    else:
                        act(xbr, vr, AF.Copy, [S_v], [S_xb])
                        act(xbi, vi, AF.Copy, [S_v], [S_xb])
                        for tc in range(4):
                            mm(ps[:, tc, :], Cb[:, pr, 0, :], xbr[:, tc * 512:(tc + 1) * 512], q == 0, False, [S_cb, S_xb], [PS[tc]])
                            mm(ps[:, tc, :], Cb[:, pr, 1, :], xbi[:, tc * 512:(tc + 1) * 512], False, q == 3, [S_cb, S_xb], [PS[tc]])
                if mode == "B":
                    for tc in range(4):
                        stt(gtmp, ubf[:, tc * 512:(tc + 1) * 512], ssmd[:, cc:cc + 1], ps[:, tc, :], ALU.mult, ALU.add,
                            [S_u, S_cb, PS[tc]], [S_g])
                        act(ys[:, cc, tc * 512:(tc + 1) * 512], gtmp, AF.Gelu, [S_g], [S_ys])
            reset(m)

        def level2():
            m = A.off
            t = A.f32(4 * 32).rearrange("p (a b) -> p a b", a=4)
            S_t = Slot()
            a16r, a16i = small[:, A16R, :], small[:, A16I, :]
            R = [S_xs, S_small, S_t]
            for c in range(NC1):
                tt(t[:, 0, :], a16r, XSr[:, :, c], ALU.mult, R, [S_t])
                tt(t[:, 1, :], a16i, XSi[:, :, c], ALU.mult, R, [S_t])
                tt(t[:, 2, :], a16r, XSi[:, :, c], ALU.mult, R, [S_t])
                tt(t[:, 3, :], a16i, XSr[:, :, c], ALU.mult, R, [S_t])
                tt(t[:, 0, :], t[:, 0, :], t[:, 1, :], ALU.subtract, R, [S_t])
                tt(t[:, 2, :], t[:, 2, :], t[:, 3, :], ALU.add, R, [S_t])
                tt(XSr[:, :, c + 1], XSr[:, :, c + 1], t[:, 0, :], ALU.add, R, [S_xs])
                tt(XSi[:, :, c + 1], XSi[:, :, c + 1], t[:, 2, :], ALU.add, R, [S_xs])
            reset(m)

        S_xprev, S_xcur = Slot("xprev"), Slot("xcur")
        norm_to_hT(x_prev, S_xprev, g1_bc, 1 * D, 0 * D)
        P.op("dve", lambda: nc.vector.memset(XSr[:, :, 0:1], 0.0), writes=[S_xs])
        P.op("dve", lambda: nc.vector.memset(XSi[:, :, 0:1], 0.0), writes=[S_xs])
        ssm_pass("A")
        level2()
        ts(carry[:, 0, :], XSr[:, :, NC1], flag[:, 0:1], None, ALU.mult, None, [S_xs, S_c], [S_halo])
        ts(carry[:, 1, :], XSi[:, :, NC1], flag[:, 0:1], None, ALU.mult, None, [S_xs, S_c], [S_halo])
        m = A.off
        wbp = A.bf16(16 * 128).rearrange("p (k n) -> p k n", k=16)
        S_wbp = Slot()
        for pc in range(8):
            dma("pool", wbp, w_in.rearrange("(k p) n -> p k n", p=128)[:, :, pc * 128:(pc + 1) * 128], writes=[S_wbp])
            k = nb(4, 8)
            for kc in range(16):
                mm(ps[:, k, 0:16], wbp[:, kc, :], hT[:, kc, T - 16:T], kc == 0, kc == 15, [S_wbp, S_hT], [PS[k]])
            ts(halo[:, pc, :], ps[:, k, 0:16], flag[:, 0:1], None, ALU.mult, None, [PS[k], S_c], [S_halo])
        reset(m)

        norm_to_hT(x_cur, S_xcur, g1_bc, 1 * D, 0 * D)
        P.op("dve", lambda: nc.vector.tensor_copy(out=XSr[:, :, 0], in_=carry[:, 0, :]), reads=[S_halo], writes=[S_xs])
        P.op("dve", lambda: nc.vector.tensor_copy(out=XSi[:, :, 0], in_=carry[:, 1, :]), reads=[S_halo], writes=[S_xs])
        ssm_pass("A")
        level2()
        if dbg:
            S_dump0 = Slot()
            dma("sp", xs_dump[:, 0], XSr, reads=[S_xs], writes=[S_dump0])
            dma("sp", xs_dump[:, 1], XSi, reads=[S_xs], writes=[S_dump0])
        ssm_pass("B")

        reset(mark_ys)
        pm2 = A.bf16(8 * T).rearrange("p (c t) -> p c t", c=8)
        S_pm2 = Slot("pm2")
        m = A.off
        wbp = A.bf16(16 * 128).rearrange("p (k n) -> p k n", k=16)
        pw = A.bf16(4 * 2 * 256).rearrange("p (g k d) -> p g k d", g=4, k=2)
        psc = A.f32(8)
        U = A.f32(T + 16)
        V = A.f32(T + 16)
        Wb = A.f32(T + 16)
        rc = A.f32(T)
        pooled = A.bf16(2 * T).rearrange("p (c t) -> p c t", c=2)
        S_wbp, S_pw, S_U, S_V, S_W, S_rc, S_pl = Slot(), Slot(), Slot(), Slot(), Slot(), Slot(), Slot()
        dma("pool", pw, pool_w_l, writes=[S_pw])
        dma("sp", psc, pscale_l, writes=[S_pw])
        N = T + 16
        for g in range(4):
            dma("sp", rc, rc_bc[:, g, :], writes=[S_rc])
            for gc in range(2):
                pc = g * 2 + gc
                zT_chunk(pc * 128, None, None, wbp, S_wbp, fp32_out=U, S_f=S_U)
                P.op("dve", lambda pc=pc: nc.vector.tensor_copy(out=U[:, 0:16], in_=halo[:, pc, :]), reads=[S_halo], writes=[S_U])
                src, ssrc = U, S_U
                bufs = [(V, S_V), (Wb, S_W)]
                sh = 1
                for lev in range(g + 1):
                    dst, sdst = bufs[lev % 2]
                    tt(dst[:, sh:N], src[:, sh:N], src[:, 0:N - sh], ALU.add, [ssrc], [sdst])
                    src, ssrc = dst, sdst
                    sh *= 2
                dst, sdst = bufs[(g + 1) % 2]
                tt(dst[:, 16:N], src[:, 16:N], rc, ALU.mult, [ssrc, S_rc], [sdst])
                tt(pooled[:, gc, :], dst[:, 16:N], U[:, 16:N], ALU.subtract, [sdst, S_U], [S_pl])
            for dch in range(2):
                for tc in range(4):
                    k = nb()
                    for kch in range(2):
                        mm(ps[:, k, :], pw[:, g, kch, dch * 128:(dch + 1) * 128], pooled[:, kch, tc * 512:(tc + 1) * 512],
                           kch == 0, kch == 1, [S_pw, S_pl], [PS[k]])
                    ts(pm2[:, g * 2 + dch, tc * 512:(tc + 1) * 512], ps[:, k, :], psc[:, g * 2 + dch:g * 2 + dch + 1], None,
                       ALU.mult, None, [PS[k], S_pw], [S_pm2])
        reset(m)

        m = A.off
        bglu = A.f32(32)
        S_bg = Slot()
        dma("sp", bglu, bglu_l, writes=[S_bg])
        wgp = A.bf16(16 * 256).rearrange("p (k n) -> p k n", k=16)
        wgs = A.bf16(16 * 256).rearrange("p (k n) -> p k n", k=16)
        wpo = A.bf16(8 * 256).rearrange("p (k n) -> p k n", k=8)
        wga = A.bf16(8 * 256).rearrange("p (k n) -> p k n", k=8)
        wgb = A.bf16(8 * 256).rearrange("p (k n) -> p k n", k=8)
        S_w3 = Slot()
        e_sgp, e_sgs, e_sgb, e_m1, e_ya = [A.f32(512) for _ in range(5)]
        mo = [A.bf16(512), A.bf16(512)]
        S_e = Slot()
        S_mo = [Slot(), Slot()]
        S_mT = Slot("mT")
        w_in_v = w_in.rearrange("(k p) n -> p k n", p=128)
        wpo_v = w_pool_out.rearrange("(k p) n -> p k n", p=128)
        wgl_v = w_glu.rearrange("(k p) n -> p k n", p=128)
        moi = 0
        for blk in range(8):
            c0 = blk * 256
            dma("pool", wgp, w_in_v[:, :, 2048 + c0:2048 + c0 + 256], writes=[S_w3])
            dma("pool", wgs, w_in_v[:, :, 4096 + c0:4096 + c0 + 256], writes=[S_w3])
            dma("pool", wpo, wpo_v[:, :, c0:c0 + 256], writes=[S_w3])
            dma("pool", wga, wgl_v[:, :, c0:c0 + 256], writes=[S_w3])
            dma("pool", wgb, wgl_v[:, :, 2048 + c0:2048 + c0 + 256], writes=[S_w3])
            for tc in range(4):
                tsl = slice(tc * 512, (tc + 1) * 512)
                for d2 in range(2):
                    dd = blk * 2 + d2
                    ws = slice(d2 * 128, (d2 + 1) * 128)
                    kgp, kgs, kyp, kga, kgb = nb(), nb(), nb(), nb(), nb()
                    for kc in range(16):
                        mm(ps[:, kgp, :], wgp[:, kc, ws], hT[:, kc, tsl], kc == 0, kc == 15, [S_w3, S_hT], [PS[kgp]])
                    for kc in range(16):
                        mm(ps[:, kgs, :], wgs[:, kc, ws], hT[:, kc, tsl], kc == 0, kc == 15, [S_w3, S_hT], [PS[kgs]])
                    for kc in range(8):
                        mm(ps[:, kyp, :], wpo[:, kc, ws], pm2[:, kc, tsl], kc == 0, kc == 7, [S_w3, S_pm2], [PS[kyp]])
                    for kc in range(8):
                        mm(ps[:, kga, :], wga[:, kc, ws], ys[:, kc, tsl], kc == 0, kc == 7, [S_w3, S_ys], [PS[kga]])
                    for kc in range(8):
                        mm(ps[:, kgb, :], wgb[:, kc, ws], ys[:, kc, tsl], kc == 0, kc == 7, [S_w3, S_ys], [PS[kgb]])
                    act(e_sgp, ps[:, kgp, :], AF.Sigmoid, [PS[kgp]], [S_e])
                    act(e_sgs, ps[:, kgs, :], AF.Sigmoid, [PS[kgs]], [S_e])
                    act(e_sgb, ps[:, kgb, :], AF.Sigmoid, [PS[kgb], S_bg], [S_e], bias=bglu[:, 16 + dd:17 + dd])
                    tt(e_m1, e_sgp, ps[:, kyp, :], ALU.mult, [S_e, PS[kyp]], [S_e])
                    stt(e_ya, ps[:, kga, :], bglu[:, dd:dd + 1], e_sgb, ALU.add, ALU.mult, [PS[kga], S_bg, S_e], [S_e])
                    tt(e_ya, e_ya, e_sgs, ALU.mult, [S_e], [S_e])
                    mb, smb = mo[moi % 2], S_mo[moi % 2]
                    moi += 1
                    tt(mb, e_m1, e_ya, ALU.add, [S_e], [smb])
                    dma("sp", mT_d[dd, :, tsl], mb, reads=[smb], writes=[S_mT])
        if dbg:
            S_dump = Slot()
            dma("sp", ys_dump, ys, reads=[S_ys], writes=[S_dump])
            dma("sp", pm2_dump, pm2, reads=[S_pm2], writes=[S_dump])
            dma("sp", hT_dump, hT, reads=[S_hT], writes=[S_dump])
        reset(m)

        m = A.off
        mTt = A.bf16(16 * 512).rearrange("p (k t) -> p k t", k=16)
        wo = A.bf16(16 * 512).rearrange("p (k n) -> p k n", k=16)
        gtm = A.f32(D)
        xs4 = [A.f32(512), A.f32(512)]
        x1p = [A.f32(512), A.f32(512)]
        S_mTt, S_wo, S_gtm = Slot(), Slot(), Slot()
        S_xs4 = [Slot(), Slot()]
        S_x1p = [Slot(), Slot()]
        S_x2 = Slot("x2")
        dma("sp", gtm, modbc[:, 2 * D:3 * D], reads=[S_mod], writes=[S_gtm])
        wo_v = w_out.rearrange("(k p) n -> p k n", p=128)
        ci = 0
        for tc in range(4):
            dma("sp", mTt, mT_d.rearrange("k p t -> p k t")[:, :, tc * 512:(tc + 1) * 512], reads=[S_mT], writes=[S_mTt])
            for cb in range(4):
                csl = slice(cb * 512, (cb + 1) * 512)
                dma("pool", wo, wo_v[:, :, csl], writes=[S_wo])
                for il in range(4):
                    i = tc * 4 + il
                    k = nb()
                    for kc in range(16):
                        mm(ps[:, k, :], mTt[:, kc, il * 128:(il + 1) * 128], wo[:, kc, :], kc == 0, kc == 15, [S_mTt, S_wo], [PS[k]])
                    xb_, sxb = xs4[ci % 2], S_xs4[ci % 2]
                    xo, sxo = x1p[ci % 2], S_x1p[ci % 2]
                    ci += 1
                    dma("sp", xb_, x_cur[i * 128:(i + 1) * 128, csl], writes=[sxb])
                    tt(xo, ps[:, k, :], gtm[:, csl], ALU.mult, [PS[k], S_gtm], [sxo])
                    tt(xo, xo, xb_, ALU.add, [sxo, sxb], [sxo])
                    dma("sp", x2_d[i * 128:(i + 1) * 128, csl], xo, reads=[sxo], writes=[S_x2])
        reset(m)

        reset(mark_low)
        gate = A.f32(NT * NE).rearrange("p (i e) -> p i e", i=NT)
        S_gate = Slot("gate")
        mark_gate = A.off
        wr = A.bf16(16 * NE).rearrange("p (k e) -> p k e", k=16)
        brow = A.bf16(NE)
        lg = A.f32(NE)
        m8 = A.f32(8)
        ngm = A.f32(2)
        msk = A.f32(NE)
        S_wr, S_lg = Slot(), Slot()
        dma("pool", wr, w_router.rearrange("(k p) e -> p k e", p=128), writes=[S_wr])
        dma("pool", brow[0:1, :], b_router, writes=[S_wr])

        def router(i):
            k = nb()
            mm(ps[:, k, 0:NE], ones_b[0:1, :], brow[0:1, :], True, False, [S_c, S_wr], [PS[k]])
            for kc in range(16):
                mm(ps[:, k, 0:NE], hT[:, kc, i * 128:(i + 1) * 128], wr[:, kc, :], False, kc == 15, [S_hT, S_wr], [PS[k]])
            P.op("dve", lambda: nc.vector.tensor_copy(out=lg, in_=ps[:, k, 0:NE]), reads=[PS[k]], writes=[S_lg])
            P.op("dve", lambda: nc.vector.max(out=m8, in_=lg), reads=[S_lg], writes=[S_lg])
            ts(msk, lg, m8[:, 3:4], None, ALU.is_ge, None, [S_lg], [S_lg])
            ts(ngm[:, 0:1], m8[:, 0:1], -1.0, None, ALU.mult, None, [S_lg], [S_lg])
            act(lg, lg, AF.Exp, [S_lg], [S_lg], bias=ngm[:, 0:1])
            tt(lg, lg, msk, ALU.mult, [S_lg], [S_lg])
            P.op("dve", lambda: nc.vector.reduce_sum(out=ngm[:, 1:2], in_=lg, axis=mybir.AxisListType.X), reads=[S_lg], writes=[S_lg])
            P.op("dve", lambda: nc.vector.reciprocal(out=ngm[:, 1:2], in_=ngm[:, 1:2]), reads=[S_lg], writes=[S_lg])
            ts(gate[:, i, :], lg, ngm[:, 1:2], None, ALU.mult, None, [S_lg], [S_gate])

        norm_to_hT(x2_d, S_x2, g2_bc, 4 * D, 3 * D, extra=router)
        reset(mark_gate)

        actT = A.bf16(NT * 16 * 128).rearrange("p (i f t) -> p i f t", i=NT, f=16)
        S_actT = Slot("actT")
        gtf = A.f32(D)
        S_gtf = Slot()
        dma("sp", gtf, modbc[:, 5 * D:6 * D], reads=[S_mod], writes=[S_gtf])
        w1buf = [(A.bf16(16 * 256).rearrange("p (k n) -> p k n", k=16), A.bf16(16 * 256).rearrange("p (k n) -> p k n", k=16))
                 for _ in range(2)]
        w2buf = [A.bf16(16 * 256).rearrange("p (k n) -> p k n", k=16) for _ in range(2)]
        S_w1b = [Slot(), Slot()]
        S_w2b = [Slot(), Slot()]
        b1r = A.bf16(2 * D)
        b2r = A.bf16(D)
        S_b1, S_b2 = Slot(), Slot()
        xg, sg_, xl = A.f32(256), A.f32(256), A.f32(256)
        ab = [A.bf16(256), A.bf16(256)]
        S_ev = Slot()
        S_ab = [Slot(), Slot()]
        NY = 4
        yst = [A.f32(256) for _ in range(NY)]
        S_yst = [Slot() for _ in range(NY)]
        S_x2r = [[Slot() for _ in range(8)] for _ in range(NT)]
        for i in range(NT):
            for d8 in range(8):
                S_x2r[i][d8].w = S_x2.w
        print("MoE arena words used", A.off, "of", W)
        blocks = []
        for e in range(0 if dbg else NE):
            for j in range(8):
                blocks.append(["w1", e, j, 0])
            for j in range(8):
                blocks.append(["w2", e, j, 0])
        pcnt = {"w1": 0, "w2": 0}

        def issue_load(bi):
            if bi >= len(blocks):
                return
            kind, e, j, _ = blocks[bi]
            par = pcnt[kind] % 2
            pcnt[kind] += 1
            blocks[bi][3] = par
            if kind == "w1":
                w1v = w1[e].rearrange("(k p) n -> p k n", p=128)
                if j == 0:
                    dma("pool", b1r[0:1, :], b1[e:e + 1, :], writes=[S_b1], cls="w")
                f0 = j * 256
                dma("pool", w1buf[par][0], w1v[:, :, f0:f0 + 256], writes=[S_w1b[par]], cls="w")
                dma("pool", w1buf[par][1], w1v[:, :, D + f0:D + f0 + 256], writes=[S_w1b[par]], cls="w")
            else:
                w2v = w2[e].rearrange("(k p) n -> p k n", p=128)
                if j == 0:
                    dma("pool", b2r[0:1, :], b2[e:e + 1, :], writes=[S_b2], cls="w")
                dma("pool", w2buf[par], w2v[:, :, j * 256:(j + 1) * 256], writes=[S_w2b[par]], cls="w")

        if dbg:
            dma("sp", gate_dump, gate, reads=[S_gate], writes=[Slot()])
        issue_load(0)
        yi = 0
        abi = 0
        for bi in range(len(blocks)):
            issue_load(bi + 1)
            kind, e, j, par = blocks[bi]
            if kind == "w1":
                fs, f0 = j, j * 256
                pend = None

                def flush(pend):
                    i_, ab_, sab_ = pend
                    k2 = nb()
                    pb = ps[:, k2, :].bitcast(BF16).rearrange("p (a b) -> p a b", a=8)
                    for jj in range(2):
                        P.op("pe", lambda pb=pb, jj=jj, ab_=ab_: nc.tensor.transpose(pb[:, jj, :], ab_[:, jj * 128:(jj + 1) * 128], ident),
                             reads=[sab_, S_c], writes=[PS[k2]])
                    act(actT[:, i_, fs * 2:fs * 2 + 2, :], pb[:, 0:2, :], AF.Copy, [PS[k2]], [S_actT])
                for i in range(NT):
                    k = nb()
                    for (wblk_, c0, b0) in ((w1buf[par][0], 0, f0), (w1buf[par][1], 256, D + f0)):
                        mm(ps[:, k, c0:c0 + 256], ones_b[0:1, :], b1r[0:1, b0:b0 + 256], True, False, [S_c, S_b1], [PS[k]])
                        for kc in range(16):
                            mm(ps[:, k, c0:c0 + 256], hT[:, kc, i * 128:(i + 1) * 128], wblk_[:, kc, :], False, kc == 15,
                               [S_hT, S_w1b[par]], [PS[k]])
                    ab_, sab_ = ab[abi % 2], S_ab[abi % 2]
                    abi += 1
                    ts(xg, ps[:, k, 0:256], 7.0, None, ALU.min, None, [PS[k]], [S_ev])
                    act(sg_, xg, AF.Sigmoid, [S_ev], [S_ev], scale=1.702)
                    ts(xl, ps[:, k, 256:512], 7.0, -7.0, ALU.min, ALU.max, [PS[k]], [S_ev])
                    stt(xl, xl, 1.0, xg, ALU.add, ALU.mult, [S_ev], [S_ev])
                    tt(ab_, xl, sg_, ALU.mult, [S_ev], [sab_])
                    if pend is not None:
                        flush(pend)
                    pend = (i, ab_, sab_)
                flush(pend)
            else:
                dsl = slice(j * 256, (j + 1) * 256)
                for i in range(NT):
                    k = nb()
                    mm(ps[:, k, 0:256], ones_b[0:1, :], b2r[0:1, dsl], True, False, [S_c, S_b2], [PS[k]])
                    for fc in range(16):
                        mm(ps[:, k, 0:256], actT[:, i, fc, :], w2buf[par][:, fc, :], False, fc == 15, [S_actT, S_w2b[par]], [PS[k]])
                    yb, syb = yst[yi % NY], S_yst[yi % NY]
                    yi += 1
                    stt(yb, ps[:, k, 0:256], gate[:, i, e:e + 1], gtf[:, dsl], ALU.mult, ALU.mult, [PS[k], S_gate, S_gtf], [syb])
                    dma("pool", x2_d[i * 128:(i + 1) * 128, dsl], yb, reads=[syb, S_x2r[i][j]], writes=[S_x2r[i][j]],
                        accum_op=ALU.add, cls="a")

        reset(mark_low)
        G3, S3, sl3 = load_GS(g3_bc, 7 * D, 6 * D)
        S_out = Slot()

        def fin_consume(i, h, sh):
            dma("sp", out_d[i * 128:(i + 1) * 128, :], h, reads=[sh], writes=[S_out])
        norm_rows(x2_d, G3, S3, sl3, (lambda i: S_x2r[i]), fin_consume)

        P.emit()
    return nc


def _prep_shared(inp):
    f = np.float32
    s = {}
    s["ident"] = np.eye(128, dtype=f)
    s["ada_w"] = np.ascontiguousarray(inp["ada_w"][0])
    s["ada_b_bc"] = np.ascontiguousarray(np.broadcast_to(inp["ada_b"][0][None, :], (128, 6 * D)))
    s["final_ada_w"] = np.ascontiguousarray(inp["final_ada_w"])
    s["final_ada_b_bc"] = np.ascontiguousarray(np.broadcast_to(inp["final_ada_b"][None, :], (128, 2 * D)))
    s["g1_bc"] = np.ascontiguousarray(np.broadcast_to(inp["norm1_g"][0][None, :], (128, D)))
    s["g2_bc"] = np.ascontiguousarray(np.broadcast_to(inp["norm2_g"][0][None, :], (128, D)))
    s["g3_bc"] = np.ascontiguousarray(np.broadcast_to(inp["final_norm_g"][None, :], (128, D)))
    s["w_in"] = np.ascontiguousarray(inp["w_in"][0])
    pw = inp["pool_w"][0]
    s["pool_w_l"] = np.ascontiguousarray(pw.reshape(4, 2, 128, 256).transpose(2, 0, 1, 3))
    s["pscale_l"] = np.ascontiguousarray(inp["pool_scale"][0].reshape(8, 128).T)
    s["w_pool_out"] = np.ascontiguousarray(inp["w_pool_out"][0])

    def pairl(a):
        return np.ascontiguousarray(a.reshape(32, 2, 64).transpose(1, 2, 0).reshape(128, 32))
    s["lamre_l"] = pairl(inp["ssm_lam_re"][0])
    s["lamim_l"] = pairl(inp["ssm_lam_im"][0])
    s["logdt_l"] = pairl(np.repeat(inp["ssm_log_dt"][0][:, None], 64, axis=1))
    bre, bim = inp["ssm_b_re"][0], inp["ssm_b_im"][0]
    Bl = np.zeros((128, 8, 2, 2, 128), f)
    cre, cim = inp["ssm_c_re"][0], inp["ssm_c_im"][0]
    Cr = np.zeros((128, 32, 128), f)
    Ci = np.zeros((128, 32, 128), f)
    for g in range(64):
        pr, gl = g // 2, g % 2
        cc, q = pr // 4, pr % 4
        r0 = 32 * q + 16 * gl
        vv = 1 if q == 3 else 0
        Bl[r0:r0 + 16, cc, vv, 0, gl * 64:(gl + 1) * 64] = bre[g].T
        Bl[r0:r0 + 16, cc, vv, 1, gl * 64:(gl + 1) * 64] = bim[g].T
        m0 = 32 * q + 16 * gl
        Cr[gl * 64:(gl + 1) * 64, pr, m0:m0 + 16] = cre[g].T
        Ci[gl * 64:(gl + 1) * 64, pr, m0:m0 + 16] = cim[g].T
    s["B_l"], s["CTre_l"], s["CTim_l"] = Bl, Cr, Ci
    s["ssmd_l"] = np.ascontiguousarray(inp["ssm_d"][0].reshape(8, 128).T)
    s["w_glu"] = np.ascontiguousarray(inp["w_glu"][0])
    s["bglu_l"] = np.ascontiguousarray(inp["b_glu"][0].reshape(32, 128).T)
    s["w_out"] = np.ascontiguousarray(inp["w_out"][0])
    s["w_router"] = np.ascontiguousarray(inp["w_router"][0])
    s["b_router"] = np.ascontiguousarray(inp["b_router"][0][None, :])
    s["w1"] = np.ascontiguousarray(inp["w1"][0])
    s["b1"] = np.ascontiguousarray(inp["b1"][0])
    s["w2"] = np.ascontiguousarray(inp["w2"][0])
    s["b2"] = np.ascontiguousarray(inp["b2"][0])
    return s


def kernel(**inp):
    inp = {k: np.asarray(v) for k, v in inp.items()}
    f = np.float32
    shared = _prep_shared(inp)
    x, c = inp["x"], inp["c"]
    in_maps = []
    win = np.array([2.0, 4.0, 8.0, 16.0], f)
    for core in range(8):
        b, half = core // 2, core % 2
        mp = dict(shared)
        mp["x_cur"] = np.ascontiguousarray(x[b, half * T:(half + 1) * T])
        mp["x_prev"] = np.ascontiguousarray(x[b, 0:T]) if half == 1 else np.zeros((T, D), f)
        mp["flag"] = np.full((128, 1), float(half), f)
        mp["cT"] = np.ascontiguousarray(c[b].reshape(16, 128).T)
        pos = np.arange(1, T + 1, dtype=f) + (T if half == 1 else 0)
        rc = (1.0 / np.minimum(pos[None, :], win[:, None])).astype(f)
        mp["rc_bc"] = np.ascontiguousarray(np.broadcast_to(rc[None], (128, 4, T)))
        in_maps.append(mp)
    nc = build()
    res = run_bass_kernel_spmd(nc, in_maps, core_ids=list(range(8)))
    out = np.zeros((4, 2 * T, D), f)
    for core in range(8):
        b, half = core // 2, core % 2
        out[b, half * T:(half + 1) * T] = res.results[core]["out"]
    return out
```

```python
import math
import numpy as np
import concourse.bass as bass
import concourse.mybir as mybir
from concourse.bass_utils import run_bass_kernel_spmd

F32 = mybir.dt.float32
BF16 = mybir.dt.bfloat16
AF = mybir.ActivationFunctionType
ALU = mybir.AluOpType

D = 2048
T = 2048
NT = 16
NE = 32
L1 = 16
NC1 = T // L1


class Slot:
    __slots__ = ("name", "w", "rs")

    def __init__(self, name=""):
        self.name = name
        self.w = None
        self.rs = []


class Op:
    __slots__ = ("eng", "fn", "deps", "need", "cnt", "dma", "dsem", "dcnt", "prev")

    def __init__(self, eng, fn, dma):
        self.eng = eng
        self.fn = fn
        self.deps = []
        self.need = False
        self.cnt = None
        self.dma = dma
        self.dsem = None
        self.dcnt = None
        self.prev = 0


class Prog:
    ENG = ("pe", "act", "dve", "pool", "sp")

    def __init__(self, nc, ndma=8):
        self.nc = nc
        self.ops = []
        self.last = {}
        self.dcur = {"sp": 0, "act": 0, "pool": 0}
        self.dlast = {}
        self.h = {"pe": nc.tensor, "act": nc.scalar, "dve": nc.vector, "pool": nc.gpsimd, "sp": nc.sync}
        self.ndma = ndma

    NDMA = {("sp", ""): 8, ("act", ""): 2, ("pool", ""): 3, ("pool", "w"): 3, ("pool", "a"): 6}

    def op(self, eng, fn, reads=(), writes=(), dma=False, cls=""):
        o = Op(eng, fn, dma)
        deps = set()
        for s in reads:
            if s.w is not None:
                deps.add(s.w)
        for s in writes:
            if s.w is not None:
                deps.add(s.w)
            for r in s.rs:
                deps.add(r)
        for d in deps:
            if (not d.dma) and (not dma) and d.eng == eng and eng == "pe":
                continue
            o.deps.append(d)
            d.need = True
        for s in reads:
            if dma:
                s.rs.append(o)
            else:
                s.rs = [r for r in s.rs if r.dma or r.eng != eng]
                s.rs.append(o)
        for s in writes:
            s.w = o
            s.rs = []
        self.ops.append(o)
        if dma:
            q = (eng, cls)
            i = self.dcur.get(q, 0) % self.NDMA[q]
            self.dcur[q] = self.dcur.get(q, 0) + 1
            o.dsem = (q, i)
            self.dlast[o.dsem] = o
        else:
            self.last[eng] = o
        return o

    def barrier(self):
        lst = list(self.last.values()) + list(self.dlast.values())
        for d in lst:
            d.need = True
        self.ops.append(("barrier", lst))

    def emit(self):
        nc = self.nc
        sems = {e: nc.alloc_semaphore("s_" + e) for e in self.ENG}
        dq = list(self.NDMA.keys())
        dsems = {q: [nc.alloc_semaphore("d_%s%s_%d" % (q[0], q[1], i)) for i in range(self.NDMA[q])] for q in dq}
        cnt = {e: 0 for e in self.ENG}
        dcount = {q: [0] * self.NDMA[q] for q in dq}
        for o in self.ops:
            if isinstance(o, tuple):
                continue
            if o.dma:
                q, i = o.dsem
                o.prev = dcount[q][i]
                dcount[q][i] += 16
                o.dcnt = dcount[q][i]
            elif o.need:
                cnt[o.eng] += 1
                o.cnt = cnt[o.eng]
        seen = {}
        pending = {e: [] for e in self.ENG}
        for o in self.ops:
            if isinstance(o, tuple):
                for e in self.ENG:
                    pending[e] = list(o[1])
                continue
            h = self.h[o.eng]
            waits = {}
            dl = o.deps
            if pending[o.eng]:
                dl = dl + pending[o.eng]
                pending[o.eng] = []
            for d in dl:
                if d.dma:
                    key = ("d", d.dsem)
                    val = d.dcnt
                    sh = dsems[d.dsem[0]][d.dsem[1]]
                else:
                    key = ("c", d.eng)
                    val = d.cnt
                    sh = sems[d.eng]
                if seen.get((o.eng, key), 0) >= val:
                    continue
                if key not in waits or waits[key][1] < val:
                    waits[key] = (sh, val)
            if o.dma and o.prev > 0:
                key = ("d", o.dsem)
                if seen.get((o.eng, key), 0) < o.prev:
                    if key not in waits or waits[key][1] < o.prev:
                        waits[key] = (dsems[o.dsem[0]][o.dsem[1]], o.prev)
            for key, (sh, val) in waits.items():
                h.wait_ge(sh, val)
                seen[(o.eng, key)] = val
            ins = o.fn()
            if o.dma:
                ins.then_inc(dsems[o.dsem[0]][o.dsem[1]], 16)
            elif o.need:
                ins.then_inc(sems[o.eng], 1)
        for q in dq:
            for i in range(self.NDMA[q]):
                if dcount[q][i] > 0:
                    nc.sync.wait_ge(dsems[q][i], dcount[q][i])
        self.ops = None


class Arena:
    def __init__(self, ap, words):
        self.ap = ap
        self.words = words
        self.off = 0

    def f32(self, n):
        assert self.off + n <= self.words, ("sbuf arena overflow", self.off, n)
        a = self.ap[:, self.off:self.off + n]
        self.off += n
        return a

    def bf16(self, n):
        w = (n + 1) // 2
        assert self.off + w <= self.words, ("sbuf arena overflow", self.off, n)
        a = self.ap[:, self.off:self.off + w].bitcast(BF16)
        self.off += w
        return a


def build(dbg=False):
    okind = "ExternalOutput" if dbg else "Internal"
    nc = bass.Bass("TRN2", target_bir_lowering=False)

    def din(name, shape):
        return nc.dram_tensor(name, list(shape), F32, kind="ExternalInput").ap()

    x_cur = din("x_cur", [T, D])
    x_prev = din("x_prev", [T, D])
    flag_d = din("flag", [128, 1])
    cT_d = din("cT", [128, 16])
    ident_d = din("ident", [128, 128])
    ada_w = din("ada_w", [D, 6 * D])
    ada_b_bc = din("ada_b_bc", [128, 6 * D])
    fada_w = din("final_ada_w", [D, 2 * D])
    fada_b_bc = din("final_ada_b_bc", [128, 2 * D])
    g1_bc = din("g1_bc", [128, D])
    g2_bc = din("g2_bc", [128, D])
    g3_bc = din("g3_bc", [128, D])
    w_in = din("w_in", [D, 6144])
    pool_w_l = din("pool_w_l", [128, 4, 2, 256])
    pscale_l = din("pscale_l", [128, 8])
    rc_bc = din("rc_bc", [128, 4, T])
    w_pool_out = din("w_pool_out", [1024, D])
    lamre_l = din("lamre_l", [128, 32])
    lamim_l = din("lamim_l", [128, 32])
    logdt_l = din("logdt_l", [128, 32])
    B_l = din("B_l", [128, 8, 2, 2, 128])
    CTre_l = din("CTre_l", [128, 32, 128])
    CTim_l = din("CTim_l", [128, 32, 128])
    ssmd_l = din("ssmd_l", [128, 8])
    w_glu = din("w_glu", [1024, 2 * D])
    bglu_l = din("bglu_l", [128, 32])
    w_out = din("w_out", [D, D])
    w_router = din("w_router", [D, NE])
    b_router = din("b_router", [1, NE])
    if not dbg:
        w1 = din("w1", [NE, D, 2 * D])
        b1 = din("b1", [NE, 2 * D])
        w2 = din("w2", [NE, D, D])
        b2 = din("b2", [NE, D])
    out_d = nc.dram_tensor("out", [T, D], F32, kind="ExternalOutput").ap()
    modbc = nc.dram_tensor("modbc", [128, 8 * D], F32, kind=okind).ap()
    x2_d = nc.dram_tensor("x2s", [T, D], F32, kind=okind).ap()
    mT_d = nc.dram_tensor("mTs", [16, 128, T], BF16, kind=okind).ap()
    if dbg:
        ys_dump = nc.dram_tensor("ys_dump", [128, 8, T], BF16, kind=okind).ap()
        pm2_dump = nc.dram_tensor("pm2_dump", [128, 8, T], BF16, kind=okind).ap()
        hT_dump = nc.dram_tensor("hT_dump", [128, 16, T], BF16, kind=okind).ap()
        xs_dump = nc.dram_tensor("xs_dump", [128, 2, 32, 129], F32, kind=okind).ap()
        gate_dump = nc.dram_tensor("gate_dump", [128, NT, NE], F32, kind=okind).ap()

    P = Prog(nc)
    W = 53200
    with nc.sbuf_tensor("arena", [128, W], F32) as ar_t, nc.psum_tensor("ps", [128, 8, 512], F32) as ps:
        A = Arena(ar_t, W)

        def reset(mark):
            A.off = mark
            P.barrier()
        PS = [Slot("ps%d" % k) for k in range(8)]
        bank_ctr = [0]

        def nb(lo=0, hi=8):
            k = lo + bank_ctr[0] % (hi - lo)
            bank_ctr[0] += 1
            return k

        def dma(eng, out, in_, reads=(), writes=(), cls="", **kw):
            h = P.h[eng]
            return P.op(eng, lambda: h.dma_start(out=out, in_=in_, **kw), reads=reads, writes=writes, dma=True, cls=cls)

        def mm(out, lhsT, rhs, start, stop, reads, writes):
            return P.op("pe", lambda: nc.tensor.matmul(out, lhsT=lhsT, rhs=rhs, start=start, stop=stop),
                        reads=reads, writes=writes)

        def act(out, in_, func, reads, writes, **kw):
            return P.op("act", lambda: nc.scalar.activation(out=out, in_=in_, func=func, **kw),
                        reads=reads, writes=writes)

        def stt(out, in0, scalar, in1, op0, op1, reads, writes):
            return P.op("dve", lambda: nc.vector.scalar_tensor_tensor(out=out, in0=in0, scalar=scalar, in1=in1,
                                                                       op0=op0, op1=op1), reads=reads, writes=writes)

        def tt(out, in0, in1, op, reads, writes, eng="dve"):
            h = P.h[eng]
            return P.op(eng, lambda: h.tensor_tensor(out=out, in0=in0, in1=in1, op=op), reads=reads, writes=writes)

        def ts(out, in0, s1, s2, op0, op1, reads, writes):
            if s2 is None:
                return P.op("dve", lambda: nc.vector.tensor_scalar(out=out, in0=in0, scalar1=s1, scalar2=None, op0=op0),
                            reads=reads, writes=writes)
            return P.op("dve", lambda: nc.vector.tensor_scalar(out=out, in0=in0, scalar1=s1, scalar2=s2, op0=op0, op1=op1),
                        reads=reads, writes=writes)

        hT = A.bf16(16 * T).rearrange("p (k t) -> p k t", k=16)
        S_hT = Slot("hT")
        ident = A.bf16(128)
        S_c = Slot("consts")
        ones_b = A.bf16(128)
        flag = A.f32(1)
        S_small = Slot("small")
        dma("pool", ident, ident_d, writes=[S_c])
        dma("sp", flag, flag_d, writes=[S_c])
        P.op("dve", lambda: nc.vector.memset(ones_b, 1.0), writes=[S_c])
        mark_low = A.off
        small = A.f32(32 * 24).rearrange("p (a b) -> p a b", b=32)
        carry = A.f32(64).rearrange("p (r a) -> p r a", r=2)
        halo = A.f32(8 * 16).rearrange("p (c h) -> p c h", c=8)
        S_halo = Slot("halo")
        ys = A.bf16(8 * T).rearrange("p (c t) -> p c t", c=8)
        S_ys = Slot("ys")
        mark_ys = A.off
        mark0 = A.off

        cT = A.f32(16)
        csg = A.f32(16)
        carep = A.f32(16 * 128).rearrange("p (k m) -> p k m", k=16)
        wblk = A.f32(16 * 512).rearrange("p (k n) -> p k n", k=16)
        bblk = A.f32(512)
        oblk = A.f32(512)
        S_ca, S_wblk, S_bblk, S_oblk, S_mod = Slot(), Slot(), Slot(), Slot(), Slot("modbc")
        dma("sp", cT, cT_d, writes=[S_ca])
        act(csg, cT, AF.Sigmoid, [S_ca], [S_ca])
        tt(cT, cT, csg, ALU.mult, [S_ca], [S_ca])
        P.op("dve", lambda: nc.vector.tensor_copy(out=carep, in_=cT.unsqueeze(2).to_broadcast([128, 16, 128])),
             reads=[S_ca], writes=[S_ca])
        for (wd, bd, nblk, off) in ((ada_w, ada_b_bc, 24, 0), (fada_w, fada_b_bc, 8, 6 * D)):
            wv = wd.rearrange("(k p) n -> p k n", p=128)
            for n in range(nblk):
                dma("sp", wblk, wv[:, :, n * 512:(n + 1) * 512], writes=[S_wblk])
                dma("sp", bblk, bd[:, n * 512:(n + 1) * 512], writes=[S_bblk])
                k = nb()
                for kc in range(16):
                    mm(ps[:, k, :], carep[:, kc, :], wblk[:, kc, :], kc == 0, kc == 15, [S_ca, S_wblk], [PS[k]])
                tt(oblk, ps[:, k, :], bblk, ALU.add, [PS[k], S_bblk], [S_oblk])
                dma("sp", modbc[:, off + n * 512: off + (n + 1) * 512], oblk, reads=[S_oblk], writes=[S_mod])
        reset(mark0)

        def load_GS(gbc, sc_off, sh_off):
            G = A.f32(D)
            S = A.f32(D)
            tmp = A.f32(D)
            sl = Slot()
            dma("sp", G, gbc, writes=[sl])
            dma("sp", tmp, modbc[:, sc_off:sc_off + D], reads=[S_mod], writes=[sl])
            dma("sp", S, modbc[:, sh_off:sh_off + D], reads=[S_mod], writes=[sl])
            stt(G, tmp, 1.0, G, ALU.add, ALU.mult, [sl], [sl])
            return G, S, sl

        def norm_rows(src, G, S, sl_gs, S_src, consume, nbuf_mark=None):
            nbx = 2 if (A.words - A.off) >= 3 * D + 16 else 1
            xs = [A.f32(D) for _ in range(nbx)]
            sq = A.f32(D)
            st = A.f32(4)
            S_x = [Slot() for _ in range(nbx)]
            S_sq, S_st = Slot(), Slot()
            for i in range(NT):
                xb, sx = xs[i % nbx], S_x[i % nbx]
                dma("sp", xb, src[i * 128:(i + 1) * 128, :], reads=(S_src(i) if callable(S_src) else [S_src]), writes=[sx])
                act(sq, xb, AF.Square, [sx], [S_sq, S_st], accum_out=st[:, 0:1])
                ts(st[:, 1:2], st[:, 0:1], 1.0 / D, 1e-6, ALU.mult, ALU.add, [S_st], [S_st])
                act(st[:, 2:3], st[:, 1:2], AF.Sqrt, [S_st], [S_st])
                P.op("dve", lambda: nc.vector.reciprocal(out=st[:, 3:4], in_=st[:, 2:3]), reads=[S_st], writes=[S_st])
                stt(sq, xb, st[:, 3:4], G, ALU.mult, ALU.mult, [sx, S_st, sl_gs], [S_sq])
                tt(xb, sq, S, ALU.add, [S_sq, sl_gs], [sx])
                consume(i, xb, sx)

        def to_hT(i, h, sh, hb, S_hb):
            act(hb, h, AF.Copy, [sh], [S_hb])
            for half in range(2):
                k = nb()
                pb = ps[:, k, :].bitcast(BF16).rearrange("p (a b) -> p a b", a=8)
                for j in range(8):
                    kc = half * 8 + j
                    P.op("pe", lambda pb=pb, j=j, kc=kc: nc.tensor.transpose(pb[:, j, :], hb[:, kc * 128:(kc + 1) * 128], ident),
                         reads=[S_hb, S_c], writes=[PS[k]])
                P.op("dve", lambda pb=pb, half=half: nc.vector.tensor_copy(out=hT[:, half * 8:(half + 1) * 8, i * 128:(i + 1) * 128], in_=pb),
                     reads=[PS[k]], writes=[S_hT])

        def norm_to_hT(src, S_src, gbc, sc_off, sh_off, extra=None):
            m = A.off
            G, S, sl = load_GS(gbc, sc_off, sh_off)
            hb = A.bf16(D)
            S_hb = Slot()

            def consume(i, h, sh):
                to_hT(i, h, sh, hb, S_hb)
                if extra is not None:
                    extra(i)
            norm_rows(src, G, S, sl, S_src, consume)
            reset(m)

        (LRE, LIM, DT, AR, AI, NAI, A16R, A16I, FRE, FIM, NFRE, NFIM, T0, T1, T2, T3, T4) = range(17)

        def sm(i):
            return small[:, i, :]
        dma("sp", sm(LRE), lamre_l, writes=[S_small])
        dma("sp", sm(LIM), lamim_l, writes=[S_small])
        dma("sp", sm(DT), logdt_l, writes=[S_small])
        RS, WS = [S_small], [S_small]
        act(sm(DT), sm(DT), AF.Exp, RS, WS)
        ts(sm(LRE), sm(LRE), -1e-4, None, ALU.min, None, RS, WS)
        tt(sm(T0), sm(LRE), sm(DT), ALU.mult, RS, WS)
        act(sm(T0), sm(T0), AF.Exp, RS, WS)
        tt(sm(T1), sm(LIM), sm(DT), ALU.mult, RS, WS)
        halfpi = A.f32(1)
        P.op("dve", lambda: nc.vector.memset(halfpi, math.pi / 2), writes=WS)
        act(sm(T2), sm(T1), AF.Sin, RS, WS, scale=1.0 / 64)
        act(sm(T3), sm(T1), AF.Sin, RS, WS, scale=1.0 / 64, bias=halfpi[:, 0:1])

        def csquare(cr, ci, t):
            tt(sm(t), sm(cr), sm(ci), ALU.mult, RS, WS)
            tt(sm(cr), sm(cr), sm(cr), ALU.mult, RS, WS)
            tt(sm(ci), sm(ci), sm(ci), ALU.mult, RS, WS)
            tt(sm(cr), sm(cr), sm(ci), ALU.subtract, RS, WS)
            ts(sm(ci), sm(t), 2.0, None, ALU.mult, None, RS, WS)
        for _ in range(6):
            csquare(T3, T2, T4)
        tt(sm(AR), sm(T0), sm(T3), ALU.mult, RS, WS)
        tt(sm(AI), sm(T0), sm(T2), ALU.mult, RS, WS)
        ts(sm(NAI), sm(AI), -1.0, None, ALU.mult, None, RS, WS)
        P.op("dve", lambda: nc.vector.tensor_copy(out=sm(A16R), in_=sm(AR)), reads=RS, writes=WS)
        P.op("dve", lambda: nc.vector.tensor_copy(out=sm(A16I), in_=sm(AI)), reads=RS, writes=WS)
        for _ in range(4):
            csquare(A16R, A16I, T4)
        tt(sm(T0), sm(LRE), sm(LRE), ALU.mult, RS, WS)
        tt(sm(T1), sm(LIM), sm(LIM), ALU.mult, RS, WS)
        tt(sm(T0), sm(T0), sm(T1), ALU.add, RS, WS)
        P.op("dve", lambda: nc.vector.reciprocal(out=sm(T0), in_=sm(T0)), reads=RS, writes=WS)
        ts(sm(T1), sm(AR), -1.0, None, ALU.add, None, RS, WS)
        tt(sm(T2), sm(T1), sm(LRE), ALU.mult, RS, WS)
        tt(sm(T3), sm(AI), sm(LIM), ALU.mult, RS, WS)
        tt(sm(T2), sm(T2), sm(T3), ALU.add, RS, WS)
        tt(sm(FRE), sm(T2), sm(T0), ALU.mult, RS, WS)
        tt(sm(T2), sm(AI), sm(LRE), ALU.mult, RS, WS)
        tt(sm(T3), sm(T1), sm(LIM), ALU.mult, RS, WS)
        tt(sm(T2), sm(T2), sm(T3), ALU.subtract, RS, WS)
        tt(sm(FIM), sm(T2), sm(T0), ALU.mult, RS, WS)
        ts(sm(NFRE), sm(FRE), -1.0, None, ALU.mult, None, RS, WS)
        ts(sm(NFIM), sm(FIM), -1.0, None, ALU.mult, None, RS, WS)

        Cb = A.bf16(32 * 2 * 128).rearrange("p (a r m) -> p a r m", a=32, r=2)
        Bsb = A.bf16(8 * 2 * 2 * 128).rearrange("p (c v r m) -> p c v r m", c=8, v=2, r=2)
        ssmd = A.f32(8)
        S_cb = Slot("Cb")
        dma("pool", Bsb, B_l, writes=[S_cb])
        dma("sp", ssmd, ssmd_l, writes=[S_cb])
        m = A.off
        cre = A.f32(32 * 128).rearrange("p (a m) -> p a m", a=32)
        cim = A.f32(32 * 128).rearrange("p (a m) -> p a m", a=32)
        ctmp = A.f32(128)
        S_cl = Slot()
        dma("sp", cre, CTre_l, writes=[S_cl])
        dma("sp", cim, CTim_l, writes=[S_cl])
        for pr in range(32):
            ts(ctmp, cim[:, pr, :], small[:, FIM, pr:pr + 1], None, ALU.mult, None, [S_cl, S_small], [S_cl])
            stt(Cb[:, pr, 0, :], cre[:, pr, :], small[:, FRE, pr:pr + 1], ctmp, ALU.mult, ALU.subtract, [S_cl, S_small], [S_cb])
            ts(ctmp, cim[:, pr, :], small[:, NFRE, pr:pr + 1], None, ALU.mult, None, [S_cl, S_small], [S_cl])
            stt(Cb[:, pr, 1, :], cre[:, pr, :], small[:, NFIM, pr:pr + 1], ctmp, ALU.mult, ALU.add, [S_cl, S_small], [S_cb])
        reset(m)
        XSr = A.f32(32 * 129).rearrange("p (a c) -> p a c", a=32)
        XSi = A.f32(32 * 129).rearrange("p (a c) -> p a c", a=32)
        S_xs = Slot("XS")
        mark1 = A.off

        def zT_chunk(col0, ubf, S_u, wb, S_wb, tcs=range(4), fp32_out=None, S_f=None):
            dma("pool", wb, w_in.rearrange("(k p) n -> p k n", p=128)[:, :, col0:col0 + 128], writes=[S_wb])
            for tc in tcs:
                k = nb(4, 8)
                for kc in range(16):
                    mm(ps[:, k, :], wb[:, kc, :], hT[:, kc, tc * 512:(tc + 1) * 512], kc == 0, kc == 15, [S_wb, S_hT], [PS[k]])
                if fp32_out is None:
                    act(ubf[:, tc * 512:(tc + 1) * 512], ps[:, k, :], AF.Copy, [PS[k]], [S_u])
                else:
                    act(fp32_out[:, 16 + tc * 512:16 + (tc + 1) * 512], ps[:, k, :], AF.Copy, [PS[k]], [S_f])

        def ssm_pass(mode):
            m = A.off
            wb = A.bf16(16 * 128).rearrange("p (k n) -> p k n", k=16)
            ubf = A.bf16(T)
            vr = A.f32(T)
            vi = A.f32(T)
            S_wb, S_u = Slot(), Slot()
            S_vr = [Slot() for _ in range(L1)]
            S_vi = [Slot() for _ in range(L1)]
            S_vall = S_vr + S_vi
            vr3 = vr.rearrange("p (j c) -> p j c", j=L1)
            vi3 = vi.rearrange("p (j c) -> p j c", j=L1)
            vrT = vr.rearrange("p (j c) -> p c j", j=L1)
            viT = vi.rearrange("p (j c) -> p c j", j=L1)
            if mode == "B":
                xbr = A.bf16(T)
                xbi = A.bf16(T)
                gtmp = A.f32(512)
                S_xb, S_g = Slot(), Slot()
            for cc in range(8):
                zT_chunk(1024 + cc * 128, ubf, S_u, wb, S_wb)
                for q in range(4):
                    pr = cc * 4 + q
                    for tc in range(4):
                        for ri, v in ((0, vrT), (1, viT)):
                            k = nb(4, 8)
                            p0, p1, vv = ((0, 32, 0), (32, 64, 0), (64, 96, 0), (64, 128, 1))[q]
                            mm(ps[:, k, :],
                               Bsb[p0:p1, cc, vv, ri, :], ubf[p0:p1, tc * 512:(tc + 1) * 512],
                               True, True, [S_cb, S_u], [PS[k]])
                            act(v[:, tc * 32:(tc + 1) * 32, :], ps[:, k, :].rearrange("p (c j) -> p c j", j=L1), AF.Copy, [PS[k]],
                                S_vr if ri == 0 else S_vi)
                    ar_, ai_, nai_ = small[:, AR, pr:pr + 1], small[:, AI, pr:pr + 1], small[:, NAI, pr:pr + 1]
                    if mode == "B":
                        xpr, xpi = XSr[:, pr, 0:NC1], XSi[:, pr, 0:NC1]
                        RX = [S_small, S_xs]
                        stt(vr3[:, 0, :], xpi, nai_, vr3[:, 0, :], ALU.mult, ALU.add, RX + [S_vr[0]], [S_vr[0]])
                        stt(vi3[:, 0, :], xpr, ai_, vi3[:, 0, :], ALU.mult, ALU.add, RX + [S_vi[0]], [S_vi[0]])
                        stt(vr3[:, 0, :], xpr, ar_, vr3[:, 0, :], ALU.mult, ALU.add, RX + [S_vr[0]], [S_vr[0]])
                        stt(vi3[:, 0, :], xpi, ar_, vi3[:, 0, :], ALU.mult, ALU.add, RX + [S_vi[0]], [S_vi[0]])
                    for j in range(1, L1):
                        o1 = lambda: stt(vr3[:, j, :], vi3[:, j - 1, :], nai_, vr3[:, j, :], ALU.mult, ALU.add,
                                         [S_small, S_vi[j - 1], S_vr[j]], [S_vr[j]])
                        o2 = lambda: stt(vi3[:, j, :], vr3[:, j - 1, :], ai_, vi3[:, j, :], ALU.mult, ALU.add,
                                         [S_small, S_vr[j - 1], S_vi[j]], [S_vi[j]])
                        o3 = lambda: stt(vr3[:, j, :], vr3[:, j - 1, :], ar_, vr3[:, j, :], ALU.mult, ALU.add,
                                         [S_small, S_vr[j - 1], S_vr[j]], [S_vr[j]])
                        o4 = lambda: stt(vi3[:, j, :], vi3[:, j - 1, :], ar_, vi3[:, j, :], ALU.mult, ALU.add,
                                         [S_small, S_vi[j - 1], S_vi[j]], [S_vi[j]])
                        for f_ in ((o1, o2, o3, o4) if j % 2 == 1 else (o2, o1, o4, o3)):
                            f_()
                    if mode == "A":
                        P.op("dve", lambda pr=pr: nc.vector.tensor_copy(out=XSr[:, pr, 1:NC1 + 1], in_=vr3[:, L1 - 1, :]),
                             reads=[S_vr[L1 - 1]], writes=[S_xs])
                        P.op("dve", lambda pr=pr: nc.vector.tensor_copy(out=XSi[:, pr, 1:NC1 + 1], in_=vi3[:, L1 - 1, :]),
                             reads=[S_vi[L1 - 1]], writes=[S_xs])
                    else:
                        act(xbr.rearrange("p (c j) -> p c j", j=L1), vrT, AF.Copy, S_vr, [S_xb])
                        act(xbi.rearrange("p (c j) -> p c j", j=L1), viT, AF.Copy, S_vi, [S_xb])
                        for tc in range(4):
                            mm(ps[:, tc, :], Cb[:, pr, 0, :], xbr[:, tc * 512:(tc + 1) * 512], q == 0, False, [S_cb, S_xb], [PS[tc]])
                            mm(ps[:, tc, :], Cb[:, pr, 1, :], xbi[:, tc * 512:(tc + 1) * 512], False, q == 3, [S_cb, S_xb], [PS[tc]])
                if mode == "B":
                    for tc in range(4):
                        stt(gtmp, ubf[:, tc * 512:(tc + 1) * 512], ssmd[:, cc:cc + 1], ps[:, tc, :], ALU.mult, ALU.add,
                            [S_u, S_cb, PS[tc]], [S_g])
                        act(ys[:, cc, tc * 512:(tc + 1) * 512], gtmp, AF.Gelu, [S_g], [S_ys])
            reset(m)

        def level2():
            m = A.off
            t = A.f32(4 * 32).rearrange("p (a b) -> p a b", a=4)
            S_t = Slot()
            a16r, a16i = small[:, A16R, :], small[:, A16I, :]
            R = [S_xs, S_small, S_t]
            for c in range(NC1):
                tt(t[:, 0, :], a16r, XSr[:, :, c], ALU.mult, R, [S_t])
                tt(t[:, 1, :], a16i, XSi[:, :, c], ALU.mult, R, [S_t])
                tt(t[:, 2, :], a16r, XSi[:, :, c], ALU.mult, R, [S_t])
                tt(t[:, 3, :], a16i, XSr[:, :, c], ALU.mult, R, [S_t])
                tt(t[:, 0, :], t[:, 0, :], t[:, 1, :], ALU.subtract, R, [S_t])
                tt(t[:, 2, :], t[:, 2, :], t[:, 3, :], ALU.add, R, [S_t])
                tt(XSr[:, :, c + 1], XSr[:, :, c + 1], t[:, 0, :], ALU.add, R, [S_xs])
                tt(XSi[:, :, c + 1], XSi[:, :, c + 1], t[:, 2, :], ALU.add, R, [S_xs])
            reset(m)

        S_xprev, S_xcur = Slot("xprev"), Slot("xcur")
        norm_to_hT(x_prev, S_xprev, g1_bc, 1 * D, 0 * D)
        P.op("dve", lambda: nc.vector.memset(XSr[:, :, 0:1], 0.0), writes=[S_xs])
        P.op("dve", lambda: nc.vector.memset(XSi[:, :, 0:1], 0.0), writes=[S_xs])
        ssm_pass("A")
        level2()
        ts(carry[:, 0, :], XSr[:, :, NC1], flag[:, 0:1], None, ALU.mult, None, [S_xs, S_c], [S_halo])
        ts(carry[:, 1, :], XSi[:, :, NC1], flag[:, 0:1], None, ALU.mult, None, [S_xs, S_c], [S_halo])
        m = A.off
        wbp = A.bf16(16 * 128).rearrange("p (k n) -> p k n", k=16)
        S_wbp = Slot()
        for pc in range(8):
            dma("pool", wbp, w_in.rearrange("(k p) n -> p k n", p=128)[:, :, pc * 128:(pc + 1) * 128], writes=[S_wbp])
            k = nb(4, 8)
            for kc in range(16):
                mm(ps[:, k, 0:16], wbp[:, kc, :], hT[:, kc, T - 16:T], kc == 0, kc == 15, [S_wbp, S_hT], [PS[k]])
            ts(halo[:, pc, :], ps[:, k, 0:16], flag[:, 0:1], None, ALU.mult, None, [PS[k], S_c], [S_halo])
        reset(m)

        norm_to_hT(x_cur, S_xcur, g1_bc, 1 * D, 0 * D)
        P.op("dve", lambda: nc.vector.tensor_copy(out=XSr[:, :, 0], in_=carry[:, 0, :]), reads=[S_halo], writes=[S_xs])
        P.op("dve", lambda: nc.vector.tensor_copy(out=XSi[:, :, 0], in_=carry[:, 1, :]), reads=[S_halo], writes=[S_xs])
        ssm_pass("A")
        level2()
        if dbg:
            S_dump0 = Slot()
            dma("sp", xs_dump[:, 0], XSr, reads=[S_xs], writes=[S_dump0])
            dma("sp", xs_dump[:, 1], XSi, reads=[S_xs], writes=[S_dump0])
        ssm_pass("B")

        reset(mark_ys)
        pm2 = A.bf16(8 * T).rearrange("p (c t) -> p c t", c=8)
        S_pm2 = Slot("pm2")
        m = A.off
        wbp = A.bf16(16 * 128).rearrange("p (k n) -> p k n", k=16)
        pw = A.bf16(4 * 2 * 256).rearrange("p (g k d) -> p g k d", g=4, k=2)
        psc = A.f32(8)
        U = A.f32(T + 16)
        V = A.f32(T + 16)
        Wb = A.f32(T + 16)
        rc = A.f32(T)
        pooled = A.bf16(2 * T).rearrange("p (c t) -> p c t", c=2)
        S_wbp, S_pw, S_U, S_V, S_W, S_rc, S_pl = Slot(), Slot(), Slot(), Slot(), Slot(), Slot(), Slot()
        dma("pool", pw, pool_w_l, writes=[S_pw])
        dma("sp", psc, pscale_l, writes=[S_pw])
        N = T + 16
        for g in range(4):
            dma("sp", rc, rc_bc[:, g, :], writes=[S_rc])
            for gc in range(2):
                pc = g * 2 + gc
                zT_chunk(pc * 128, None, None, wbp, S_wbp, fp32_out=U, S_f=S_U)
                P.op("dve", lambda pc=pc: nc.vector.tensor_copy(out=U[:, 0:16], in_=halo[:, pc, :]), reads=[S_halo], writes=[S_U])
                src, ssrc = U, S_U
                bufs = [(V, S_V), (Wb, S_W)]
                sh = 1
                for lev in range(g + 1):
                    dst, sdst = bufs[lev % 2]
                    tt(dst[:, sh:N], src[:, sh:N], src[:, 0:N - sh], ALU.add, [ssrc], [sdst])
                    src, ssrc = dst, sdst
                    sh *= 2
                dst, sdst = bufs[(g + 1) % 2]
                tt(dst[:, 16:N], src[:, 16:N], rc, ALU.mult, [ssrc, S_rc], [sdst])
                tt(pooled[:, gc, :], dst[:, 16:N], U[:, 16:N], ALU.subtract, [sdst, S_U], [S_pl])
            for dch in range(2):
                for tc in range(4):
                    k = nb()
                    for kch in range(2):
                        mm(ps[:, k, :], pw[:, g, kch, dch * 128:(dch + 1) * 128], pooled[:, kch, tc * 512:(tc + 1) * 512],
                           kch == 0, kch == 1, [S_pw, S_pl], [PS[k]])
                    ts(pm2[:, g * 2 + dch, tc * 512:(tc + 1) * 512], ps[:, k, :], psc[:, g * 2 + dch:g * 2 + dch + 1], None,
                       ALU.mult, None, [PS[k], S_pw], [S_pm2])
        reset(m)

        m = A.off
        bglu = A.f32(32)
        S_bg = Slot()
        dma("sp", bglu, bglu_l, writes=[S_bg])
        wgp = A.bf16(16 * 256).rearrange("p (k n) -> p k n", k=16)
        wgs = A.bf16(16 * 256).rearrange("p (k n) -> p k n", k=16)
        wpo = A.bf16(8 * 256).rearrange("p (k n) -> p k n", k=8)
        wga = A.bf16(8 * 256).rearrange("p (k n) -> p k n", k=8)
        wgb = A.bf16(8 * 256).rearrange("p (k n) -> p k n", k=8)
        S_w3 = Slot()
        e_sgp, e_sgs, e_sgb, e_m1, e_ya = [A.f32(512) for _ in range(5)]
        mo = [A.bf16(512), A.bf16(512)]
        S_e = Slot()
        S_mo = [Slot(), Slot()]
        S_mT = Slot("mT")
        w_in_v = w_in.rearrange("(k p) n -> p k n", p=128)
        wpo_v = w_pool_out.rearrange("(k p) n -> p k n", p=128)
        wgl_v = w_glu.rearrange("(k p) n -> p k n", p=128)
        moi = 0
        for blk in range(8):
            c0 = blk * 256
            dma("pool", wgp, w_in_v[:, :, 2048 + c0:2048 + c0 + 256], writes=[S_w3])
            dma("pool", wgs, w_in_v[:, :, 4096 + c0:4096 + c0 + 256], writes=[S_w3])
            dma("pool", wpo, wpo_v[:, :, c0:c0 + 256], writes=[S_w3])
            dma("pool", wga, wgl_v[:, :, c0:c0 + 256], writes=[S_w3])
            dma("pool", wgb, wgl_v[:, :, 2048 + c0:2048 + c0 + 256], writes=[S_w3])
            for tc in range(4):
                tsl = slice(tc * 512, (tc + 1) * 512)
                for d2 in range(2):
                    dd = blk * 2 + d2
                    ws = slice(d2 * 128, (d2 + 1) * 128)
                    kgp, kgs, kyp, kga, kgb = nb(), nb(), nb(), nb(), nb()
                    for kc in range(16):
                        mm(ps[:, kgp, :], wgp[:, kc, ws], hT[:, kc, tsl], kc == 0, kc == 15, [S_w3, S_hT], [PS[kgp]])
                    for kc in range(16):
                        mm(ps[:, kgs, :], wgs[:, kc, ws], hT[:, kc, tsl], kc == 0, kc == 15, [S_w3, S_hT], [PS[kgs]])
                    for kc in range(8):
                        mm(ps[:, kyp, :], wpo[:, kc, ws], pm2[:, kc, tsl], kc == 0, kc == 7, [S_w3, S_pm2], [PS[kyp]])
                    for kc in range(8):
                        mm(ps[:, kga, :], wga[:, kc, ws], ys[:, kc, tsl], kc == 0, kc == 7, [S_w3, S_ys], [PS[kga]])
                    for kc in range(8):
                        mm(ps[:, kgb, :], wgb[:, kc, ws], ys[:, kc, tsl], kc == 0, kc == 7, [S_w3, S_ys], [PS[kgb]])
                    act(e_sgp, ps[:, kgp, :], AF.Sigmoid, [PS[kgp]], [S_e])
                    act(e_sgs, ps[:, kgs, :], AF.Sigmoid, [PS[kgs]], [S_e])
                    act(e_sgb, ps[:, kgb, :], AF.Sigmoid, [PS[kgb], S_bg], [S_e], bias=bglu[:, 16 + dd:17 + dd])
                    tt(e_m1, e_sgp, ps[:, kyp, :], ALU.mult, [S_e, PS[kyp]], [S_e])
                    stt(e_ya, ps[:, kga, :], bglu[:, dd:dd + 1], e_sgb, ALU.add, ALU.mult, [PS[kga], S_bg, S_e], [S_e])
                    tt(e_ya, e_ya, e_sgs, ALU.mult, [S_e], [S_e])
                    mb, smb = mo[moi % 2], S_mo[moi % 2]
                    moi += 1
                    tt(mb, e_m1, e_ya, ALU.add, [S_e], [smb])
                    dma("sp", mT_d[dd, :, tsl], mb, reads=[smb], writes=[S_mT])
        if dbg:
            S_dump = Slot()
            dma("sp", ys_dump, ys, reads=[S_ys], writes=[S_dump])
            dma("sp", pm2_dump, pm2, reads=[S_pm2], writes=[S_dump])
            dma("sp", hT_dump, hT, reads=[S_hT], writes=[S_dump])
        reset(m)

        m = A.off
        mTt = A.bf16(16 * 512).rearrange("p (k t) -> p k t", k=16)
        wo = A.bf16(16 * 512).rearrange("p (k n) -> p k n", k=16)
        gtm = A.f32(D)
        xs4 = [A.f32(512), A.f32(512)]
        x1p = [A.f32(512), A.f32(512)]
        S_mTt, S_wo, S_gtm = Slot(), Slot(), Slot()
        S_xs4 = [Slot(), Slot()]
        S_x1p = [Slot(), Slot()]
        S_x2 = Slot("x2")
        dma("sp", gtm, modbc[:, 2 * D:3 * D], reads=[S_mod], writes=[S_gtm])
        wo_v = w_out.rearrange("(k p) n -> p k n", p=128)
        ci = 0
        for tc in range(4):
            dma("sp", mTt, mT_d.rearrange("k p t -> p k t")[:, :, tc * 512:(tc + 1) * 512], reads=[S_mT], writes=[S_mTt])
            for cb in range(4):
                csl = slice(cb * 512, (cb + 1) * 512)
                dma("pool", wo, wo_v[:, :, csl], writes=[S_wo])
                for il in range(4):
                    i = tc * 4 + il
                    k = nb()
                    for kc in range(16):
                        mm(ps[:, k, :], mTt[:, kc, il * 128:(il + 1) * 128], wo[:, kc, :], kc == 0, kc == 15, [S_mTt, S_wo], [PS[k]])
                    xb_, sxb = xs4[ci % 2], S_xs4[ci % 2]
                    xo, sxo = x1p[ci % 2], S_x1p[ci % 2]
                    ci += 1
                    dma("sp", xb_, x_cur[i * 128:(i + 1) * 128, csl], writes=[sxb])
                    tt(xo, ps[:, k, :], gtm[:, csl], ALU.mult, [PS[k], S_gtm], [sxo])
                    tt(xo, xo, xb_, ALU.add, [sxo, sxb], [sxo])
                    dma("sp", x2_d[i * 128:(i + 1) * 128, csl], xo, reads=[sxo], writes=[S_x2])
        reset(m)

        reset(mark_low)
        gate = A.f32(NT * NE).rearrange("p (i e) -> p i e", i=NT)
        S_gate = Slot("gate")
        mark_gate = A.off
        wr = A.bf16(16 * NE).rearrange("p (k e) -> p k e", k=16)
        brow = A.bf16(NE)
        lg = A.f32(NE)
        m8 = A.f32(8)
        ngm = A.f32(2)
        msk = A.f32(NE)
        S_wr, S_lg = Slot(), Slot()
        dma("pool", wr, w_router.rearrange("(k p) e -> p k e", p=128), writes=[S_wr])
        dma("pool", brow[0:1, :], b_router, writes=[S_wr])

        def router(i):
            k = nb()
            mm(ps[:, k, 0:NE], ones_b[0:1, :], brow[0:1, :], True, False, [S_c, S_wr], [PS[k]])
            for kc in range(16):
                mm(ps[:, k, 0:NE], hT[:, kc, i * 128:(i + 1) * 128], wr[:, kc, :], False, kc == 15, [S_hT, S_wr], [PS[k]])
            P.op("dve", lambda: nc.vector.tensor_copy(out=lg, in_=ps[:, k, 0:NE]), reads=[PS[k]], writes=[S_lg])
            P.op("dve", lambda: nc.vector.max(out=m8, in_=lg), reads=[S_lg], writes=[S_lg])
            ts(msk, lg, m8[:, 3:4], None, ALU.is_ge, None, [S_lg], [S_lg])
            ts(ngm[:, 0:1], m8[:, 0:1], -1.0, None, ALU.mult, None, [S_lg], [S_lg])
            act(lg, lg, AF.Exp, [S_lg], [S_lg], bias=ngm[:, 0:1])
            tt(lg, lg, msk, ALU.mult, [S_lg], [S_lg])
            P.op("dve", lambda: nc.vector.reduce_sum(out=ngm[:, 1:2], in_=lg, axis=mybir.AxisListType.X), reads=[S_lg], writes=[S_lg])
            P.op("dve", lambda: nc.vector.reciprocal(out=ngm[:, 1:2], in_=ngm[:, 1:2]), reads=[S_lg], writes=[S_lg])
            ts(gate[:, i, :], lg, ngm[:, 1:2], None, ALU.mult, None, [S_lg], [S_gate])

        norm_to_hT(x2_d, S_x2, g2_bc, 4 * D, 3 * D, extra=router)
        reset(mark_gate)

        actT = A.bf16(NT * 16 * 128).rearrange("p (i f t) -> p i f t", i=NT, f=16)
        S_actT = Slot("actT")
        gtf = A.f32(D)
        S_gtf = Slot()
        dma("sp", gtf, modbc[:, 5 * D:6 * D], reads=[S_mod], writes=[S_gtf])
        w1buf = [(A.bf16(16 * 256).rearrange("p (k n) -> p k n", k=16), A.bf16(16 * 256).rearrange("p (k n) -> p k n", k=16))
                 for _ in range(2)]
        w2buf = [A.bf16(16 * 256).rearrange("p (k n) -> p k n", k=16) for _ in range(2)]
        S_w1b = [Slot(), Slot()]
        S_w2b = [Slot(), Slot()]
        b1r = A.bf16(2 * D)
        b2r = A.bf16(D)
        S_b1, S_b2 = Slot(), Slot()
        xg, sg_, xl = A.f32(256), A.f32(256), A.f32(256)
        ab = [A.bf16(256), A.bf16(256)]
        S_ev = Slot()
        S_ab = [Slot(), Slot()]
        NY = 4
        yst = [A.f32(256) for _ in range(NY)]
        S_yst = [Slot() for _ in range(NY)]
        S_x2r = [[Slot() for _ in range(8)] for _ in range(NT)]
        for i in range(NT):
            for d8 in range(8):
                S_x2r[i][d8].w = S_x2.w
        print("MoE arena words used", A.off, "of", W)
        blocks = []
        for e in range(0 if dbg else NE):
            for j in range(8):
                blocks.append(["w1", e, j, 0])
            for j in range(8):
                blocks.append(["w2", e, j, 0])
        pcnt = {"w1": 0, "w2": 0}

        def issue_load(bi):
            if bi >= len(blocks):
                return
            kind, e, j, _ = blocks[bi]
            par = pcnt[kind] % 2
            pcnt[kind] += 1
            blocks[bi][3] = par
            if kind == "w1":
                w1v = w1[e].rearrange("(k p) n -> p k n", p=128)
                if j == 0:
                    dma("pool", b1r[0:1, :], b1[e:e + 1, :], writes=[S_b1], cls="w")
                f0 = j * 256
                dma("pool", w1buf[par][0], w1v[:, :, f0:f0 + 256], writes=[S_w1b[par]], cls="w")
                dma("pool", w1buf[par][1], w1v[:, :, D + f0:D + f0 + 256], writes=[S_w1b[par]], cls="w")
            else:
                w2v = w2[e].rearrange("(k p) n -> p k n", p=128)
                if j == 0:
                    dma("pool", b2r[0:1, :], b2[e:e + 1, :], writes=[S_b2], cls="w")
                dma("pool", w2buf[par], w2v[:, :, j * 256:(j + 1) * 256], writes=[S_w2b[par]], cls="w")

        if dbg:
            dma("sp", gate_dump, gate, reads=[S_gate], writes=[Slot()])
        issue_load(0)
        yi = 0
        abi = 0
        for bi in range(len(blocks)):
            issue_load(bi + 1)
            kind, e, j, par = blocks[bi]
            if kind == "w1":
                fs, f0 = j, j * 256
                pend = None

                def flush(pend):
                    i_, ab_, sab_ = pend
                    k2 = nb()
                    pb = ps[:, k2, :].bitcast(BF16).rearrange("p (a b) -> p a b", a=8)
                    for jj in range(2):
                        P.op("pe", lambda pb=pb, jj=jj, ab_=ab_: nc.tensor.transpose(pb[:, jj, :], ab_[:, jj * 128:(jj + 1) * 128], ident),
                             reads=[sab_, S_c], writes=[PS[k2]])
                    act(actT[:, i_, fs * 2:fs * 2 + 2, :], pb[:, 0:2, :], AF.Copy, [PS[k2]], [S_actT])
                for i in range(NT):
                    k = nb()
                    for (wblk_, c0, b0) in ((w1buf[par][0], 0, f0), (w1buf[par][1], 256, D + f0)):
                        mm(ps[:, k, c0:c0 + 256], ones_b[0:1, :], b1r[0:1, b0:b0 + 256], True, False, [S_c, S_b1], [PS[k]])
                        for kc in range(16):
                            mm(ps[:, k, c0:c0 + 256], hT[:, kc, i * 128:(i + 1) * 128], wblk_[:, kc, :], False, kc == 15,
                               [S_hT, S_w1b[par]], [PS[k]])
                    ab_, sab_ = ab[abi % 2], S_ab[abi % 2]
                    abi += 1
                    ts(xg, ps[:, k, 0:256], 7.0, None, ALU.min, None, [PS[k]], [S_ev])
                    act(sg_, xg, AF.Sigmoid, [S_ev], [S_ev], scale=1.702)
                    ts(xl, ps[:, k, 256:512], 7.0, -7.0, ALU.min, ALU.max, [PS[k]], [S_ev])
                    stt(xl, xl, 1.0, xg, ALU.add, ALU.mult, [S_ev], [S_ev])
                    tt(ab_, xl, sg_, ALU.mult, [S_ev], [sab_])
                    if pend is not None:
                        flush(pend)
                    pend = (i, ab_, sab_)
                flush(pend)
            else:
                dsl = slice(j * 256, (j + 1) * 256)
                for i in range(NT):
                    k = nb()
                    mm(ps[:, k, 0:256], ones_b[0:1, :], b2r[0:1, dsl], True, False, [S_c, S_b2], [PS[k]])
                    for fc in range(16):
                        mm(ps[:, k, 0:256], actT[:, i, fc, :], w2buf[par][:, fc, :], False, fc == 15, [S_actT, S_w2b[par]], [PS[k]])
                    yb, syb = yst[yi % NY], S_yst[yi % NY]
                    yi += 1
                    stt(yb, ps[:, k, 0:256], gate[:, i, e:e + 1], gtf[:, dsl], ALU.mult, ALU.mult, [PS[k], S_gate, S_gtf], [syb])
                    dma("pool", x2_d[i * 128:(i + 1) * 128, dsl], yb, reads=[syb, S_x2r[i][j]], writes=[S_x2r[i][j]],
                        accum_op=ALU.add, cls="a")

        reset(mark_low)
        G3, S3, sl3 = load_GS(g3_bc, 7 * D, 6 * D)
        S_out = Slot()

        def fin_consume(i, h, sh):
            dma("sp", out_d[i * 128:(i + 1) * 128, :], h, reads=[sh], writes=[S_out])
        norm_rows(x2_d, G3, S3, sl3, (lambda i: S_x2r[i]), fin_consume)

        P.emit()
    return nc


def _prep_shared(inp):
    f = np.float32
    s = {}
    s["ident"] = np.eye(128, dtype=f)
    s["ada_w"] = np.ascontiguousarray(inp["ada_w"][0])
    s["ada_b_bc"] = np.ascontiguousarray(np.broadcast_to(inp["ada_b"][0][None, :], (128, 6 * D)))
    s["final_ada_w"] = np.ascontiguousarray(inp["final_ada_w"])
    s["final_ada_b_bc"] = np.ascontiguousarray(np.broadcast_to(inp["final_ada_b"][None, :], (128, 2 * D)))
    s["g1_bc"] = np.ascontiguousarray(np.broadcast_to(inp["norm1_g"][0][None, :], (128, D)))
    s["g2_bc"] = np.ascontiguousarray(np.broadcast_to(inp["norm2_g"][0][None, :], (128, D)))
    s["g3_bc"] = np.ascontiguousarray(np.broadcast_to(inp["final_norm_g"][None, :], (128, D)))
    s["w_in"] = np.ascontiguousarray(inp["w_in"][0])
    pw = inp["pool_w"][0]
    s["pool_w_l"] = np.ascontiguousarray(pw.reshape(4, 2, 128, 256).transpose(2, 0, 1, 3))
    s["pscale_l"] = np.ascontiguousarray(inp["pool_scale"][0].reshape(8, 128).T)
    s["w_pool_out"] = np.ascontiguousarray(inp["w_pool_out"][0])

    def pairl(a):
        return np.ascontiguousarray(a.reshape(32, 2, 64).transpose(1, 2, 0).reshape(128, 32))
    s["lamre_l"] = pairl(inp["ssm_lam_re"][0])
    s["lamim_l"] = pairl(inp["ssm_lam_im"][0])
    s["logdt_l"] = pairl(np.repeat(inp["ssm_log_dt"][0][:, None], 64, axis=1))
    bre, bim = inp["ssm_b_re"][0], inp["ssm_b_im"][0]
    Bl = np.zeros((128, 8, 2, 2, 128), f)
    cre, cim = inp["ssm_c_re"][0], inp["ssm_c_im"][0]
    Cr = np.zeros((128, 32, 128), f)
    Ci = np.zeros((128, 32, 128), f)
    for g in range(64):
        pr, gl = g // 2, g % 2
        cc, q = pr // 4, pr % 4
        r0 = 32 * q + 16 * gl
        vv = 1 if q == 3 else 0
        Bl[r0:r0 + 16, cc, vv, 0, gl * 64:(gl + 1) * 64] = bre[g].T
        Bl[r0:r0 + 16, cc, vv, 1, gl * 64:(gl + 1) * 64] = bim[g].T
        m0 = 32 * q + 16 * gl
        Cr[gl * 64:(gl + 1) * 64, pr, m0:m0 + 16] = cre[g].T
        Ci[gl * 64:(gl + 1) * 64, pr, m0:m0 + 16] = cim[g].T
    s["B_l"], s["CTre_l"], s["CTim_l"] = Bl, Cr, Ci
    s["ssmd_l"] = np.ascontiguousarray(inp["ssm_d"][0].reshape(8, 128).T)
    s["w_glu"] = np.ascontiguousarray(inp["w_glu"][0])
    s["bglu_l"] = np.ascontiguousarray(inp["b_glu"][0].reshape(32, 128).T)
    s["w_out"] = np.ascontiguousarray(inp["w_out"][0])
    s["w_router"] = np.ascontiguousarray(inp["w_router"][0])
    s["b_router"] = np.ascontiguousarray(inp["b_router"][0][None, :])
    s["w1"] = np.ascontiguousarray(inp["w1"][0])
    s["b1"] = np.ascontiguousarray(inp["b1"][0])
    s["w2"] = np.ascontiguousarray(inp["w2"][0])
    s["b2"] = np.ascontiguousarray(inp["b2"][0])
    return s


def kernel(**inp):
    inp = {k: np.asarray(v) for k, v in inp.items()}
    f = np.float32
    shared = _prep_shared(inp)
    x, c = inp["x"], inp["c"]
    in_maps = []
    win = np.array([2.0, 4.0, 8.0, 16.0], f)
    for core in range(8):
        b, half = core // 2, core % 2
        mp = dict(shared)
        mp["x_cur"] = np.ascontiguousarray(x[b, half * T:(half + 1) * T])
        mp["x_prev"] = np.ascontiguousarray(x[b, 0:T]) if half == 1 else np.zeros((T, D), f)
        mp["flag"] = np.full((128, 1), float(half), f)
        mp["cT"] = np.ascontiguousarray(c[b].reshape(16, 128).T)
        pos = np.arange(1, T + 1, dtype=f) + (T if half == 1 else 0)
        rc = (1.0 / np.minimum(pos[None, :], win[:, None])).astype(f)
        mp["rc_bc"] = np.ascontiguousarray(np.broadcast_to(rc[None], (128, 4, T)))
        in_maps.append(mp)
    nc = build()
    res = run_bass_kernel_spmd(nc, in_maps, core_ids=list(range(8)))
    out = np.zeros((4, 2 * T, D), f)
    for core in range(8):
        b, half = core // 2, core % 2
        out[b, half * T:(half + 1) * T] = res.results[core]["out"]
    return out
```
